# Optimizing a Trainium2 kernel written in Bass

```python
import jax, jax.numpy as jnp
from jax import lax
import numpy as np

D_MODEL = 1024
BATCH = 4
SEQ = 4096
DEPTH = 2

EPS = 1e-6
CHUNK = 128
GMLP_GROUPS = 4
GMLP_HEAD = 64
GMLP_WIDTH = GMLP_GROUPS * GMLP_HEAD
FOX_HEADS = 12
HEAD_DIM = 64
FOX_WIDTH = FOX_HEADS * HEAD_DIM
Q_BLOCK = 128
EVEN_SPLITS = (GMLP_WIDTH, 2 * GMLP_WIDTH, 2 * GMLP_WIDTH + FOX_WIDTH,
               2 * GMLP_WIDTH + 2 * FOX_WIDTH, 2 * GMLP_WIDTH + 3 * FOX_WIDTH)
EVEN_PROJ = 2 * GMLP_WIDTH + 3 * FOX_WIDTH + FOX_HEADS
EVEN_MIX = GMLP_WIDTH + FOX_WIDTH
CONV_WIDTH = 512
CONV_K = 3
POOL_WINDOWS = (2, 4, 8, 16)
POOL_GROUPS = 4
POOL_GROUP = 128
POOL_WIDTH = POOL_GROUPS * POOL_GROUP
ODD_SPLITS = (CONV_WIDTH, 2 * CONV_WIDTH, 3 * CONV_WIDTH)
ODD_PROJ = 3 * CONV_WIDTH + POOL_WIDTH
ODD_MIX = CONV_WIDTH + POOL_WIDTH
N_GROUPS = 4
EXPERTS_PER_GROUP = 4
N_EXPERTS = N_GROUPS * EXPERTS_PER_GROUP
EXPERT_HIDDEN = 256
TOP_K_INNER = 2

kernel_name = "hybrid_gmlp_fox_conv_pool_hmoe"


def rms_norm(x, g):
    xf = x.astype(jnp.float32)
    y = xf * lax.rsqrt(jnp.mean(xf * xf, axis=-1, keepdims=True) + EPS)
    return (y * g.astype(jnp.float32)).astype(x.dtype)


def gmlp_mixer(u, v, w_s, b_s, g_v):
    bsz, s_len, _ = u.shape
    n_chunks = s_len // CHUNK
    u = jax.nn.gelu(u)
    v = rms_norm(jax.nn.gelu(v).reshape(bsz, s_len, GMLP_GROUPS, GMLP_HEAD), g_v)
    v = v.reshape(bsz, n_chunks, CHUNK, GMLP_GROUPS, GMLP_HEAD)
    w = w_s * jnp.tril(jnp.ones((CHUNK, CHUNK), dtype=w_s.dtype))
    s = jnp.einsum('gts,bcsgd->bctgd', w, v) + b_s.T[None, None, :, :, None]
    return u * s.reshape(bsz, s_len, GMLP_WIDTH)


def fox_attention(q, k, v, f_logit, g_q, g_k):
    bsz, s_len = q.shape[0], q.shape[1]
    q = rms_norm(q, g_q).transpose(0, 2, 1, 3)
    k = rms_norm(k, g_k).transpose(0, 2, 1, 3)
    v = v.transpose(0, 2, 1, 3)
    log_f = jax.nn.log_sigmoid(f_logit.astype(jnp.float32))
    cum_f = jnp.cumsum(log_f, axis=1).transpose(0, 2, 1)
    scale = HEAD_DIM ** -0.5
    outs = []
    for blk in range(s_len // Q_BLOCK):
        q0 = blk * Q_BLOCK
        q1 = q0 + Q_BLOCK
        logits = jnp.einsum('bhqd,bhkd->bhqk', q[:, :, q0:q1], k[:, :, :q1]).astype(jnp.float32) * scale
        logits = logits + cum_f[:, :, q0:q1, None] - cum_f[:, :, None, :q1]
        causal = jnp.arange(q1)[None, :] <= jnp.arange(q0, q1)[:, None]
        logits = jnp.where(causal, logits, -jnp.inf)
        p = jax.nn.softmax(logits, axis=-1).astype(v.dtype)
        outs.append(jnp.einsum('bhqk,bhkd->bhqd', p, v[:, :, :q1]))
    o = jnp.concatenate(outs, axis=2)
    return o.transpose(0, 2, 1, 3).reshape(bsz, s_len, FOX_WIDTH)


def even_mixer(xn, w_in, b_forget, w_s, b_s, g_v, g_q, g_k, w_out):
    bsz, s_len, _ = xn.shape
    h = xn @ w_in
    u, v, q, k, val, f = jnp.split(h, EVEN_SPLITS, axis=-1)
    a = gmlp_mixer(u, v, w_s, b_s, g_v)
    hs = (bsz, s_len, FOX_HEADS, HEAD_DIM)
    b = fox_attention(q.reshape(hs), k.reshape(hs), val.reshape(hs), f + b_forget, g_q, g_k)
    return jnp.concatenate([a, b], axis=-1) @ w_out


def short_conv(bg, cg, hc, conv_w):
    s_len = hc.shape[1]
    z = cg * hc
    zp = jnp.pad(z, ((0, 0), (CONV_K - 1, 0), (0, 0)))
    y = conv_w[0] * zp[:, 0:s_len]
    for tap in range(1, CONV_K):
        y = y + conv_w[tap] * zp[:, tap:tap + s_len]
    return bg * y


def pool_mixer(p, w_pool, pool_scale):
    bsz, s_len, _ = p.shape
    pf = p.astype(jnp.float32).reshape(bsz, s_len, POOL_GROUPS, POOL_GROUP)
    cs = jnp.concatenate([jnp.zeros_like(pf[:, :1]), jnp.cumsum(pf, axis=1)], axis=1)
    hi = jnp.arange(1, s_len + 1)
    groups = []
    for gi, win in enumerate(POOL_WINDOWS):
        lo = jnp.maximum(hi - win, 0)
        cnt = (hi - lo).astype(jnp.float32)
        mean = (cs[:, hi, gi] - cs[:, lo, gi]) / cnt[None, :, None]
        groups.append(mean - pf[:, :, gi])
    pooled = jnp.stack(groups, axis=2).astype(p.dtype)
    y = jnp.einsum('bsgc,gcd->bsgd', pooled, w_pool).reshape(bsz, s_len, POOL_WIDTH)
    return y * pool_scale


def odd_mixer(xn, w_in, conv_w, w_pool, pool_scale, w_out):
    h = xn @ w_in
    bg, cg, hc, p = jnp.split(h, ODD_SPLITS, axis=-1)
    c = short_conv(bg, cg, hc, conv_w)
    d = pool_mixer(p, w_pool, pool_scale)
    return jnp.concatenate([c, d], axis=-1) @ w_out


def hier_moe(xn, w_group, b_group, w_router, b_router, w_gate, w_up, w_down):
    bsz, s_len, d = xn.shape
    xt = xn.reshape(bsz * s_len, d)
    g_prob = jax.nn.softmax((xt @ w_group).astype(jnp.float32) + b_group.astype(jnp.float32), axis=-1)
    g_top_p, g_idx = lax.top_k(g_prob, 1)
    e_logits_all = jnp.einsum('td,dge->tge', xt, w_router).astype(jnp.float32) + b_router.astype(jnp.float32)
    g_sel = jax.nn.one_hot(g_idx[:, 0], N_GROUPS, dtype=jnp.float32)
    e_logits = jnp.einsum('tge,tg->te', e_logits_all, g_sel)
    e_prob = jax.nn.softmax(e_logits, axis=-1)
    e_top_p, e_idx = lax.top_k(e_prob, TOP_K_INNER)
    e_top_p = e_top_p / jnp.sum(e_top_p, axis=-1, keepdims=True)
    weights = g_top_p * e_top_p
    expert_id = g_idx * EXPERTS_PER_GROUP + e_idx
    combine = jnp.einsum('tk,tke->te', weights, jax.nn.one_hot(expert_id, N_EXPERTS, dtype=jnp.float32))
    hid = jax.nn.silu(jnp.einsum('td,edh->teh', xt, w_gate)) * jnp.einsum('td,edh->teh', xt, w_up)
    hid = hid * combine[:, :, None].astype(hid.dtype)
    y = jnp.einsum('teh,ehd->td', hid, w_down)
    return y.reshape(bsz, s_len, d)


def setup_inputs(seed: int = 0) -> dict:
    key = jax.random.key(seed)
    ks = iter(jax.random.split(key, 32))
    n_even = (DEPTH + 1) // 2
    n_odd = DEPTH // 2
    f32 = jnp.float32

    def nrm(shape, scale):
        return jax.random.normal(next(ks), shape, f32) * scale

    def gain(shape):
        return 1.0 + 0.05 * jax.random.normal(next(ks), shape, f32)

    return {
        "x": jax.random.normal(next(ks), (BATCH, SEQ, D_MODEL), f32),
        "ev_norm": gain((n_even, D_MODEL)),
        "ev_w_in": nrm((n_even, D_MODEL, EVEN_PROJ), D_MODEL ** -0.5),
        "ev_b_forget": jax.random.uniform(next(ks), (n_even, FOX_HEADS), f32, 1.0, 5.0),
        "ev_w_s": nrm((n_even, GMLP_GROUPS, CHUNK, CHUNK), CHUNK ** -0.5),
        "ev_b_s": gain((n_even, GMLP_GROUPS, CHUNK)),
        "ev_g_v": gain((n_even, GMLP_GROUPS, GMLP_HEAD)),
        "ev_g_q": gain((n_even, HEAD_DIM)),
        "ev_g_k": gain((n_even, HEAD_DIM)),
        "ev_w_out": nrm((n_even, EVEN_MIX, D_MODEL), EVEN_MIX ** -0.5),
        "od_norm": gain((n_odd, D_MODEL)),
        "od_w_in": nrm((n_odd, D_MODEL, ODD_PROJ), D_MODEL ** -0.5),
        "od_conv_w": nrm((n_odd, CONV_K, CONV_WIDTH), CONV_K ** -0.5),
        "od_w_pool": nrm((n_odd, POOL_GROUPS, POOL_GROUP, POOL_GROUP), POOL_GROUP ** -0.5),
        "od_pool_scale": gain((n_odd, POOL_WIDTH)),
        "od_w_out": nrm((n_odd, ODD_MIX, D_MODEL), ODD_MIX ** -0.5),
        "moe_norm": gain((DEPTH, D_MODEL)),
        "moe_w_group": nrm((DEPTH, D_MODEL, N_GROUPS), D_MODEL ** -0.5),
        "moe_b_group": nrm((DEPTH, N_GROUPS), 0.01),
        "moe_w_router": nrm((DEPTH, D_MODEL, N_GROUPS, EXPERTS_PER_GROUP), D_MODEL ** -0.5),
        "moe_b_router": nrm((DEPTH, N_GROUPS, EXPERTS_PER_GROUP), 0.01),
        "moe_w_gate": nrm((DEPTH, N_EXPERTS, D_MODEL, EXPERT_HIDDEN), D_MODEL ** -0.5),
        "moe_w_up": nrm((DEPTH, N_EXPERTS, D_MODEL, EXPERT_HIDDEN), D_MODEL ** -0.5),
        "moe_w_down": nrm((DEPTH, N_EXPERTS, EXPERT_HIDDEN, D_MODEL), EXPERT_HIDDEN ** -0.5),
    }


def reference(x, ev_norm, ev_w_in, ev_b_forget, ev_w_s, ev_b_s, ev_g_v, ev_g_q, ev_g_k, ev_w_out,
              od_norm, od_w_in, od_conv_w, od_w_pool, od_pool_scale, od_w_out,
              moe_norm, moe_w_group, moe_b_group, moe_w_router, moe_b_router,
              moe_w_gate, moe_w_up, moe_w_down):
    for layer in range(DEPTH):
        j = layer // 2
        if layer % 2 == 0:
            x = x + even_mixer(rms_norm(x, ev_norm[j]), ev_w_in[j], ev_b_forget[j], ev_w_s[j], ev_b_s[j],
                               ev_g_v[j], ev_g_q[j], ev_g_k[j], ev_w_out[j])
        else:
            x = x + odd_mixer(rms_norm(x, od_norm[j]), od_w_in[j], od_conv_w[j], od_w_pool[j],
                              od_pool_scale[j], od_w_out[j])
        x = x + hier_moe(rms_norm(x, moe_norm[layer]), moe_w_group[layer], moe_b_group[layer],
                         moe_w_router[layer], moe_b_router[layer], moe_w_gate[layer],
                         moe_w_up[layer], moe_w_down[layer])
    return x
```

```python
import contextlib
import os
CUT = int(os.environ.get('KCUT', '99'))
KSUB = int(os.environ.get('KSUB', '0'))
import numpy as np
import concourse.bass as bass
import concourse.mybir as mybir
from concourse.bass_utils import run_bass_kernel_spmd

F32 = mybir.dt.float32
BF16 = mybir.dt.bfloat16
AF = mybir.ActivationFunctionType
ALU = mybir.AluOpType
AX = mybir.AxisListType

EPOCH = 28000
NT = 17
NPRE = 15
NSEQ = 32
D = 1024
EPS = 1e-6
NEG = -30000.0


class Dep:
    __slots__ = ("w", "rs")

    def __init__(self):
        self.w = None
        self.rs = {}


class Eng:
    def __init__(self, kb, eng, name, sync_self):
        self.kb = kb
        self.eng = eng
        self.name = name
        self.sync_self = sync_self
        self.sem = kb.nc.alloc_semaphore(name=f"s_{name}_0")
        self.nep = 0
        self.cnt = 0
        self.waited = {}
        self.nins = 0
        self.allsems = [self.sem]

    def _collect(self, reads, writes):
        need = {}

        def add(ev):
            if ev is None:
                return
            sem, val, src = ev
            if src is self and not self.sync_self:
                return
            k = id(sem)
            if self.waited.get(k, 0) >= val:
                return
            if k not in need or need[k][1] < val:
                need[k] = (sem, val)

        for d in reads:
            add(d.w)
        for d in writes:
            add(d.w)
            for ev in d.rs.values():
                add(ev)
        return list(need.values())

    def op(self, fn, r=(), w=(), dma=False):
        items = self._collect(r, w)
        if dma:
            sem, val, prev = self.kb.dma_slot("sw" if self.name == "pool" else "hw")
            if prev > 0 and self.waited.get(id(sem), 0) < prev:
                items.append((sem, prev))
        for (s, v) in items[1:]:
            self.eng.wait_ge(s, v)
            self.waited[id(s)] = max(self.waited.get(id(s), 0), v)
        ins = fn(self.eng)
        if items:
            s, v = items[0]
            ins._wait_ge(s, v)
            self.waited[id(s)] = max(self.waited.get(id(s), 0), v)
        self.nins += 1
        if dma:
            ins.then_inc(sem, 16)
            ev = (sem, val, None)
        else:
            if self.cnt >= EPOCH:
                self.nep += 1
                self.sem = self.kb.nc.alloc_semaphore(name=f"s_{self.name}_{self.nep}")
                self.allsems.append(self.sem)
                self.cnt = 0
            self.cnt += 1
            ins.then_inc(self.sem, 1)
            ev = (self.sem, self.cnt, self)
        for d in r:
            d.rs[id(ev[0])] = ev
        for d in w:
            d.w = ev
            d.rs = {}
        return ev

    def wait_sv(self, sem, val):
        if val <= 0 or self.waited.get(id(sem), 0) >= val:
            return
        self.eng.wait_ge(sem, val)
        self.waited[id(sem)] = val


class KB:
    def __init__(self, nc, n_dma_sems=48):
        self.nc = nc
        self.pe = Eng(self, nc.tensor, "pe", False)
        self.act = Eng(self, nc.scalar, "act", True)
        self.dve = Eng(self, nc.vector, "dve", True)
        self.pool = Eng(self, nc.gpsimd, "pool", True)
        self.sp = Eng(self, nc.sync, "sp", True)
        self.engs = [self.pe, self.act, self.dve, self.pool, self.sp]
        self.dsems_hw = [[nc.alloc_semaphore(name=f"dmah{i}"), 0] for i in range(32)]
        self.dsems_sw = [[nc.alloc_semaphore(name=f"dmas{i}"), 0] for i in range(24)]
        self.dsems = self.dsems_hw + self.dsems_sw
        self.dnext = {"hw": 0, "sw": 0}
        self.nt = 0

    def dma_slot(self, kind):
        lst = self.dsems_hw if kind == "hw" else self.dsems_sw
        slot = lst[self.dnext[kind]]
        self.dnext[kind] = (self.dnext[kind] + 1) % len(lst)
        prev = slot[1]
        slot[1] += 16
        return slot[0], slot[1], prev

    def barrier(self, engs=None):
        for e in (engs or self.engs):
            for x in self.engs:
                if x is e:
                    continue
                e.wait_sv(x.sem, x.cnt)
            for sem, val in self.dsems:
                e.wait_sv(sem, val)


class T:
    def __init__(self, t):
        self.t = t
        self.d = Dep()

    def __getitem__(self, k):
        return self.t[k]


class Scope:
    def __init__(self, kb):
        self.kb = kb
        self.st = contextlib.ExitStack()

    def sb(self, shape, dtype, name=None):
        self.kb.nt += 1
        t = self.st.enter_context(self.kb.nc.sbuf_tensor(f"sb{self.kb.nt}_{name or 't'}", list(shape), dtype))
        return T(t)

    def close(self):
        self.kb.barrier()
        self.st.close()


def build(layers=(0, 1), dbg=None):
    nc = bass.Bass("TRN2", target_bir_lowering=False)
    kb = KB(nc)
    pe, act, dve, pool, sp = kb.pe, kb.act, kb.dve, kb.pool, kb.sp
    L0 = 0 in layers
    L1 = 1 in layers

    def din(name, shape, dt=F32):
        return nc.dram_tensor(name, list(shape), dt, kind="ExternalInput").ap()

    xo_d = din("xo", [NT * 128, D])
    xp_d = din("xp", [NPRE * 128, D])
    cst_d = din("cst", [128, 640])
    selall_d = din("selall", [16, 2048])
    pc_d = din("pc", [128, 68])
    sm_d = din("smalls", [128, 454])
    gains_d = din("gains", [4, 128, D])
    ev_w_in_d = din("ev_w_in", [D, 2828])
    ev_wsT_d = din("ev_wsT", [4, 128, 128])
    ev_w_out_d = din("ev_w_out", [D, D])
    od_w_in_d = din("od_w_in", [D, 2048])
    od_w_pool_d = din("od_w_pool", [4, 128, 128])
    od_w_out_d = din("od_w_out", [D, D])
    wr_d = din("wr", [2, D, 20])
    wg_d = din("moe_w_gate", [2, 16, D, 256])
    wu_d = din("moe_w_up", [2, 16, D, 256])
    wd_d = din("moe_w_down", [2, 16, 256, D])
    if L1:
        out_d = nc.dram_tensor("out", [16 * 128, D], F32, kind="ExternalOutput").ap()
    else:
        out_d = nc.dram_tensor("out", [NT * 128, D], F32, kind="ExternalOutput").ap()
    dbg_d = None
    if dbg:
        dbg_d = nc.dram_tensor("dbg", [NT * 128, D], F32, kind="ExternalOutput").ap()
    kT_d = T(nc.dram_tensor("kT_scr", [12, 70, NSEQ * 128], BF16).ap())
    qT_d = T(nc.dram_tensor("qT_scr", [12, 70, NT * 128], BF16).ap())
    V_d = T(nc.dram_tensor("V_scr", [NSEQ, 128, 780], BF16).ap())

    pbig = [nc.alloc_psum_tensor(f"bankpair{i}", [128, 1024], F32) for i in range(4)]
    pb = [T(pbig[i // 2][:, (i % 2) * 512:(i % 2 + 1) * 512]) for i in range(8)]

    def pbf(i):
        return pb[i][:].bitcast(BF16).rearrange("p (a b) -> p a b", b=128)

    G = Scope(kb)
    xres = G.sb([128, NT, D], F32, "xres")
    xres_d = [Dep() for _ in range(NT)]
    cst32 = G.sb([128, 640], F32, "cst32")
    cstb = G.sb([128, 640], BF16, "cstb")
    pc = G.sb([128, 68], F32, "pc")
    sm = G.sb([128, 454], F32, "smalls_sb")
    ident_b = cstb[:, 0:128]
    maskT_b = cstb[:, 384:512]
    shift_b = cstb[0:64, 512:640]
    ident_f = cst32[:, 0:128]
    U_f = cst32[:, 128:256]
    sel127_f = cst32[:, 256:384]
    ones_f = G.sb([128, 128], F32, "ones_f")
    bfb = sm[:, 0:12]
    gq_b = sm[:, 12:76]
    gk_b = sm[:, 76:140]
    gvcol = sm[:, 140:142]
    bsl = sm[:, 142:398]
    cw = sm[:, 398:410]
    pscale = sm[:, 410:414]
    brt = sm[:, 414:454]
    valid = pc[:, 0:1]
    nvalid = pc[:, 1:2]
    icnt = pc[:, 4:68]

    sp.op(lambda e: e.dma_start(out=cst32[:], in_=cst_d), w=[cst32.d], dma=True)
    pool.op(lambda e: e.dma_start(out=cstb[:], in_=cst_d), w=[cstb.d], dma=True)
    sp.op(lambda e: e.dma_start(out=pc[:], in_=pc_d), w=[pc.d], dma=True)
    sp.op(lambda e: e.dma_start(out=sm[:], in_=sm_d), w=[sm.d], dma=True)
    dve.op(lambda e: e.memset(ones_f[:], 1.0), w=[ones_f.d])

    def load_xres():
        for j in range(NT):
            sp.op(lambda e: e.dma_start(out=xres[:, j, :], in_=xo_d[j * 128:(j + 1) * 128, :]), w=[xres_d[j]], dma=True)

    def rstd_from_ss(S, ss, n, scale, width):
        dve.op(lambda e: e.tensor_scalar(out=ss[:, 0:width], in0=ss[:, 0:width], scalar1=1.0 / n, scalar2=EPS,
                                         op0=ALU.mult, op1=ALU.add), r=[ss.d], w=[ss.d])
        act.op(lambda e: e.activation(out=ss[:, 0:width], in_=ss[:, 0:width], func=AF.Ln), r=[ss.d], w=[ss.d])
        act.op(lambda e: e.activation(out=ss[:, 0:width], in_=ss[:, 0:width], func=AF.Exp, scale=-0.5), r=[ss.d], w=[ss.d])

    def nt_a(xsrc_ap, xsrc_dep, gn, ss, xs):
        act.op(lambda e: e.activation(out=xs[:], in_=xsrc_ap, func=AF.Square, accum_out=ss[:, 0:1]),
               r=[xsrc_dep], w=[xs.d, ss.d])
        rstd_from_ss(None, ss, D, 1.0, 1)
        dve.op(lambda e: e.scalar_tensor_tensor(out=xs[:], in0=xsrc_ap, scalar=ss[:, 0:1], in1=gn[:],
                                                op0=ALU.mult, op1=ALU.mult), r=[xsrc_dep, ss.d, gn.d], w=[xs.d])

    def nt_pe(xs, bank):
        pv = pbf(bank)
        for c in range(8):
            pe.op(lambda e: e.transpose(out=pv[:, c, :], in_=xs[:, c * 128:(c + 1) * 128], identity=ident_b),
                  r=[xs.d, cstb.d], w=[pb[bank].d])

    def nt_copy(dstT_ap, dst_dep, bank, on_dve=False):
        if on_dve:
            dve.op(lambda e: e.tensor_copy(out=dstT_ap, in_=pbf(bank)), r=[pb[bank].d], w=[dst_dep])
        else:
            act.op(lambda e: e.copy(out=dstT_ap, in_=pbf(bank)), r=[pb[bank].d], w=[dst_dep])

    def norm_transpose(S, xsrc_ap, xsrc_dep, gn, junk, ss, xs, dstT_ap, dst_dep, bank):
        nt_a(xsrc_ap, xsrc_dep, gn, ss, xs)
        nt_pe(xs, bank)
        nt_copy(dstT_ap, dst_dep, bank)

    def nt_sequence(tile_src, n, gn, ss2, xs2, dst, after=None):
        a0, d0 = tile_src(0)
        nt_a(a0, d0, gn, ss2[0], xs2[0])
        for i in range(n):
            bank = 0 if i % 2 == 0 else 2
            nt_pe(xs2[i % 2], bank)
            if i + 1 < n:
                a1, d1 = tile_src(i + 1)
                nt_a(a1, d1, gn, ss2[(i + 1) % 2], xs2[(i + 1) % 2])
            da, dd = dst(i)
            nt_copy(da, dd, bank, on_dve=(i % 2 == 0))
            if after is not None and i >= 1:
                after(i - 1)
        if after is not None:
            after(n - 1)

    def load_w_bf16(dst, src_ap, nchunk, ncols, colsplit=1024):
        for c0 in range(0, ncols, colsplit):
            c1 = min(ncols, c0 + colsplit)
            pool.op(lambda e: e.dma_start(out=dst[:, :, c0:c1],
                                          in_=src_ap[:, c0:c1].rearrange("(c p) n -> p c n", p=128)),
                    w=[dst.d], dma=True)

    def dbg_dump():
        if dbg_d is None:
            return
        for j in range(NT):
            sp.op(lambda e: e.dma_start(out=dbg_d[j * 128:(j + 1) * 128, :], in_=xres[:, j, :]), r=[xres_d[j]], dma=True)

    def moe_layer(l, tiles, on_final=None):
        t0 = tiles[0]
        ntl = len(tiles)
        ntok = ntl * 128
        S = Scope(kb)
        gn = S.sb([128, D], F32)
        sp.op(lambda e: e.dma_start(out=gn[:], in_=gains_d[(1 if l == 0 else 3)]), w=[gn.d], dma=True)
        wr = S.sb([128, 8, 20], BF16)
        pool.op(lambda e: e.dma_start(out=wr[:], in_=wr_d[l].rearrange("(c p) n -> p c n", p=128)), w=[wr.d], dma=True)
        xnT = S.sb([128, 8, ntok], BF16)
        xnT_d = [Dep() for _ in range(ntl)]
        lg = S.sb([128, ntl, 20], F32)
        ss2 = [S.sb([128, 4], F32) for _ in range(2)]
        xs2 = [S.sb([128, D], BF16) for _ in range(2)]
        wg = [S.sb([128, 8, 256], BF16) for _ in range(2)]
        wu = [S.sb([128, 8, 256], BF16) for _ in range(2)]
        wd = [S.sb([128, 2, D], BF16) for _ in range(2)]

        def load_expert(e_):
            b = e_ % 2
            pool.op(lambda e: e.dma_start(out=wg[b][:], in_=wg_d[l, e_].rearrange("(c p) n -> p c n", p=128)), w=[wg[b].d], dma=True)
            pool.op(lambda e: e.dma_start(out=wu[b][:], in_=wu_d[l, e_].rearrange("(c p) n -> p c n", p=128)), w=[wu[b].d], dma=True)
            pool.op(lambda e: e.dma_start(out=wd[b][:], in_=wd_d[l, e_].rearrange("(c p) n -> p c n", p=128)), w=[wd[b].d], dma=True)

        load_expert(0)
        load_expert(1)
        def router_(i):
            for c in range(8):
                pe.op(lambda e: e.matmul(pb[1][:, 0:20], lhsT=xnT[:, c, i * 128:(i + 1) * 128], rhs=wr[:, c, :],
                                         start=(c == 0), stop=(c == 7)), r=[xnT_d[i], wr.d], w=[pb[1].d])
            dve.op(lambda e: e.tensor_tensor(out=lg[:, i, :], in0=pb[1][:, 0:20], in1=brt[:, l * 20:(l + 1) * 20], op=ALU.add),
                   r=[pb[1].d, sm.d], w=[lg.d])
        nt_sequence(lambda i: (xres[:, tiles[i], :], xres_d[tiles[i]]), ntl, gn, ss2, xs2,
                    lambda i: (xnT[:, :, i * 128:(i + 1) * 128], xnT_d[i]), after=router_)
        n4 = [128, ntl, 4]
        gmax = S.sb([128, ntl], F32)
        ge = S.sb(n4, F32)
        gsum = S.sb([128, ntl], F32)
        gsel = S.sb(n4, F32)
        tmp16 = S.sb([128, ntl, 16], F32)
        el = S.sb(n4, F32)
        emax = S.sb([128, ntl], F32)
        ee = S.sb(n4, F32)
        oh1 = S.sb(n4, F32)
        ee2 = S.sb(n4, F32)
        m2 = S.sb([128, ntl], F32)
        oh2 = S.sb(n4, F32)
        w1 = S.sb([128, ntl], F32)
        w2 = S.sb([128, ntl], F32)
        cw4 = S.sb(n4, F32)
        comb = S.sb([128, ntl, 16], F32)
        glog = lg[:, :, 0:4]
        elog = lg[:, :, 4:20]

        def bc_last(ap2, n):
            return ap2.unsqueeze(2).to_broadcast([128, ntl, n])

        dve.op(lambda e: e.tensor_reduce(out=gmax[:], in_=glog, axis=AX.X, op=ALU.max), r=[lg.d], w=[gmax.d])
        dve.op(lambda e: e.tensor_tensor(out=ge[:], in0=glog, in1=bc_last(gmax[:], 4), op=ALU.subtract), r=[lg.d, gmax.d], w=[ge.d])
        dve.op(lambda e: e.tensor_single_scalar(out=gsel[:], in_=ge[:], scalar=0.0, op=ALU.is_ge), r=[ge.d], w=[gsel.d])
        act.op(lambda e: e.activation(out=ge[:], in_=ge[:], func=AF.Exp), r=[ge.d], w=[ge.d])
        dve.op(lambda e: e.tensor_reduce(out=gsum[:], in_=ge[:], axis=AX.X, op=ALU.add), r=[ge.d], w=[gsum.d])
        dve.op(lambda e: e.reciprocal(out=gsum[:], in_=gsum[:]), r=[gsum.d], w=[gsum.d])
        dve.op(lambda e: e.tensor_tensor(out=tmp16[:].rearrange("p t (g e) -> p t g e", e=4),
                                         in0=elog.rearrange("p t (g e) -> p t g e", e=4),
                                         in1=gsel[:].unsqueeze(3).to_broadcast([128, ntl, 4, 4]), op=ALU.mult),
               r=[lg.d, gsel.d], w=[tmp16.d])
        dve.op(lambda e: e.tensor_reduce(out=el[:], in_=tmp16[:].rearrange("p t (g e) -> p t e g", e=4), axis=AX.X, op=ALU.add),
               r=[tmp16.d], w=[el.d])
        dve.op(lambda e: e.tensor_reduce(out=emax[:], in_=el[:], axis=AX.X, op=ALU.max), r=[el.d], w=[emax.d])
        dve.op(lambda e: e.tensor_tensor(out=ee[:], in0=el[:], in1=bc_last(emax[:], 4), op=ALU.subtract), r=[el.d, emax.d], w=[ee.d])
        dve.op(lambda e: e.tensor_single_scalar(out=oh1[:], in_=ee[:], scalar=0.0, op=ALU.is_ge), r=[ee.d], w=[oh1.d])
        act.op(lambda e: e.activation(out=ee[:], in_=ee[:], func=AF.Exp), r=[ee.d], w=[ee.d])
        dve.op(lambda e: e.scalar_tensor_tensor(out=ee2[:], in0=oh1[:], scalar=-4.0, in1=ee[:], op0=ALU.mult, op1=ALU.add),
               r=[oh1.d, ee.d], w=[ee2.d])
        dve.op(lambda e: e.tensor_reduce(out=m2[:], in_=ee2[:], axis=AX.X, op=ALU.max), r=[ee2.d], w=[m2.d])
        dve.op(lambda e: e.tensor_tensor(out=oh2[:], in0=ee2[:], in1=bc_last(m2[:], 4), op=ALU.is_ge), r=[ee2.d, m2.d], w=[oh2.d])
        dve.op(lambda e: e.tensor_scalar(out=w1[:], in0=m2[:], scalar1=1.0, scalar2=None, op0=ALU.add), r=[m2.d], w=[w1.d])
        dve.op(lambda e: e.reciprocal(out=w1[:], in_=w1[:]), r=[w1.d], w=[w1.d])
        dve.op(lambda e: e.tensor_tensor(out=w1[:], in0=w1[:], in1=gsum[:], op=ALU.mult), r=[w1.d, gsum.d], w=[w1.d])
        dve.op(lambda e: e.tensor_tensor(out=w2[:], in0=w1[:], in1=m2[:], op=ALU.mult), r=[w1.d, m2.d], w=[w2.d])
        dve.op(lambda e: e.tensor_tensor(out=cw4[:], in0=oh1[:], in1=bc_last(w1[:], 4), op=ALU.mult), r=[oh1.d, w1.d], w=[cw4.d])
        dve.op(lambda e: e.tensor_tensor(out=oh2[:], in0=oh2[:], in1=bc_last(w2[:], 4), op=ALU.mult), r=[oh2.d, w2.d], w=[oh2.d])
        dve.op(lambda e: e.tensor_tensor(out=cw4[:], in0=cw4[:], in1=oh2[:], op=ALU.add), r=[cw4.d, oh2.d], w=[cw4.d])
        dve.op(lambda e: e.tensor_copy(out=comb[:].rearrange("p t (g e) -> p t g e", e=4),
                                       in_=cw4[:].unsqueeze(2).to_broadcast([128, ntl, 4, 4])), r=[cw4.d], w=[comb.d])
        dve.op(lambda e: e.tensor_tensor(out=comb[:].rearrange("p t (g e) -> p t g e", e=4),
                                         in0=comb[:].rearrange("p t (g e) -> p t g e", e=4),
                                         in1=gsel[:].unsqueeze(3).to_broadcast([128, ntl, 4, 4]), op=ALU.mult),
               r=[comb.d, gsel.d], w=[comb.d])
        groups = []
        c0 = 0
        while c0 < ntok:
            n = min(512, ntok - c0)
            groups.append((c0, n))
            c0 += n
        sg = [S.sb([128, 512], F32) for _ in range(2)]
        hid = [[S.sb([128, 512], BF16) for _ in range(2)] for _ in range(2)]
        items = [(e_, tok0, n) for e_ in range(16) for (tok0, n) in groups]
        ybank = [0]

        def GU(i):
            e_, tok0, n = items[i]
            b = e_ % 2
            k2 = i % 2
            for hc in range(2):
                gb, ub = pb[hc * 2], pb[hc * 2 + 1]
                xd = [xnT_d[i_] for i_ in range(tok0 // 128, (tok0 + n) // 128)]
                for c in range(8):
                    pe.op(lambda e: e.matmul(gb[:, 0:n], lhsT=wg[b][:, c, hc * 128:(hc + 1) * 128], rhs=xnT[:, c, tok0:tok0 + n],
                                             start=(c == 0), stop=(c == 7)), r=[wg[b].d] + xd, w=[gb.d])
                for c in range(8):
                    pe.op(lambda e: e.matmul(ub[:, 0:n], lhsT=wu[b][:, c, hc * 128:(hc + 1) * 128], rhs=xnT[:, c, tok0:tok0 + n],
                                             start=(c == 0), stop=(c == 7)), r=[wu[b].d] + xd, w=[ub.d])
                act.op(lambda e: e.activation(out=sg[hc][:, 0:n], in_=gb[:, 0:n], func=AF.Silu), r=[gb.d], w=[sg[hc].d])
                dve.op(lambda e: e.tensor_tensor(out=hid[k2][hc][:, 0:n], in0=sg[hc][:, 0:n], in1=ub[:, 0:n], op=ALU.mult),
                       r=[sg[hc].d, ub.d], w=[hid[k2][hc].d])

        def DOWN(i):
            e_, tok0, n = items[i]
            b = e_ % 2
            k2 = i % 2
            for tt in range(n // 128):
                j = t0 + tok0 // 128 + tt
                for nn in range(2):
                    yb = pb[5 + ybank[0]]
                    ybank[0] = (ybank[0] + 1) % 3
                    for hc in range(2):
                        pe.op(lambda e: e.matmul(yb[:, :], lhsT=hid[k2][hc][:, tt * 128:(tt + 1) * 128], rhs=wd[b][:, hc, nn * 512:(nn + 1) * 512],
                                                 start=(hc == 0), stop=(hc == 1)), r=[hid[k2][hc].d, wd[b].d], w=[yb.d])
                    ti = tok0 // 128 + tt
                    dve.op(lambda e: e.scalar_tensor_tensor(out=xres[:, j, nn * 512:(nn + 1) * 512], in0=yb[:, :], scalar=comb[:, ti, e_:e_ + 1],
                                                            in1=xres[:, j, nn * 512:(nn + 1) * 512], op0=ALU.mult, op1=ALU.add),
                           r=[yb.d, comb.d, xres_d[j]], w=[xres_d[j]])
                if on_final is not None and e_ == 15:
                    on_final(j)
            if (i + 1 == len(items) or items[i + 1][0] != e_) and e_ + 2 < 16:
                load_expert(e_ + 2)

        GU(0)
        for i in range(len(items)):
            if i + 1 < len(items):
                GU(i + 1)
            DOWN(i)
        S.close()

    def wout_phase(S, mixT, mix_d, w_src, tiles, col_of_tile, wo=None):
        if wo is None:
            wo = S.sb([128, 8, D], BF16)
            load_w_bf16(wo, w_src, 8, D)
        bank = 0
        for j in tiles:
            cc = col_of_tile(j)
            for nn in range(2):
                yb = pb[bank]
                bank = (bank + 1) % 4
                for c in range(8):
                    pe.op(lambda e: e.matmul(yb[:, :], lhsT=mixT[:, c, cc:cc + 128], rhs=wo[:, c, nn * 512:(nn + 1) * 512],
                                             start=(c == 0), stop=(c == 7)), r=[mix_d, wo.d], w=[yb.d])
                dve.op(lambda e: e.tensor_tensor(out=xres[:, j, nn * 512:(nn + 1) * 512], in0=xres[:, j, nn * 512:(nn + 1) * 512],
                                                 in1=yb[:, :], op=ALU.add), r=[yb.d, xres_d[j]], w=[xres_d[j]])

    def even_mixer():
        SL = Scope(kb)
        mixT = SL.sb([128, 8, NT * 128], BF16, "mixT0")
        S = Scope(kb)
        gn = S.sb([128, D], F32)
        sp.op(lambda e: e.dma_start(out=gn[:], in_=gains_d[0]), w=[gn.d], dma=True)
        load_xres()
        wev = S.sb([128, 8, 2828], BF16)
        for (c0_, c1_) in ((1280, 2054), (2054, 2828)):
            pool.op(lambda e: e.dma_start(out=wev[:, :, c0_:c1_], in_=ev_w_in_d[:, c0_:c1_].rearrange("(c p) n -> p c n", p=128)), w=[wev.d], dma=True)
        wev_q = Dep()
        wsT = S.sb([128, 4, 128], BF16)
        pool.op(lambda e: e.dma_start(out=wsT[:], in_=ev_wsT_d.rearrange("g s t -> s g t")), w=[wsT.d], dma=True)
        Ub = cstb[:, 128:256]
        dve.op(lambda e: e.tensor_tensor(out=wsT[:], in0=wsT[:], in1=Ub.unsqueeze(1).to_broadcast([128, 4, 128]), op=ALU.mult),
               r=[wsT.d, cstb.d], w=[wsT.d])
        gqk = S.sb([128, 64], F32)
        dve.op(lambda e: e.scalar_tensor_tensor(out=gqk[:], in0=gq_b, scalar=0.125, in1=gk_b, op0=ALU.mult, op1=ALU.mult),
               r=[sm.d], w=[gqk.d])
        xin = [S.sb([128, D], F32) for _ in range(2)]
        ss2 = [S.sb([128, 4], F32) for _ in range(2)]
        xs2 = [S.sb([128, D], BF16) for _ in range(2)]
        xnT = [S.sb([128, 8, 128], BF16) for _ in range(2)]
        sq = S.sb([128, 768], BF16)
        Kraw = S.sb([128, 768], BF16)
        Qraw = S.sb([128, 768], BF16)
        ssall = S.sb([128, 28], F32)
        kaug = [S.sb([128, 12, 70], BF16) for _ in range(2)]
        qaug = [S.sb([128, 12, 70], BF16) for _ in range(2)]
        vaug = [S.sb([128, 12, 65], BF16) for _ in range(2)]
        kTs = [S.sb([70, 12, 128], BF16) for _ in range(2)]
        qTs = [S.sb([70, 12, 128], BF16) for _ in range(2)]
        zt = S.sb([128, 12], F32)
        ls = S.sb([128, 12], F32)
        Fc = [S.sb([128, 12], F32) for _ in range(2)]
        Fh = S.sb([128, 12], BF16)
        Fm = S.sb([128, 12], BF16)
        Fl = S.sb([128, 12], BF16)
        r1 = S.sb([128, 12], F32)
        qtmp = sq
        gv = S.sb([128, 256], F32)
        vpad = S.sb([128, 4, 128], BF16)
        guT = [S.sb([128, 256], F32) for _ in range(2)]
        stmp = S.sb([128, 256], F32)
        gv2 = stmp
        for b_ in range(2):
            dve.op(lambda e: e.memset(kaug[b_][:, :, 64:67], 1.0), w=[kaug[b_].d])
            dve.op(lambda e: e.memset(qaug[b_][:, :, 67:70], 1.0), w=[qaug[b_].d])
        dve.op(lambda e: e.memset(vpad[:], 0.0), w=[vpad.d])

        def own_(i):
            return i >= NPRE

        def S1a(i):
            b2 = i % 2
            if own_(i):
                j = i - NPRE
                xsrc, xdep = xres[:, j, :], xres_d[j]
            else:
                pool.op(lambda e: e.dma_start(out=xin[b2][:], in_=xp_d[i * 128:(i + 1) * 128, :]), w=[xin[b2].d], dma=True)
                xsrc, xdep = xin[b2][:], xin[b2].d
            nt_a(xsrc, xdep, gn, ss2[b2], xs2[b2])

        def S1b(i):
            b2 = i % 2
            nt_pe(xs2[b2], 0)
            nt_copy(xnT[b2][:], xnT[b2].d, 0, on_dve=(i % 2 == 0))

        def S2(i):
            xT = xnT[i % 2]

            def proj(bank, c_lo, c_hi, w_lo):
                n = c_hi - c_lo
                wdep = wev.d if w_lo >= 1280 else wev_q
                for c in range(8):
                    pe.op(lambda e: e.matmul(pb[bank][:, c_lo:c_hi], lhsT=xT[:, c, :], rhs=wev[:, c, w_lo:w_lo + n],
                                             start=(c == 0), stop=(c == 7)), r=[xT.d, wdep], w=[pb[bank].d])
            proj(1, 0, 512, 1280)
            proj(2, 0, 256, 1792)
            proj(2, 256, 512, 2048)
            proj(3, 0, 512, 2304)
            proj(4, 0, 12, 2816)
            if own_(i):
                proj(5, 0, 512, 512)
                proj(6, 0, 256, 1024)
                proj(6, 256, 512, 256)
                for uc in range(2):
                    for c in range(8):
                        pe.op(lambda e: e.matmul(pb[7][:, uc * 128:(uc + 1) * 128], lhsT=wev[:, c, uc * 128:(uc + 1) * 128], rhs=xT[:, c, :],
                                                 start=(c == 0), stop=(c == 7)), r=[xT.d, wev_q], w=[pb[7].d])

        def S3(i):
            b2 = i % 2
            vflag = valid if i <= NPRE else 1.0
            nvflag = nvalid if i <= NPRE else -1.0
            va = vaug[b2]
            dve.op(lambda e: e.tensor_tensor(out=zt[:], in0=pb[4][:, 0:12], in1=bfb, op=ALU.add), r=[pb[4].d, sm.d], w=[zt.d])
            act.op(lambda e: e.activation(out=zt[:], in_=zt[:], func=AF.Exp, scale=-1.0), r=[zt.d], w=[zt.d])
            act.op(lambda e: e.activation(out=Kraw[:, 0:512], in_=pb[1][:, 0:512], func=AF.Copy), r=[pb[1].d], w=[Kraw.d])
            act.op(lambda e: e.activation(out=Kraw[:, 512:768], in_=pb[2][:, 0:256], func=AF.Copy), r=[pb[2].d], w=[Kraw.d])
            dve.op(lambda e: e.tensor_scalar(out=zt[:], in0=zt[:], scalar1=1.0, scalar2=None, op0=ALU.add), r=[zt.d], w=[zt.d])
            act.op(lambda e: e.activation(out=zt[:], in_=zt[:], func=AF.Ln), r=[zt.d], w=[zt.d])
            dve.op(lambda e: e.tensor_scalar(out=va[:, 0:4, 0:64], in0=pb[2][:, 256:512].rearrange("p (h d) -> p h d", d=64),
                                             scalar1=vflag, scalar2=None, op0=ALU.mult), r=[pb[2].d, pc.d], w=[va.d])
            dve.op(lambda e: e.tensor_scalar(out=va[:, 4:12, 0:64], in0=pb[3][:, 0:512].rearrange("p (h d) -> p h d", d=64),
                                             scalar1=vflag, scalar2=None, op0=ALU.mult), r=[pb[3].d, pc.d], w=[va.d])
            dve.op(lambda e: e.tensor_scalar(out=va[:, :, 64], in0=ones_f[:, 0:12], scalar1=vflag, scalar2=None, op0=ALU.mult),
                   r=[ones_f.d, pc.d], w=[va.d])
            sp.op(lambda e: e.dma_start(out=V_d[i], in_=va[:].rearrange("p h d -> p (h d)")), r=[va.d], w=[V_d.d], dma=True)
            dve.op(lambda e: e.tensor_scalar(out=ls[:], in0=zt[:], scalar1=nvflag, scalar2=None, op0=ALU.mult), r=[zt.d, pc.d], w=[ls.d])
            if own_(i):
                act.op(lambda e: e.activation(out=Qraw[:, 0:512], in_=pb[5][:, 0:512], func=AF.Copy), r=[pb[5].d], w=[Qraw.d])
                act.op(lambda e: e.activation(out=Qraw[:, 512:768], in_=pb[6][:, 0:256], func=AF.Copy), r=[pb[6].d], w=[Qraw.d])
                act.op(lambda e: e.activation(out=gv[:], in_=pb[6][:, 256:512], func=AF.Gelu_apprx_tanh), r=[pb[6].d], w=[gv.d])
                act.op(lambda e: e.activation(out=guT[b2][:], in_=pb[7][:, 0:256], func=AF.Gelu_apprx_tanh), r=[pb[7].d], w=[guT[b2].d])

        def S4(i):
            b2 = i % 2
            Fcur, Fprev = Fc[b2], Fc[1 - b2]
            ka, qa = kaug[b2], qaug[b2]
            pe.op(lambda e: e.matmul(pb[4][:, 16:28], lhsT=U_f, rhs=ls[:], start=True, stop=(i == 0)), r=[cst32.d, ls.d], w=[pb[4].d])
            if i > 0:
                pe.op(lambda e: e.matmul(pb[4][:, 16:28], lhsT=sel127_f, rhs=Fprev[:], start=False, stop=True), r=[cst32.d, Fprev.d], w=[pb[4].d])
            dve.op(lambda e: e.tensor_copy(out=Fcur[:], in_=pb[4][:, 16:28]), r=[pb[4].d], w=[Fcur.d])
            dve.op(lambda e: e.tensor_scalar(out=ka[:, :, 67], in0=Fcur[:], scalar1=-1.0, scalar2=None, op0=ALU.mult), r=[Fcur.d], w=[ka.d])
            dve.op(lambda e: e.tensor_tensor(out=r1[:], in0=Fcur[:], in1=ka[:, :, 67], op=ALU.add), r=[Fcur.d, ka.d], w=[r1.d])
            dve.op(lambda e: e.tensor_scalar(out=ka[:, :, 68], in0=r1[:], scalar1=-1.0, scalar2=None, op0=ALU.mult), r=[r1.d], w=[ka.d])
            dve.op(lambda e: e.tensor_tensor(out=r1[:], in0=r1[:], in1=ka[:, :, 68], op=ALU.add), r=[r1.d, ka.d], w=[r1.d])
            dve.op(lambda e: e.tensor_scalar(out=ka[:, :, 69], in0=r1[:], scalar1=-1.0, scalar2=None, op0=ALU.mult), r=[r1.d], w=[ka.d])
            if own_(i):
                dve.op(lambda e: e.tensor_scalar(out=qa[:, :, 64:67], in0=ka[:, :, 67:70], scalar1=-1.0, scalar2=None, op0=ALU.mult), r=[ka.d], w=[qa.d])

        def S5(i):
            b2 = i % 2
            ka, qa = kaug[b2], qaug[b2]
            own = own_(i)
            pool.op(lambda e: e.tensor_tensor(out=sq[:], in0=Kraw[:], in1=Kraw[:], op=ALU.mult), r=[Kraw.d], w=[sq.d])
            dve.op(lambda e: e.tensor_reduce(out=ssall[:, 0:12], in_=sq[:].rearrange("p (h d) -> p h d", d=64), axis=AX.X, op=ALU.add), r=[sq.d], w=[ssall.d])
            if own:
                pool.op(lambda e: e.tensor_tensor(out=sq[:], in0=Qraw[:], in1=Qraw[:], op=ALU.mult), r=[Qraw.d], w=[sq.d])
                dve.op(lambda e: e.tensor_reduce(out=ssall[:, 12:24], in_=sq[:].rearrange("p (h d) -> p h d", d=64), axis=AX.X, op=ALU.add), r=[sq.d], w=[ssall.d])
                pool.op(lambda e: e.tensor_tensor(out=gv2[:], in0=gv[:], in1=gv[:], op=ALU.mult), r=[gv.d], w=[gv2.d])
                dve.op(lambda e: e.tensor_reduce(out=ssall[:, 24:28], in_=gv2[:].rearrange("p (g d) -> p g d", d=64), axis=AX.X, op=ALU.add), r=[gv2.d], w=[ssall.d])
            rstd_from_ss(S, ssall, 64, 1.0, 28 if own else 12)
            dve.op(lambda e: e.tensor_tensor(out=ka[:, :, 0:64], in0=Kraw[:].rearrange("p (h d) -> p h d", d=64),
                                             in1=ssall[:, 0:12].unsqueeze(2).to_broadcast([128, 12, 64]), op=ALU.mult), r=[Kraw.d, ssall.d], w=[ka.d])
            if not own:
                return
            dve.op(lambda e: e.tensor_tensor(out=qtmp[:].rearrange("p (h d) -> p h d", d=64), in0=Qraw[:].rearrange("p (h d) -> p h d", d=64),
                                             in1=ssall[:, 12:24].unsqueeze(2).to_broadcast([128, 12, 64]), op=ALU.mult), r=[Qraw.d, ssall.d], w=[qtmp.d])
            dve.op(lambda e: e.tensor_tensor(out=qa[:, :, 0:64], in0=qtmp[:].rearrange("p (h d) -> p h d", d=64),
                                             in1=gqk[:].unsqueeze(1).to_broadcast([128, 12, 64]), op=ALU.mult), r=[qtmp.d, gqk.d], w=[qa.d])
            for g in range(4):
                off = 64 * (g % 2)
                dve.op(lambda e: e.tensor_scalar(out=vpad[:, g, off:off + 64], in0=gv[:, g * 64:(g + 1) * 64], scalar1=ssall[:, 24 + g:25 + g], scalar2=None,
                                                 op0=ALU.mult), r=[gv.d, ssall.d], w=[vpad.d])

        def S6(i):
            b2 = i % 2
            j = i - NPRE
            ka, qa = kaug[b2], qaug[b2]
            for h in range(12):
                bk = 1 if h < 8 else 2
                pe.op(lambda e: e.transpose(out=pbf(bk)[0:70, h % 8, :], in_=ka[:, h, :], identity=ident_b), r=[ka.d, cstb.d], w=[pb[bk].d])
            ks = kTs[b2]
            act.op(lambda e: e.copy(out=ks[:, 0:8, :], in_=pbf(1)[0:70, :, :]), r=[pb[1].d], w=[ks.d])
            act.op(lambda e: e.copy(out=ks[:, 8:12, :], in_=pbf(2)[0:70, 0:4, :]), r=[pb[2].d], w=[ks.d])
            sp.op(lambda e: e.dma_start(out=kT_d[:, :, i * 128:(i + 1) * 128].rearrange("h r t -> r h t"), in_=ks[:]), r=[ks.d], w=[kT_d.d], dma=True)
            if not own_(i):
                return
            for h in range(12):
                bk = 5 if h < 8 else 6
                pe.op(lambda e: e.transpose(out=pbf(bk)[0:70, h % 8, :], in_=qa[:, h, :], identity=ident_b), r=[qa.d, cstb.d], w=[pb[bk].d])
            qs = qTs[b2]
            dve.op(lambda e: e.tensor_copy(out=qs[:, 0:8, :], in_=pbf(5)[0:70, :, :]), r=[pb[5].d], w=[qs.d])
            dve.op(lambda e: e.tensor_copy(out=qs[:, 8:12, :], in_=pbf(6)[0:70, 0:4, :]), r=[pb[6].d], w=[qs.d])
            sp.op(lambda e: e.dma_start(out=qT_d[:, :, j * 128:(j + 1) * 128].rearrange("h r t -> r h t"), in_=qs[:]), r=[qs.d], w=[qT_d.d], dma=True)
            for c in range(2):
                for gg in range(2):
                    g = 2 * c + gg
                    pe.op(lambda e: e.matmul(pb[4][:, 128 + c * 128:128 + (c + 1) * 128], lhsT=vpad[:, g, :], rhs=wsT[:, g, :], start=(gg == 0), stop=(gg == 1)),
                          r=[vpad.d, wsT.d], w=[pb[4].d])
            gu = guT[b2]
            for c in range(2):
                dve.op(lambda e: e.scalar_tensor_tensor(out=stmp[:, c * 128:(c + 1) * 128], in0=pb[4][:, 128 + c * 128:128 + (c + 1) * 128], scalar=gvcol[:, c:c + 1],
                                                        in1=bsl[:, c * 128:(c + 1) * 128], op0=ALU.mult, op1=ALU.add), r=[pb[4].d, sm.d], w=[stmp.d])
                dve.op(lambda e: e.tensor_tensor(out=mixT[:, c, j * 128:(j + 1) * 128], in0=stmp[:, c * 128:(c + 1) * 128], in1=gu[:, c * 128:(c + 1) * 128],
                                                 op=ALU.mult), r=[stmp.d, gu.d], w=[mixT.d])

        S1a(0)
        S1a(1)
        for (c0_, c1_) in ((0, 640), (640, 1280)):
            pool.op(lambda e: e.dma_start(out=wev[:, :, c0_:c1_], in_=ev_w_in_d[:, c0_:c1_].rearrange("(c p) n -> p c n", p=128)), w=[wev_q], dma=True)
        S1b(0)
        S2(0)
        S3(0)
        for i in range(NSEQ):
            if i + 2 < NSEQ:
                S1a(i + 2)
            if i + 1 < NSEQ:
                S1b(i + 1)
            S4(i)
            if i + 1 < NSEQ:
                S2(i + 1)
            S5(i)
            if i + 1 < NSEQ:
                S3(i + 1)
            S6(i)
        S.close()
        if CUT == 10:
            SL.close(); return

        SBC = Scope(kb)
        wo_ev = SBC.sb([128, 8, D], BF16)
        load_w_bf16(wo_ev, ev_w_out_d, 8, D)
        S = Scope(kb)
        kTh = [S.sb([70, NSEQ * 128], BF16) for _ in range(2)]
        qTh = [S.sb([70, NT * 128], BF16) for _ in range(2)]
        Vg = [S.sb([128, NSEQ, 324], BF16) for _ in range(2)]
        for b_ in range(2):
            pool.op(lambda e: e.memset(Vg[b_][:, :, 260:324], 0.0), w=[Vg[b_].d])
        PT = [S.sb([128, 1024], BF16) for _ in range(3)]
        rc = [S.sb([128, 512], F32) for _ in range(2)]
        bcs = [S.sb([64, 512], F32) for _ in range(2)]
        otmp = [S.sb([64, 512], BF16) for _ in range(2)]
        QC = [(0, 128), (128, 512), (640, 512), (1152, 512), (1664, 512)]
        STP = [pbig[0], pbig[1], pbig[2]]
        STD = [Dep(), Dep(), Dep()]
        for k_ in range(3):
            pb[2 * k_].d = STD[k_]
            pb[2 * k_ + 1].d = STD[k_]
        free_slots = [0, 1, 2]

        def take_slot():
            return free_slots.pop(0)

        def give_slot(k_):
            free_slots.append(k_)

        def load_head(h):
            b = h % 2
            sp.op(lambda e: e.dma_start(out=kTh[b][:], in_=kT_d[h]), r=[kT_d.d], w=[kTh[b].d], dma=True)
            sp.op(lambda e: e.dma_start(out=qTh[b][:], in_=qT_d[h]), r=[qT_d.d], w=[qTh[b].d], dma=True)

        def load_vgroup(g):
            b = g % 2
            sp.op(lambda e: e.dma_start(out=Vg[b][:, :, 0:260], in_=V_d[:, :, g * 260:(g + 1) * 260].rearrange("t p c -> p t c")),
                  r=[V_d.d], w=[Vg[b].d], dma=True)

        load_head(0)
        load_vgroup(0)
        load_head(1)
        load_vgroup(1)
        units = []
        nchunk = 0
        for h in range(12):
            for (q0, nq) in QC:
                jlo = q0 // 128
                jhi = jlo + nq // 128 - 1
                last = NPRE + jhi
                if nq == 128:
                    for k0 in range(0, last + 1, 4):
                        subs = [(kt, (kt - k0) * 128, 0, 128, kt == last) for kt in range(k0, k0 + 4)]
                        units.append((h, q0, nq, last, nchunk, subs, 0, 512))
                else:
                    nfull = NPRE + jlo
                    for kt in range(0, nfull, 2):
                        units.append((h, q0, nq, last, nchunk, [(kt, 0, 0, 512, False), (kt + 1, 512, 0, 512, False)], 0, 1024))
                    for dj in range(4):
                        units.append((h, q0, nq, last, nchunk, [(nfull + dj, 0, dj * 128, 512, True)], dj * 128, 512))
                nchunk += 1
        NU = len(units)
        deferred = []
        uslot = {}

        def emit_qk(ui):
            h, q0, nq, last, ck, subs, lo, hi = units[ui]
            kt_, qt_ = kTh[h % 2], qTh[h % 2]
            k_ = take_slot()
            uslot[ui] = k_
            ST, sd = STP[k_], STD[k_]
            for (kt, off, c0, n, diag) in subs:
                lhs = kt_[:, kt * 128:(kt + 1) * 128]
                if diag:
                    pe.op(lambda e: e.matmul(ST[:, off + c0:off + c0 + 128], lhsT=lhs, rhs=qt_[:, q0 + c0:q0 + c0 + 128], start=True, stop=False),
                          r=[kt_.d, qt_.d], w=[sd])
                    pe.op(lambda e: e.matmul(ST[:, off + c0:off + c0 + 128], lhsT=ident_b, rhs=maskT_b, start=False, stop=True),
                          r=[cstb.d], w=[sd])
                    if c0 + 128 < n:
                        pe.op(lambda e: e.matmul(ST[:, off + c0 + 128:off + n], lhsT=lhs, rhs=qt_[:, q0 + c0 + 128:q0 + n], start=True, stop=True),
                              r=[kt_.d, qt_.d], w=[sd])
                else:
                    pe.op(lambda e: e.matmul(ST[:, off + c0:off + n], lhsT=lhs, rhs=qt_[:, q0 + c0:q0 + n], start=True, stop=True),
                          r=[kt_.d, qt_.d], w=[sd])
            if ui + 1 == NU or units[ui + 1][0] != h:
                if h + 2 < 12:
                    load_head(h + 2)

        def emit_exp_pv(ui):
            h, q0, nq, last, ck, subs, lo, hi = units[ui]
            k_ = uslot[ui]
            ST, sd = STP[k_], STD[k_]
            ptb = PT[ui % 3]
            OB = pb[6 + ck % 2]
            vg_ = Vg[(h // 4) % 2]
            hv = (h % 4) * 65
            act.op(lambda e: e.activation(out=ptb[:, lo:hi], in_=ST[:, lo:hi], func=AF.Exp), r=[sd], w=[ptb.d])
            give_slot(k_)
            for (kt, off, c0, n, diag) in subs:
                pe.op(lambda e: e.matmul(OB[:, c0:n], lhsT=vg_[:, kt, hv:hv + 128], rhs=ptb[:, off + c0:off + n], start=(kt == 0), stop=(kt == last),
                                         skip_group_check=True), r=[vg_.d, ptb.d], w=[OB.d])
            if subs[-1][0] == last:
                if h == 3 and q0 == QC[-1][0]:
                    load_vgroup(2)
                k2 = ck % 2
                rc_, bcs_, ot_ = rc[k2], bcs[k2], otmp[k2]
                ch = 2 + h // 2
                dve.op(lambda e: e.tensor_scalar(out=rc_[64:65, 0:nq], in0=OB[64:65, 0:nq], scalar1=1e-30, scalar2=None, op0=ALU.add), r=[OB.d], w=[rc_.d])
                dve.op(lambda e: e.reciprocal(out=rc_[64:65, 0:nq], in_=rc_[64:65, 0:nq]), r=[rc_.d], w=[rc_.d])

                def stage2():
                    kk = take_slot()
                    give_slot(kk)
                    BB, bd = STP[kk], STD[kk]
                    pe.op(lambda e: e.matmul(BB[0:64, 0:nq], lhsT=ones_f[64:65, 0:64], rhs=rc_[64:65, 0:nq], start=True, stop=True), r=[ones_f.d, rc_.d], w=[bd])
                    dve.op(lambda e: e.tensor_copy(out=bcs_[:, 0:nq], in_=BB[0:64, 0:nq]), r=[bd], w=[bcs_.d])
                    if h % 2 == 0:
                        dve.op(lambda e: e.tensor_tensor(out=mixT[0:64, ch, q0:q0 + nq], in0=OB[0:64, 0:nq], in1=bcs_[:, 0:nq], op=ALU.mult),
                               r=[OB.d, bcs_.d], w=[mixT.d])
                    else:
                        dve.op(lambda e: e.tensor_tensor(out=ot_[:, 0:nq], in0=OB[0:64, 0:nq], in1=bcs_[:, 0:nq], op=ALU.mult),
                               r=[OB.d, bcs_.d], w=[ot_.d])

                def stage3():
                    if h % 2 == 1:
                        kk = take_slot()
                        give_slot(kk)
                        SB_, bd = STP[kk], STD[kk]
                        pe.op(lambda e: e.matmul(SB_[:, 0:nq], lhsT=shift_b, rhs=ot_[:, 0:nq], start=True, stop=True), r=[cstb.d, ot_.d], w=[bd])
                        dve.op(lambda e: e.tensor_copy(out=mixT[64:128, ch, q0:q0 + nq], in_=SB_[64:128, 0:nq]), r=[bd], w=[mixT.d])
                deferred.append((ui + 2, stage2))
                deferred.append((ui + 4, stage3))

        LA = 2
        for i in range(NU + LA):
            if i < NU:
                emit_qk(i)
            j = i - LA
            if j >= 0:
                emit_exp_pv(j)
            while deferred and deferred[0][0] <= j:
                deferred.pop(0)[1]()
        for _, fn in deferred:
            fn()
        for k_ in range(6):
            pb[k_].d = Dep()
        S.close()
        S = Scope(kb)
        wout_phase(S, mixT, mixT.d, ev_w_out_d, list(range(NT)), lambda j: j * 128, wo=wo_ev)
        S.close()
        SBC.close()
        SL.close()

    def odd_mixer():
        SL = Scope(kb)
        NTOK = 16 * 128
        mixT = SL.sb([128, 8, NTOK], BF16, "mixT1")
        wo_od = SL.sb([128, 8, D], BF16)
        S = Scope(kb)
        wod = S.sb([128, 8, 2048], BF16)
        load_w_bf16(wod, od_w_in_d, 8, 2048, colsplit=512)
        load_w_bf16(wo_od, od_w_out_d, 8, D)
        wpl = S.sb([128, 4, 128], BF16)
        xnT = S.sb([128, 8, NT * 128], BF16)
        xnT_d = [Dep() for _ in range(NT)]
        S1s = Scope(kb)
        wpl32 = S1s.sb([128, 4, 128], F32)
        sp.op(lambda e: e.dma_start(out=wpl32[:], in_=od_w_pool_d.rearrange("g c d -> c g d")), w=[wpl32.d], dma=True)
        dve.op(lambda e: e.tensor_copy(out=wpl[:], in_=wpl32[:]), r=[wpl32.d], w=[wpl.d])
        gn = S1s.sb([128, D], F32)
        sp.op(lambda e: e.dma_start(out=gn[:], in_=gains_d[2]), w=[gn.d], dma=True)
        ss2 = [S1s.sb([128, 4], F32) for _ in range(2)]
        xs2 = [S1s.sb([128, D], BF16) for _ in range(2)]
        nt_sequence(lambda j: (xres[:, j, :], xres_d[j]), NT, gn, ss2, xs2, lambda j: (xnT[:, :, j * 128:(j + 1) * 128], xnT_d[j]))
        S1s.close()
        if CUT <= 1:
            S.close(); SL.close(); return
        HL = 16
        W = HL + 512
        zb = [S.sb([128, W], F32) for _ in range(2)]
        pbuf = [S.sb([128, W], F32) for _ in range(2)]
        bgs = [S.sb([128, 512], BF16) for _ in range(2)]
        zc = S.sb([128, 4, HL], F32)
        pcar = S.sb([128, 4, HL], F32)
        cgs = S.sb([128, 512], BF16)
        y = S.sb([128, 512], F32)
        s1 = S.sb([128, W], F32)
        s2 = S.sb([128, W], F32)
        pooled = cgs
        dve.op(lambda e: e.memset(s1[:], 0.0), w=[s1.d])
        dve.op(lambda e: e.memset(s2[:], 0.0), w=[s2.d])
        groups = [(128 - HL, HL)] + [(128 + 512 * i, 512) for i in range(4)]
        work = [(gi, k) for gi in range(len(groups)) for k in range(4)]
        PB7 = [0, 1, 2, 3, 5, 6, 7]
        rot = [0]
        state = {}

        def nextbank():
            bk = PB7[rot[0] % 7]
            rot[0] += 1
            return bk

        def Pstage(n):
            gi, k = work[n]
            tok0, nn_ = groups[gi]
            xd = [xnT_d[i] for i in range(tok0 // 128, (tok0 + nn_ - 1) // 128 + 1)]
            banks = {}
            cols = [("cg", 512 + k * 128), ("hc", 1024 + k * 128), ("p", 1536 + k * 128)]
            if gi > 0:
                cols.append(("bg", k * 128))
            for nm, col in cols:
                bk = nextbank()
                banks[nm] = bk
                for c in range(8):
                    pe.op(lambda e: e.matmul(pb[bk][:, 0:nn_], lhsT=wod[:, c, col:col + 128], rhs=xnT[:, c, tok0:tok0 + nn_],
                                             start=(c == 0), stop=(c == 7)), r=[wod.d] + xd, w=[pb[bk].d])
            state[n] = banks

        def Estage(n):
            gi, k = work[n]
            tok0, nn_ = groups[gi]
            banks = state[n]
            z, p = zb[n % 2], pbuf[n % 2]
            act.op(lambda e: e.copy(out=cgs[:, 0:nn_], in_=pb[banks["cg"]][:, 0:nn_]), r=[pb[banks["cg"]].d], w=[cgs.d])
            if gi == 0:
                dve.op(lambda e: e.tensor_tensor(out=zc[:, k, :], in0=cgs[:, 0:nn_], in1=pb[banks["hc"]][:, 0:nn_], op=ALU.mult),
                       r=[cgs.d, pb[banks["hc"]].d], w=[zc.d])
                act.op(lambda e: e.copy(out=pcar[:, k, :], in_=pb[banks["p"]][:, 0:nn_]), r=[pb[banks["p"]].d], w=[pcar.d])
                return
            act.op(lambda e: e.copy(out=p[:, HL:W], in_=pb[banks["p"]][:, 0:nn_]), r=[pb[banks["p"]].d], w=[p.d])
            act.op(lambda e: e.copy(out=bgs[n % 2][:], in_=pb[banks["bg"]][:, 0:nn_]), r=[pb[banks["bg"]].d], w=[bgs[n % 2].d])
            dve.op(lambda e: e.tensor_tensor(out=z[:, HL:W], in0=cgs[:, 0:nn_], in1=pb[banks["hc"]][:, 0:nn_], op=ALU.mult),
                   r=[cgs.d, pb[banks["hc"]].d], w=[z.d])

        def Bstage(n):
            gi, k = work[n]
            if gi == 0:
                return
            tok0, nn_ = groups[gi]
            mcol = tok0 - 128
            z, p = zb[n % 2], pbuf[n % 2]
            dve.op(lambda e: e.tensor_copy(out=z[:, 0:HL], in_=zc[:, k, :]), r=[zc.d], w=[z.d])
            dve.op(lambda e: e.tensor_copy(out=p[:, 0:HL], in_=pcar[:, k, :]), r=[pcar.d], w=[p.d])
            dve.op(lambda e: e.tensor_copy(out=zc[:, k, :], in_=z[:, W - HL:W]), r=[z.d], w=[zc.d])
            dve.op(lambda e: e.tensor_copy(out=pcar[:, k, :], in_=p[:, W - HL:W]), r=[p.d], w=[pcar.d])
            dve.op(lambda e: e.tensor_scalar(out=y[:], in0=z[:, HL:W], scalar1=cw[:, k * 3 + 2:k * 3 + 3], scalar2=None, op0=ALU.mult),
                   r=[z.d, sm.d], w=[y.d])
            dve.op(lambda e: e.scalar_tensor_tensor(out=y[:], in0=z[:, HL - 1:W - 1], scalar=cw[:, k * 3 + 1:k * 3 + 2], in1=y[:], op0=ALU.mult, op1=ALU.add),
                   r=[z.d, sm.d, y.d], w=[y.d])
            dve.op(lambda e: e.scalar_tensor_tensor(out=y[:], in0=z[:, HL - 2:W - 2], scalar=cw[:, k * 3:k * 3 + 1], in1=y[:], op0=ALU.mult, op1=ALU.add),
                   r=[z.d, sm.d, y.d], w=[y.d])
            dve.op(lambda e: e.tensor_tensor(out=mixT[:, k, mcol:mcol + 512], in0=y[:], in1=bgs[n % 2][:], op=ALU.mult), r=[y.d, bgs[n % 2].d], w=[mixT.d])
            src, sd = p[:, :], p.d
            bufs = [s1, s2]
            sh = 1
            for m in range(k + 1):
                dst = bufs[m % 2]
                pool.op(lambda e: e.tensor_tensor(out=dst[:, sh:W], in0=src[:, sh:W], in1=src[:, 0:W - sh], op=ALU.add),
                        r=[sd], w=[dst.d])
                src, sd = dst[:, :], dst.d
                sh *= 2
            win = 2 ** (k + 1)
            dve.op(lambda e: e.scalar_tensor_tensor(out=pooled[:], in0=src[:, HL:W], scalar=1.0 / win, in1=p[:, HL:W], op0=ALU.mult, op1=ALU.subtract),
                   r=[sd, p.d], w=[pooled.d])
            if gi == 1:
                dve.op(lambda e: e.tensor_tensor(out=y[:, 0:16], in0=src[:, HL:HL + 16], in1=icnt[:, k * 16:(k + 1) * 16], op=ALU.mult),
                       r=[sd, pc.d, y.d], w=[y.d])
                dve.op(lambda e: e.tensor_tensor(out=pooled[:, 0:16], in0=y[:, 0:16], in1=p[:, HL:HL + 16], op=ALU.subtract),
                       r=[y.d, p.d], w=[pooled.d])
            pe.op(lambda e: e.matmul(pb[4][:, 0:512], lhsT=wpl[:, k, :], rhs=pooled[:], start=True, stop=True), r=[wpl.d, pooled.d], w=[pb[4].d])
            act.op(lambda e: e.activation(out=mixT[:, 4 + k, mcol:mcol + 512], in_=pb[4][:, 0:512], func=AF.Copy, scale=pscale[:, k:k + 1]),
                   r=[pb[4].d, sm.d], w=[mixT.d])

        NW = len(work)
        Pstage(0)
        Estage(0)
        for n in range(NW):
            if n + 1 < NW:
                Pstage(n + 1)
            Bstage(n)
            if n + 1 < NW:
                Estage(n + 1)
        S.close()
        if CUT <= 4:
            SL.close(); return
        S = Scope(kb)
        wout_phase(S, mixT, mixT.d, od_w_out_d, list(range(1, NT)), lambda j: (j - 1) * 128, wo=wo_od)
        S.close()
        SL.close()

    if L0:
        even_mixer()
        if dbg == "m0":
            dbg_dump()
        if CUT != 10:
            moe_layer(0, list(range(NT)))
        if dbg == "e0":
            dbg_dump()
    if L1:
        if not L0:
            load_xres()
        if CUT > 0:
            odd_mixer()
        if dbg == "m1":
            dbg_dump()
        out_evs = []
        def store_tile(j):
            out_evs.append(sp.op(lambda e: e.dma_start(out=out_d[(j - 1) * 128:j * 128, :], in_=xres[:, j, :]), r=[xres_d[j]], dma=True))
        if CUT > 5:
            moe_layer(1, list(range(1, NT)), on_final=store_tile)
    evs = []
    if L1:
        if CUT <= 5:
            for j in range(1, NT):
                evs.append(sp.op(lambda e: e.dma_start(out=out_d[(j - 1) * 128:j * 128, :], in_=xres[:, j, :]), r=[xres_d[j]], dma=True))
    else:
        for j in range(NT):
            evs.append(sp.op(lambda e: e.dma_start(out=out_d[j * 128:(j + 1) * 128, :], in_=xres[:, j, :]), r=[xres_d[j]], dma=True))
    kb.barrier([sp])
    return nc, kb


def _consts():
    cst = np.zeros((128, 640), np.float32)
    idx = np.arange(128)
    cst[:, 0:128] = np.eye(128, dtype=np.float32)
    cst[:, 128:256] = (idx[:, None] <= idx[None, :]).astype(np.float32)
    cst[127, 256:384] = 1.0
    cst[:, 384:512] = np.where(idx[:, None] <= idx[None, :], 0.0, NEG)
    cst[np.arange(64), 512 + 64 + np.arange(64)] = 1.0
    selall = np.zeros((16, 2048), np.float32)
    for e in range(16):
        selall[e, e * 128:(e + 1) * 128] = 1.0
    return cst, selall


def _percore(hf):
    pc = np.zeros((128, 68), np.float32)
    v = 1.0 if hf == 1 else 0.0
    pc[:, 0] = v
    pc[:, 1] = -v
    pc[:, 2] = 1.0
    pc[:, 3] = -1.0
    t = np.arange(16)
    for k, win in enumerate((2, 4, 8, 16)):
        cnt = np.minimum(t + 1, win) if hf == 0 else np.full(16, win)
        pc[:, 4 + k * 16:4 + (k + 1) * 16] = (1.0 / cnt)[None, :]
    return pc


def _make_inputs(inputs):
    f = lambda a: np.ascontiguousarray(np.asarray(a, dtype=np.float32))
    x = f(inputs["x"])
    cst, selall = _consts()
    p = np.arange(128)
    sm = np.zeros((128, 454), np.float32)
    sm[:, 0:12] = f(inputs["ev_b_forget"])[0][None, :]
    sm[:, 12:76] = f(inputs["ev_g_q"])[0][None, :]
    sm[:, 76:140] = f(inputs["ev_g_k"])[0][None, :]
    gv = f(inputs["ev_g_v"])[0]
    bs = f(inputs["ev_b_s"])[0]
    for c in range(2):
        sm[:, 140 + c] = gv[2 * c + p // 64, p % 64]
        sm[:, 142 + c * 128:142 + (c + 1) * 128] = bs[2 * c + p // 64, :]
    cwv = f(inputs["od_conv_w"])[0]
    for k in range(4):
        for tap in range(3):
            sm[:, 398 + k * 3 + tap] = cwv[tap, k * 128 + p]
        sm[:, 410 + k] = f(inputs["od_pool_scale"])[0][k * 128 + p]
    bg = f(inputs["moe_b_group"])
    br = f(inputs["moe_b_router"]).reshape(2, 16)
    for l in range(2):
        sm[:, 414 + l * 20:414 + l * 20 + 4] = bg[l][None, :]
        sm[:, 414 + l * 20 + 4:414 + (l + 1) * 20] = br[l][None, :]
    gains = np.stack([np.broadcast_to(f(inputs["ev_norm"])[0], (128, D)), np.broadcast_to(f(inputs["moe_norm"])[0], (128, D)),
                      np.broadcast_to(f(inputs["od_norm"])[0], (128, D)), np.broadcast_to(f(inputs["moe_norm"])[1], (128, D))]).astype(np.float32)
    wr = np.concatenate([f(inputs["moe_w_group"]), f(inputs["moe_w_router"]).reshape(2, D, 16)], axis=2)
    shared = {
        "cst": cst, "selall": selall, "smalls": sm, "gains": np.ascontiguousarray(gains),
        "ev_w_in": f(inputs["ev_w_in"])[0], "ev_wsT": np.ascontiguousarray(f(inputs["ev_w_s"])[0].transpose(0, 2, 1)),
        "ev_w_out": f(inputs["ev_w_out"])[0], "od_w_in": f(inputs["od_w_in"])[0], "od_w_pool": f(inputs["od_w_pool"])[0],
        "od_w_out": f(inputs["od_w_out"])[0], "wr": np.ascontiguousarray(wr),
        "moe_w_gate": f(inputs["moe_w_gate"]), "moe_w_up": f(inputs["moe_w_up"]), "moe_w_down": f(inputs["moe_w_down"]),
    }
    in_maps = []
    for c in range(8):
        b, hf = c // 2, c % 2
        m = dict(shared)
        m["pc"] = _percore(hf)
        if hf == 1:
            m["xp"] = np.ascontiguousarray(x[b, 0:NPRE * 128])
            m["xo"] = np.ascontiguousarray(x[b, NPRE * 128:4096])
        else:
            m["xp"] = np.zeros((NPRE * 128, D), np.float32)
            m["xo"] = np.ascontiguousarray(np.concatenate([np.zeros((128, D), np.float32), x[b, 0:2048]], axis=0))
        in_maps.append(m)
    return in_maps


_CACHE = {}


def kernel(**inputs):
    in_maps = _make_inputs(inputs)
    if "nc" not in _CACHE:
        _CACHE["nc"] = build((0, 1))[0]
    nc = _CACHE["nc"]
    res = run_bass_kernel_spmd(nc, in_maps, core_ids=list(range(8)))
    out = np.zeros((4, 4096, D), np.float32)
    for c in range(8):
        b, hf = c // 2, c % 2
        out[b, hf * 2048:(hf + 1) * 2048] = res.results[c]["out"]
    return out
```

```python
import contextlib
import os
CUT = int(os.environ.get('KCUT', '99'))
KSUB = int(os.environ.get('KSUB', '0'))
import numpy as np
import concourse.bass as bass
import concourse.mybir as mybir
from concourse.bass_utils import run_bass_kernel_spmd

F32 = mybir.dt.float32
BF16 = mybir.dt.bfloat16
AF = mybir.ActivationFunctionType
ALU = mybir.AluOpType
AX = mybir.AxisListType

EPOCH = 28000
NT = 17
NPRE = 15
NSEQ = 32
D = 1024
EPS = 1e-6
NEG = -30000.0


class Dep:
    __slots__ = ("w", "rs")

    def __init__(self):
        self.w = None
        self.rs = {}


class Eng:
    def __init__(self, kb, eng, name, sync_self):
        self.kb = kb
        self.eng = eng
        self.name = name
        self.sync_self = sync_self
        self.sem = kb.nc.alloc_semaphore(name=f"s_{name}_0")
        self.nep = 0
        self.cnt = 0
        self.waited = {}
        self.nins = 0
        self.allsems = [self.sem]

    def _collect(self, reads, writes):
        need = {}

        def add(ev):
            if ev is None:
                return
            sem, val, src = ev
            if src is self and not self.sync_self:
                return
            k = id(sem)
            if self.waited.get(k, 0) >= val:
                return
            if k not in need or need[k][1] < val:
                need[k] = (sem, val)

        for d in reads:
            add(d.w)
        for d in writes:
            add(d.w)
            for ev in d.rs.values():
                add(ev)
        return list(need.values())

    def op(self, fn, r=(), w=(), dma=False):
        items = self._collect(r, w)
        if dma:
            sem, val, prev = self.kb.dma_slot("sw" if self.name == "pool" else "hw")
            if prev > 0 and self.waited.get(id(sem), 0) < prev:
                items.append((sem, prev))
        for (s, v) in items[1:]:
            self.eng.wait_ge(s, v)
            self.waited[id(s)] = max(self.waited.get(id(s), 0), v)
        ins = fn(self.eng)
        if items:
            s, v = items[0]
            ins._wait_ge(s, v)
            self.waited[id(s)] = max(self.waited.get(id(s), 0), v)
        self.nins += 1
        if dma:
            ins.then_inc(sem, 16)
            ev = (sem, val, None)
        else:
            if self.cnt >= EPOCH:
                self.nep += 1
                self.sem = self.kb.nc.alloc_semaphore(name=f"s_{self.name}_{self.nep}")
                self.allsems.append(self.sem)
                self.cnt = 0
            self.cnt += 1
            ins.then_inc(self.sem, 1)
            ev = (self.sem, self.cnt, self)
        for d in r:
            d.rs[id(ev[0])] = ev
        for d in w:
            d.w = ev
            d.rs = {}
        return ev

    def wait_sv(self, sem, val):
        if val <= 0 or self.waited.get(id(sem), 0) >= val:
            return
        self.eng.wait_ge(sem, val)
        self.waited[id(sem)] = val


class KB:
    def __init__(self, nc, n_dma_sems=48):
        self.nc = nc
        self.pe = Eng(self, nc.tensor, "pe", False)
        self.act = Eng(self, nc.scalar, "act", True)
        self.dve = Eng(self, nc.vector, "dve", True)
        self.pool = Eng(self, nc.gpsimd, "pool", True)
        self.sp = Eng(self, nc.sync, "sp", True)
        self.engs = [self.pe, self.act, self.dve, self.pool, self.sp]
        self.dsems_hw = [[nc.alloc_semaphore(name=f"dmah{i}"), 0] for i in range(32)]
        self.dsems_sw = [[nc.alloc_semaphore(name=f"dmas{i}"), 0] for i in range(24)]
        self.dsems = self.dsems_hw + self.dsems_sw
        self.dnext = {"hw": 0, "sw": 0}
        self.nt = 0

    def dma_slot(self, kind):
        lst = self.dsems_hw if kind == "hw" else self.dsems_sw
        slot = lst[self.dnext[kind]]
        self.dnext[kind] = (self.dnext[kind] + 1) % len(lst)
        prev = slot[1]
        slot[1] += 16
        return slot[0], slot[1], prev

    def barrier(self, engs=None):
        for e in (engs or self.engs):
            for x in self.engs:
                if x is e:
                    continue
                e.wait_sv(x.sem, x.cnt)
            for sem, val in self.dsems:
                e.wait_sv(sem, val)


class T:
    def __init__(self, t):
        self.t = t
        self.d = Dep()

    def __getitem__(self, k):
        return self.t[k]


class Scope:
    def __init__(self, kb):
        self.kb = kb
        self.st = contextlib.ExitStack()

    def sb(self, shape, dtype, name=None):
        self.kb.nt += 1
        t = self.st.enter_context(self.kb.nc.sbuf_tensor(f"sb{self.kb.nt}_{name or 't'}", list(shape), dtype))
        return T(t)

    def close(self):
        self.kb.barrier()
        self.st.close()


def build(layers=(0, 1), dbg=None):
    nc = bass.Bass("TRN2", target_bir_lowering=False)
    kb = KB(nc)
    pe, act, dve, pool, sp = kb.pe, kb.act, kb.dve, kb.pool, kb.sp
    L0 = 0 in layers
    L1 = 1 in layers

    def din(name, shape, dt=F32):
        return nc.dram_tensor(name, list(shape), dt, kind="ExternalInput").ap()

    xo_d = din("xo", [NT * 128, D])
    xp_d = din("xp", [NPRE * 128, D])
    cst_d = din("cst", [128, 640])
    selall_d = din("selall", [16, 2048])
    pc_d = din("pc", [128, 68])
    sm_d = din("smalls", [128, 454])
    gains_d = din("gains", [4, 128, D])
    ev_w_in_d = din("ev_w_in", [D, 2828])
    ev_wsT_d = din("ev_wsT", [4, 128, 128])
    ev_w_out_d = din("ev_w_out", [D, D])
    od_w_in_d = din("od_w_in", [D, 2048])
    od_w_pool_d = din("od_w_pool", [4, 128, 128])
    od_w_out_d = din("od_w_out", [D, D])
    wr_d = din("wr", [2, D, 20])
    wg_d = din("moe_w_gate", [2, 16, D, 256])
    wu_d = din("moe_w_up", [2, 16, D, 256])
    wd_d = din("moe_w_down", [2, 16, 256, D])
    if L1:
        out_d = nc.dram_tensor("out", [16 * 128, D], F32, kind="ExternalOutput").ap()
    else:
        out_d = nc.dram_tensor("out", [NT * 128, D], F32, kind="ExternalOutput").ap()
    dbg_d = None
    if dbg:
        dbg_d = nc.dram_tensor("dbg", [NT * 128, D], F32, kind="ExternalOutput").ap()
    kT_d = T(nc.dram_tensor("kT_scr", [12, 70, NSEQ * 128], BF16).ap())
    qT_d = T(nc.dram_tensor("qT_scr", [12, 70, NT * 128], BF16).ap())
    V_d = T(nc.dram_tensor("V_scr", [NSEQ, 128, 780], BF16).ap())

    pbig = [nc.alloc_psum_tensor(f"bankpair{i}", [128, 1024], F32) for i in range(4)]
    pb = [T(pbig[i // 2][:, (i % 2) * 512:(i % 2 + 1) * 512]) for i in range(8)]

    def pbf(i):
        return pb[i][:].bitcast(BF16).rearrange("p (a b) -> p a b", b=128)

    G = Scope(kb)
    xres = G.sb([128, NT, D], F32, "xres")
    xres_d = [Dep() for _ in range(NT)]
    cst32 = G.sb([128, 640], F32, "cst32")
    cstb = G.sb([128, 640], BF16, "cstb")
    pc = G.sb([128, 68], F32, "pc")
    sm = G.sb([128, 454], F32, "smalls_sb")
    ident_b = cstb[:, 0:128]
    maskT_b = cstb[:, 384:512]
    shift_b = cstb[0:64, 512:640]
    ident_f = cst32[:, 0:128]
    U_f = cst32[:, 128:256]
    sel127_f = cst32[:, 256:384]
    ones_f = G.sb([128, 128], F32, "ones_f")
    bfb = sm[:, 0:12]
    gq_b = sm[:, 12:76]
    gk_b = sm[:, 76:140]
    gvcol = sm[:, 140:142]
    bsl = sm[:, 142:398]
    cw = sm[:, 398:410]
    pscale = sm[:, 410:414]
    brt = sm[:, 414:454]
    valid = pc[:, 0:1]
    nvalid = pc[:, 1:2]
    icnt = pc[:, 4:68]

    sp.op(lambda e: e.dma_start(out=cst32[:], in_=cst_d), w=[cst32.d], dma=True)
    pool.op(lambda e: e.dma_start(out=cstb[:], in_=cst_d), w=[cstb.d], dma=True)
    sp.op(lambda e: e.dma_start(out=pc[:], in_=pc_d), w=[pc.d], dma=True)
    sp.op(lambda e: e.dma_start(out=sm[:], in_=sm_d), w=[sm.d], dma=True)
    dve.op(lambda e: e.memset(ones_f[:], 1.0), w=[ones_f.d])

    def load_xres():
        for j in range(NT):
            sp.op(lambda e: e.dma_start(out=xres[:, j, :], in_=xo_d[j * 128:(j + 1) * 128, :]), w=[xres_d[j]], dma=True)

    def rstd_from_ss(S, ss, n, scale, width):
        dve.op(lambda e: e.tensor_scalar(out=ss[:, 0:width], in0=ss[:, 0:width], scalar1=1.0 / n, scalar2=EPS,
                                         op0=ALU.mult, op1=ALU.add), r=[ss.d], w=[ss.d])
        act.op(lambda e: e.activation(out=ss[:, 0:width], in_=ss[:, 0:width], func=AF.Ln), r=[ss.d], w=[ss.d])
        act.op(lambda e: e.activation(out=ss[:, 0:width], in_=ss[:, 0:width], func=AF.Exp, scale=-0.5), r=[ss.d], w=[ss.d])

    def nt_a(xsrc_ap, xsrc_dep, gn, ss, xs):
        act.op(lambda e: e.activation(out=xs[:], in_=xsrc_ap, func=AF.Square, accum_out=ss[:, 0:1]),
               r=[xsrc_dep], w=[xs.d, ss.d])
        rstd_from_ss(None, ss, D, 1.0, 1)
        dve.op(lambda e: e.scalar_tensor_tensor(out=xs[:], in0=xsrc_ap, scalar=ss[:, 0:1], in1=gn[:],
                                                op0=ALU.mult, op1=ALU.mult), r=[xsrc_dep, ss.d, gn.d], w=[xs.d])

    def nt_pe(xs, bank):
        pv = pbf(bank)
        for c in range(8):
            pe.op(lambda e: e.transpose(out=pv[:, c, :], in_=xs[:, c * 128:(c + 1) * 128], identity=ident_b),
                  r=[xs.d, cstb.d], w=[pb[bank].d])

    def nt_copy(dstT_ap, dst_dep, bank, on_dve=False):
        if on_dve:
            dve.op(lambda e: e.tensor_copy(out=dstT_ap, in_=pbf(bank)), r=[pb[bank].d], w=[dst_dep])
        else:
            act.op(lambda e: e.copy(out=dstT_ap, in_=pbf(bank)), r=[pb[bank].d], w=[dst_dep])

    def norm_transpose(S, xsrc_ap, xsrc_dep, gn, junk, ss, xs, dstT_ap, dst_dep, bank):
        nt_a(xsrc_ap, xsrc_dep, gn, ss, xs)
        nt_pe(xs, bank)
        nt_copy(dstT_ap, dst_dep, bank)

    def nt_sequence(tile_src, n, gn, ss2, xs2, dst, after=None):
        a0, d0 = tile_src(0)
        nt_a(a0, d0, gn, ss2[0], xs2[0])
        for i in range(n):
            bank = 0 if i % 2 == 0 else 2
            nt_pe(xs2[i % 2], bank)
            if i + 1 < n:
                a1, d1 = tile_src(i + 1)
                nt_a(a1, d1, gn, ss2[(i + 1) % 2], xs2[(i + 1) % 2])
            da, dd = dst(i)
            nt_copy(da, dd, bank, on_dve=(i % 2 == 0))
            if after is not None and i >= 1:
                after(i - 1)
        if after is not None:
            after(n - 1)

    def load_w_bf16(dst, src_ap, nchunk, ncols, colsplit=1024):
        for c0 in range(0, ncols, colsplit):
            c1 = min(ncols, c0 + colsplit)
            pool.op(lambda e: e.dma_start(out=dst[:, :, c0:c1],
                                          in_=src_ap[:, c0:c1].rearrange("(c p) n -> p c n", p=128)),
                    w=[dst.d], dma=True)

    def dbg_dump():
        if dbg_d is None:
            return
        for j in range(NT):
            sp.op(lambda e: e.dma_start(out=dbg_d[j * 128:(j + 1) * 128, :], in_=xres[:, j, :]), r=[xres_d[j]], dma=True)

    def moe_layer(l, tiles, on_final=None):
        t0 = tiles[0]
        ntl = len(tiles)
        ntok = ntl * 128
        S = Scope(kb)
        gn = S.sb([128, D], F32)
        sp.op(lambda e: e.dma_start(out=gn[:], in_=gains_d[(1 if l == 0 else 3)]), w=[gn.d], dma=True)
        wr = S.sb([128, 8, 20], BF16)
        pool.op(lambda e: e.dma_start(out=wr[:], in_=wr_d[l].rearrange("(c p) n -> p c n", p=128)), w=[wr.d], dma=True)
        xnT = S.sb([128, 8, ntok], BF16)
        xnT_d = [Dep() for _ in range(ntl)]
        lg = S.sb([128, ntl, 20], F32)
        ss2 = [S.sb([128, 4], F32) for _ in range(2)]
        xs2 = [S.sb([128, D], BF16) for _ in range(2)]
        wg = [S.sb([128, 8, 256], BF16) for _ in range(2)]
        wu = [S.sb([128, 8, 256], BF16) for _ in range(2)]
        wd = [S.sb([128, 2, D], BF16) for _ in range(2)]

        def load_expert(e_):
            b = e_ % 2
            pool.op(lambda e: e.dma_start(out=wg[b][:], in_=wg_d[l, e_].rearrange("(c p) n -> p c n", p=128)), w=[wg[b].d], dma=True)
            pool.op(lambda e: e.dma_start(out=wu[b][:], in_=wu_d[l, e_].rearrange("(c p) n -> p c n", p=128)), w=[wu[b].d], dma=True)
            pool.op(lambda e: e.dma_start(out=wd[b][:], in_=wd_d[l, e_].rearrange("(c p) n -> p c n", p=128)), w=[wd[b].d], dma=True)

        load_expert(0)
        load_expert(1)
        def router_(i):
            for c in range(8):
                pe.op(lambda e: e.matmul(pb[1][:, 0:20], lhsT=xnT[:, c, i * 128:(i + 1) * 128], rhs=wr[:, c, :],
                                         start=(c == 0), stop=(c == 7)), r=[xnT_d[i], wr.d], w=[pb[1].d])
            dve.op(lambda e: e.tensor_tensor(out=lg[:, i, :], in0=pb[1][:, 0:20], in1=brt[:, l * 20:(l + 1) * 20], op=ALU.add),
                   r=[pb[1].d, sm.d], w=[lg.d])
        nt_sequence(lambda i: (xres[:, tiles[i], :], xres_d[tiles[i]]), ntl, gn, ss2, xs2,
                    lambda i: (xnT[:, :, i * 128:(i + 1) * 128], xnT_d[i]), after=router_)
        n4 = [128, ntl, 4]
        gmax = S.sb([128, ntl], F32)
        ge = S.sb(n4, F32)
        gsum = S.sb([128, ntl], F32)
        gsel = S.sb(n4, F32)
        tmp16 = S.sb([128, ntl, 16], F32)
        el = S.sb(n4, F32)
        emax = S.sb([128, ntl], F32)
        ee = S.sb(n4, F32)
        oh1 = S.sb(n4, F32)
        ee2 = S.sb(n4, F32)
        m2 = S.sb([128, ntl], F32)
        oh2 = S.sb(n4, F32)
        w1 = S.sb([128, ntl], F32)
        w2 = S.sb([128, ntl], F32)
        cw4 = S.sb(n4, F32)
        comb = S.sb([128, ntl, 16], F32)
        glog = lg[:, :, 0:4]
        elog = lg[:, :, 4:20]

        def bc_last(ap2, n):
            return ap2.unsqueeze(2).to_broadcast([128, ntl, n])

        dve.op(lambda e: e.tensor_reduce(out=gmax[:], in_=glog, axis=AX.X, op=ALU.max), r=[lg.d], w=[gmax.d])
        dve.op(lambda e: e.tensor_tensor(out=ge[:], in0=glog, in1=bc_last(gmax[:], 4), op=ALU.subtract), r=[lg.d, gmax.d], w=[ge.d])
        dve.op(lambda e: e.tensor_single_scalar(out=gsel[:], in_=ge[:], scalar=0.0, op=ALU.is_ge), r=[ge.d], w=[gsel.d])
        act.op(lambda e: e.activation(out=ge[:], in_=ge[:], func=AF.Exp), r=[ge.d], w=[ge.d])
        dve.op(lambda e: e.tensor_reduce(out=gsum[:], in_=ge[:], axis=AX.X, op=ALU.add), r=[ge.d], w=[gsum.d])
        dve.op(lambda e: e.reciprocal(out=gsum[:], in_=gsum[:]), r=[gsum.d], w=[gsum.d])
        dve.op(lambda e: e.tensor_tensor(out=tmp16[:].rearrange("p t (g e) -> p t g e", e=4),
                                         in0=elog.rearrange("p t (g e) -> p t g e", e=4),
                                         in1=gsel[:].unsqueeze(3).to_broadcast([128, ntl, 4, 4]), op=ALU.mult),
               r=[lg.d, gsel.d], w=[tmp16.d])
        dve.op(lambda e: e.tensor_reduce(out=el[:], in_=tmp16[:].rearrange("p t (g e) -> p t e g", e=4), axis=AX.X, op=ALU.add),
               r=[tmp16.d], w=[el.d])
        dve.op(lambda e: e.tensor_reduce(out=emax[:], in_=el[:], axis=AX.X, op=ALU.max), r=[el.d], w=[emax.d])
        dve.op(lambda e: e.tensor_tensor(out=ee[:], in0=el[:], in1=bc_last(emax[:], 4), op=ALU.subtract), r=[el.d, emax.d], w=[ee.d])
        dve.op(lambda e: e.tensor_single_scalar(out=oh1[:], in_=ee[:], scalar=0.0, op=ALU.is_ge), r=[ee.d], w=[oh1.d])
        act.op(lambda e: e.activation(out=ee[:], in_=ee[:], func=AF.Exp), r=[ee.d], w=[ee.d])
        dve.op(lambda e: e.scalar_tensor_tensor(out=ee2[:], in0=oh1[:], scalar=-4.0, in1=ee[:], op0=ALU.mult, op1=ALU.add),
               r=[oh1.d, ee.d], w=[ee2.d])
        dve.op(lambda e: e.tensor_reduce(out=m2[:], in_=ee2[:], axis=AX.X, op=ALU.max), r=[ee2.d], w=[m2.d])
        dve.op(lambda e: e.tensor_tensor(out=oh2[:], in0=ee2[:], in1=bc_last(m2[:], 4), op=ALU.is_ge), r=[ee2.d, m2.d], w=[oh2.d])
        dve.op(lambda e: e.tensor_scalar(out=w1[:], in0=m2[:], scalar1=1.0, scalar2=None, op0=ALU.add), r=[m2.d], w=[w1.d])
        dve.op(lambda e: e.reciprocal(out=w1[:], in_=w1[:]), r=[w1.d], w=[w1.d])
        dve.op(lambda e: e.tensor_tensor(out=w1[:], in0=w1[:], in1=gsum[:], op=ALU.mult), r=[w1.d, gsum.d], w=[w1.d])
        dve.op(lambda e: e.tensor_tensor(out=w2[:], in0=w1[:], in1=m2[:], op=ALU.mult), r=[w1.d, m2.d], w=[w2.d])
        dve.op(lambda e: e.tensor_tensor(out=cw4[:], in0=oh1[:], in1=bc_last(w1[:], 4), op=ALU.mult), r=[oh1.d, w1.d], w=[cw4.d])
        dve.op(lambda e: e.tensor_tensor(out=oh2[:], in0=oh2[:], in1=bc_last(w2[:], 4), op=ALU.mult), r=[oh2.d, w2.d], w=[oh2.d])
        dve.op(lambda e: e.tensor_tensor(out=cw4[:], in0=cw4[:], in1=oh2[:], op=ALU.add), r=[cw4.d, oh2.d], w=[cw4.d])
        dve.op(lambda e: e.tensor_copy(out=comb[:].rearrange("p t (g e) -> p t g e", e=4),
                                       in_=cw4[:].unsqueeze(2).to_broadcast([128, ntl, 4, 4])), r=[cw4.d], w=[comb.d])
        dve.op(lambda e: e.tensor_tensor(out=comb[:].rearrange("p t (g e) -> p t g e", e=4),
                                         in0=comb[:].rearrange("p t (g e) -> p t g e", e=4),
                                         in1=gsel[:].unsqueeze(3).to_broadcast([128, ntl, 4, 4]), op=ALU.mult),
               r=[comb.d, gsel.d], w=[comb.d])
        groups = []
        c0 = 0
        while c0 < ntok:
            n = min(512, ntok - c0)
            groups.append((c0, n))
            c0 += n
        sg = [S.sb([128, 512], F32) for _ in range(2)]
        hid = [[S.sb([128, 512], BF16) for _ in range(2)] for _ in range(2)]
        items = [(e_, tok0, n) for e_ in range(16) for (tok0, n) in groups]
        ybank = [0]

        def GU(i):
            e_, tok0, n = items[i]
            b = e_ % 2
            k2 = i % 2
            for hc in range(2):
                gb, ub = pb[hc * 2], pb[hc * 2 + 1]
                xd = [xnT_d[i_] for i_ in range(tok0 // 128, (tok0 + n) // 128)]
                for c in range(8):
                    pe.op(lambda e: e.matmul(gb[:, 0:n], lhsT=wg[b][:, c, hc * 128:(hc + 1) * 128], rhs=xnT[:, c, tok0:tok0 + n],
                                             start=(c == 0), stop=(c == 7)), r=[wg[b].d] + xd, w=[gb.d])
                for c in range(8):
                    pe.op(lambda e: e.matmul(ub[:, 0:n], lhsT=wu[b][:, c, hc * 128:(hc + 1) * 128], rhs=xnT[:, c, tok0:tok0 + n],
                                             start=(c == 0), stop=(c == 7)), r=[wu[b].d] + xd, w=[ub.d])
                act.op(lambda e: e.activation(out=sg[hc][:, 0:n], in_=gb[:, 0:n], func=AF.Silu), r=[gb.d], w=[sg[hc].d])
                dve.op(lambda e: e.tensor_tensor(out=hid[k2][hc][:, 0:n], in0=sg[hc][:, 0:n], in1=ub[:, 0:n], op=ALU.mult),
                       r=[sg[hc].d, ub.d], w=[hid[k2][hc].d])

        def DOWN(i):
            e_, tok0, n = items[i]
            b = e_ % 2
            k2 = i % 2
            for tt in range(n // 128):
                j = t0 + tok0 // 128 + tt
                for nn in range(2):
                    yb = pb[5 + ybank[0]]
                    ybank[0] = (ybank[0] + 1) % 3
                    for hc in range(2):
                        pe.op(lambda e: e.matmul(yb[:, :], lhsT=hid[k2][hc][:, tt * 128:(tt + 1) * 128], rhs=wd[b][:, hc, nn * 512:(nn + 1) * 512],
                                                 start=(hc == 0), stop=(hc == 1)), r=[hid[k2][hc].d, wd[b].d], w=[yb.d])
                    ti = tok0 // 128 + tt
                    dve.op(lambda e: e.scalar_tensor_tensor(out=xres[:, j, nn * 512:(nn + 1) * 512], in0=yb[:, :], scalar=comb[:, ti, e_:e_ + 1],
                                                            in1=xres[:, j, nn * 512:(nn + 1) * 512], op0=ALU.mult, op1=ALU.add),
                           r=[yb.d, comb.d, xres_d[j]], w=[xres_d[j]])
                if on_final is not None and e_ == 15:
                    on_final(j)
            if (i + 1 == len(items) or items[i + 1][0] != e_) and e_ + 2 < 16:
                load_expert(e_ + 2)

        GU(0)
        for i in range(len(items)):
            if i + 1 < len(items):
                GU(i + 1)
            DOWN(i)
        S.close()

    def wout_phase(S, mixT, mix_d, w_src, tiles, col_of_tile, wo=None):
        if wo is None:
            wo = S.sb([128, 8, D], BF16)
            load_w_bf16(wo, w_src, 8, D)
        bank = 0
        for j in tiles:
            cc = col_of_tile(j)
            for nn in range(2):
                yb = pb[bank]
                bank = (bank + 1) % 4
                for c in range(8):
                    pe.op(lambda e: e.matmul(yb[:, :], lhsT=mixT[:, c, cc:cc + 128], rhs=wo[:, c, nn * 512:(nn + 1) * 512],
                                             start=(c == 0), stop=(c == 7)), r=[mix_d, wo.d], w=[yb.d])
                dve.op(lambda e: e.tensor_tensor(out=xres[:, j, nn * 512:(nn + 1) * 512], in0=xres[:, j, nn * 512:(nn + 1) * 512],
                                                 in1=yb[:, :], op=ALU.add), r=[yb.d, xres_d[j]], w=[xres_d[j]])

    def even_mixer():
        SL = Scope(kb)
        mixT = SL.sb([128, 8, NT * 128], BF16, "mixT0")
        S = Scope(kb)
        gn = S.sb([128, D], F32)
        sp.op(lambda e: e.dma_start(out=gn[:], in_=gains_d[0]), w=[gn.d], dma=True)
        load_xres()
        wev = S.sb([128, 8, 2828], BF16)
        for (c0_, c1_) in ((1280, 2054), (2054, 2828)):
            pool.op(lambda e: e.dma_start(out=wev[:, :, c0_:c1_], in_=ev_w_in_d[:, c0_:c1_].rearrange("(c p) n -> p c n", p=128)), w=[wev.d], dma=True)
        wev_q = Dep()
        wsT = S.sb([128, 4, 128], BF16)
        pool.op(lambda e: e.dma_start(out=wsT[:], in_=ev_wsT_d.rearrange("g s t -> s g t")), w=[wsT.d], dma=True)
        Ub = cstb[:, 128:256]
        dve.op(lambda e: e.tensor_tensor(out=wsT[:], in0=wsT[:], in1=Ub.unsqueeze(1).to_broadcast([128, 4, 128]), op=ALU.mult),
               r=[wsT.d, cstb.d], w=[wsT.d])
        gqk = S.sb([128, 64], F32)
        dve.op(lambda e: e.scalar_tensor_tensor(out=gqk[:], in0=gq_b, scalar=0.125, in1=gk_b, op0=ALU.mult, op1=ALU.mult),
               r=[sm.d], w=[gqk.d])
        xin = [S.sb([128, D], F32) for _ in range(2)]
        ss2 = [S.sb([128, 4], F32) for _ in range(2)]
        xs2 = [S.sb([128, D], BF16) for _ in range(2)]
        xnT = [S.sb([128, 8, 128], BF16) for _ in range(2)]
        sq = S.sb([128, 768], BF16)
        Kraw = S.sb([128, 768], BF16)
        Qraw = S.sb([128, 768], BF16)
        ssall = S.sb([128, 28], F32)
        kaug = [S.sb([128, 12, 70], BF16) for _ in range(2)]
        qaug = [S.sb([128, 12, 70], BF16) for _ in range(2)]
        vaug = [S.sb([128, 12, 65], BF16) for _ in range(2)]
        kTs = [S.sb([70, 12, 128], BF16) for _ in range(2)]
        qTs = [S.sb([70, 12, 128], BF16) for _ in range(2)]
        zt = S.sb([128, 12], F32)
        ls = S.sb([128, 12], F32)
        Fc = [S.sb([128, 12], F32) for _ in range(2)]
        Fh = S.sb([128, 12], BF16)
        Fm = S.sb([128, 12], BF16)
        Fl = S.sb([128, 12], BF16)
        r1 = S.sb([128, 12], F32)
        qtmp = sq
        gv = S.sb([128, 256], F32)
        vpad = S.sb([128, 4, 128], BF16)
        guT = [S.sb([128, 256], F32) for _ in range(2)]
        stmp = S.sb([128, 256], F32)
        gv2 = stmp
        for b_ in range(2):
            dve.op(lambda e: e.memset(kaug[b_][:, :, 64:67], 1.0), w=[kaug[b_].d])
            dve.op(lambda e: e.memset(qaug[b_][:, :, 67:70], 1.0), w=[qaug[b_].d])
        dve.op(lambda e: e.memset(vpad[:], 0.0), w=[vpad.d])

        def own_(i):
            return i >= NPRE

        def S1a(i):
            b2 = i % 2
            if own_(i):
                j = i - NPRE
                xsrc, xdep = xres[:, j, :], xres_d[j]
            else:
                pool.op(lambda e: e.dma_start(out=xin[b2][:], in_=xp_d[i * 128:(i + 1) * 128, :]), w=[xin[b2].d], dma=True)
                xsrc, xdep = xin[b2][:], xin[b2].d
            nt_a(xsrc, xdep, gn, ss2[b2], xs2[b2])

        def S1b(i):
            b2 = i % 2
            nt_pe(xs2[b2], 0)
            nt_copy(xnT[b2][:], xnT[b2].d, 0, on_dve=(i % 2 == 0))

        def S2(i):
            xT = xnT[i % 2]

            def proj(bank, c_lo, c_hi, w_lo):
                n = c_hi - c_lo
                wdep = wev.d if w_lo >= 1280 else wev_q
                for c in range(8):
                    pe.op(lambda e: e.matmul(pb[bank][:, c_lo:c_hi], lhsT=xT[:, c, :], rhs=wev[:, c, w_lo:w_lo + n],
                                             start=(c == 0), stop=(c == 7)), r=[xT.d, wdep], w=[pb[bank].d])
            proj(1, 0, 512, 1280)
            proj(2, 0, 256, 1792)
            proj(2, 256, 512, 2048)
            proj(3, 0, 512, 2304)
            proj(4, 0, 12, 2816)
            if own_(i):
                proj(5, 0, 512, 512)
                proj(6, 0, 256, 1024)
                proj(6, 256, 512, 256)
                for uc in range(2):
                    for c in range(8):
                        pe.op(lambda e: e.matmul(pb[7][:, uc * 128:(uc + 1) * 128], lhsT=wev[:, c, uc * 128:(uc + 1) * 128], rhs=xT[:, c, :],
                                                 start=(c == 0), stop=(c == 7)), r=[xT.d, wev_q], w=[pb[7].d])

        def S3(i):
            b2 = i % 2
            vflag = valid if i <= NPRE else 1.0
            nvflag = nvalid if i <= NPRE else -1.0
            va = vaug[b2]
            dve.op(lambda e: e.tensor_tensor(out=zt[:], in0=pb[4][:, 0:12], in1=bfb, op=ALU.add), r=[pb[4].d, sm.d], w=[zt.d])
            act.op(lambda e: e.activation(out=zt[:], in_=zt[:], func=AF.Exp, scale=-1.0), r=[zt.d], w=[zt.d])
            act.op(lambda e: e.activation(out=Kraw[:, 0:512], in_=pb[1][:, 0:512], func=AF.Copy), r=[pb[1].d], w=[Kraw.d])
            act.op(lambda e: e.activation(out=Kraw[:, 512:768], in_=pb[2][:, 0:256], func=AF.Copy), r=[pb[2].d], w=[Kraw.d])
            dve.op(lambda e: e.tensor_scalar(out=zt[:], in0=zt[:], scalar1=1.0, scalar2=None, op0=ALU.add), r=[zt.d], w=[zt.d])
            act.op(lambda e: e.activation(out=zt[:], in_=zt[:], func=AF.Ln), r=[zt.d], w=[zt.d])
            dve.op(lambda e: e.tensor_scalar(out=va[:, 0:4, 0:64], in0=pb[2][:, 256:512].rearrange("p (h d) -> p h d", d=64),
                                             scalar1=vflag, scalar2=None, op0=ALU.mult), r=[pb[2].d, pc.d], w=[va.d])
            dve.op(lambda e: e.tensor_scalar(out=va[:, 4:12, 0:64], in0=pb[3][:, 0:512].rearrange("p (h d) -> p h d", d=64),
                                             scalar1=vflag, scalar2=None, op0=ALU.mult), r=[pb[3].d, pc.d], w=[va.d])
            dve.op(lambda e: e.tensor_scalar(out=va[:, :, 64], in0=ones_f[:, 0:12], scalar1=vflag, scalar2=None, op0=ALU.mult),
                   r=[ones_f.d, pc.d], w=[va.d])
            sp.op(lambda e: e.dma_start(out=V_d[i], in_=va[:].rearrange("p h d -> p (h d)")), r=[va.d], w=[V_d.d], dma=True)
            dve.op(lambda e: e.tensor_scalar(out=ls[:], in0=zt[:], scalar1=nvflag, scalar2=None, op0=ALU.mult), r=[zt.d, pc.d], w=[ls.d])
            if own_(i):
                act.op(lambda e: e.activation(out=Qraw[:, 0:512], in_=pb[5][:, 0:512], func=AF.Copy), r=[pb[5].d], w=[Qraw.d])
                act.op(lambda e: e.activation(out=Qraw[:, 512:768], in_=pb[6][:, 0:256], func=AF.Copy), r=[pb[6].d], w=[Qraw.d])
                act.op(lambda e: e.activation(out=gv[:], in_=pb[6][:, 256:512], func=AF.Gelu_apprx_tanh), r=[pb[6].d], w=[gv.d])
                act.op(lambda e: e.activation(out=guT[b2][:], in_=pb[7][:, 0:256], func=AF.Gelu_apprx_tanh), r=[pb[7].d], w=[guT[b2].d])

        def S4(i):
            b2 = i % 2
            Fcur, Fprev = Fc[b2], Fc[1 - b2]
            ka, qa = kaug[b2], qaug[b2]
            pe.op(lambda e: e.matmul(pb[4][:, 16:28], lhsT=U_f, rhs=ls[:], start=True, stop=(i == 0)), r=[cst32.d, ls.d], w=[pb[4].d])
            if i > 0:
                pe.op(lambda e: e.matmul(pb[4][:, 16:28], lhsT=sel127_f, rhs=Fprev[:], start=False, stop=True), r=[cst32.d, Fprev.d], w=[pb[4].d])
            dve.op(lambda e: e.tensor_copy(out=Fcur[:], in_=pb[4][:, 16:28]), r=[pb[4].d], w=[Fcur.d])
            dve.op(lambda e: e.tensor_scalar(out=ka[:, :, 67], in0=Fcur[:], scalar1=-1.0, scalar2=None, op0=ALU.mult), r=[Fcur.d], w=[ka.d])
            dve.op(lambda e: e.tensor_tensor(out=r1[:], in0=Fcur[:], in1=ka[:, :, 67], op=ALU.add), r=[Fcur.d, ka.d], w=[r1.d])
            dve.op(lambda e: e.tensor_scalar(out=ka[:, :, 68], in0=r1[:], scalar1=-1.0, scalar2=None, op0=ALU.mult), r=[r1.d], w=[ka.d])
            dve.op(lambda e: e.tensor_tensor(out=r1[:], in0=r1[:], in1=ka[:, :, 68], op=ALU.add), r=[r1.d, ka.d], w=[r1.d])
            dve.op(lambda e: e.tensor_scalar(out=ka[:, :, 69], in0=r1[:], scalar1=-1.0, scalar2=None, op0=ALU.mult), r=[r1.d], w=[ka.d])
            if own_(i):
                dve.op(lambda e: e.tensor_scalar(out=qa[:, :, 64:67], in0=ka[:, :, 67:70], scalar1=-1.0, scalar2=None, op0=ALU.mult), r=[ka.d], w=[qa.d])

        def S5(i):
            b2 = i % 2
            ka, qa = kaug[b2], qaug[b2]
            own = own_(i)
            pool.op(lambda e: e.tensor_tensor(out=sq[:], in0=Kraw[:], in1=Kraw[:], op=ALU.mult), r=[Kraw.d], w=[sq.d])
            dve.op(lambda e: e.tensor_reduce(out=ssall[:, 0:12], in_=sq[:].rearrange("p (h d) -> p h d", d=64), axis=AX.X, op=ALU.add), r=[sq.d], w=[ssall.d])
            if own:
                pool.op(lambda e: e.tensor_tensor(out=sq[:], in0=Qraw[:], in1=Qraw[:], op=ALU.mult), r=[Qraw.d], w=[sq.d])
                dve.op(lambda e: e.tensor_reduce(out=ssall[:, 12:24], in_=sq[:].rearrange("p (h d) -> p h d", d=64), axis=AX.X, op=ALU.add), r=[sq.d], w=[ssall.d])
                pool.op(lambda e: e.tensor_tensor(out=gv2[:], in0=gv[:], in1=gv[:], op=ALU.mult), r=[gv.d], w=[gv2.d])
                dve.op(lambda e: e.tensor_reduce(out=ssall[:, 24:28], in_=gv2[:].rearrange("p (g d) -> p g d", d=64), axis=AX.X, op=ALU.add), r=[gv2.d], w=[ssall.d])
            rstd_from_ss(S, ssall, 64, 1.0, 28 if own else 12)
            dve.op(lambda e: e.tensor_tensor(out=ka[:, :, 0:64], in0=Kraw[:].rearrange("p (h d) -> p h d", d=64),
                                             in1=ssall[:, 0:12].unsqueeze(2).to_broadcast([128, 12, 64]), op=ALU.mult), r=[Kraw.d, ssall.d], w=[ka.d])
            if not own:
                return
            dve.op(lambda e: e.tensor_tensor(out=qtmp[:].rearrange("p (h d) -> p h d", d=64), in0=Qraw[:].rearrange("p (h d) -> p h d", d=64),
                                             in1=ssall[:, 12:24].unsqueeze(2).to_broadcast([128, 12, 64]), op=ALU.mult), r=[Qraw.d, ssall.d], w=[qtmp.d])
            dve.op(lambda e: e.tensor_tensor(out=qa[:, :, 0:64], in0=qtmp[:].rearrange("p (h d) -> p h d", d=64),
                                             in1=gqk[:].unsqueeze(1).to_broadcast([128, 12, 64]), op=ALU.mult), r=[qtmp.d, gqk.d], w=[qa.d])
            for g in range(4):
                off = 64 * (g % 2)
                dve.op(lambda e: e.tensor_scalar(out=vpad[:, g, off:off + 64], in0=gv[:, g * 64:(g + 1) * 64], scalar1=ssall[:, 24 + g:25 + g], scalar2=None,
                                                 op0=ALU.mult), r=[gv.d, ssall.d], w=[vpad.d])

        def S6(i):
            b2 = i % 2
            j = i - NPRE
            ka, qa = kaug[b2], qaug[b2]
            for h in range(12):
                bk = 1 if h < 8 else 2
                pe.op(lambda e: e.transpose(out=pbf(bk)[0:70, h % 8, :], in_=ka[:, h, :], identity=ident_b), r=[ka.d, cstb.d], w=[pb[bk].d])
            ks = kTs[b2]
            act.op(lambda e: e.copy(out=ks[:, 0:8, :], in_=pbf(1)[0:70, :, :]), r=[pb[1].d], w=[ks.d])
            dve.op(lambda e: e.tensor_copy(out=ks[:, 8:12, :], in_=pbf(2)[0:70, 0:4, :]), r=[pb[2].d], w=[ks.d])
            sp.op(lambda e: e.dma_start(out=kT_d[:, :, i * 128:(i + 1) * 128].rearrange("h r t -> r h t"), in_=ks[:]), r=[ks.d], w=[kT_d.d], dma=True)
            if not own_(i):
                return
            for h in range(12):
                bk = 5 if h < 8 else 6
                pe.op(lambda e: e.transpose(out=pbf(bk)[0:70, h % 8, :], in_=qa[:, h, :], identity=ident_b), r=[qa.d, cstb.d], w=[pb[bk].d])
            qs = qTs[b2]
            dve.op(lambda e: e.tensor_copy(out=qs[:, 0:8, :], in_=pbf(5)[0:70, :, :]), r=[pb[5].d], w=[qs.d])
            dve.op(lambda e: e.tensor_copy(out=qs[:, 8:12, :], in_=pbf(6)[0:70, 0:4, :]), r=[pb[6].d], w=[qs.d])
            sp.op(lambda e: e.dma_start(out=qT_d[:, :, j * 128:(j + 1) * 128].rearrange("h r t -> r h t"), in_=qs[:]), r=[qs.d], w=[qT_d.d], dma=True)
            for c in range(2):
                for gg in range(2):
                    g = 2 * c + gg
                    pe.op(lambda e: e.matmul(pb[4][:, 128 + c * 128:128 + (c + 1) * 128], lhsT=vpad[:, g, :], rhs=wsT[:, g, :], start=(gg == 0), stop=(gg == 1)),
                          r=[vpad.d, wsT.d], w=[pb[4].d])
            gu = guT[b2]
            for c in range(2):
                dve.op(lambda e: e.scalar_tensor_tensor(out=stmp[:, c * 128:(c + 1) * 128], in0=pb[4][:, 128 + c * 128:128 + (c + 1) * 128], scalar=gvcol[:, c:c + 1],
                                                        in1=bsl[:, c * 128:(c + 1) * 128], op0=ALU.mult, op1=ALU.add), r=[pb[4].d, sm.d], w=[stmp.d])
                dve.op(lambda e: e.tensor_tensor(out=mixT[:, c, j * 128:(j + 1) * 128], in0=stmp[:, c * 128:(c + 1) * 128], in1=gu[:, c * 128:(c + 1) * 128],
                                                 op=ALU.mult), r=[stmp.d, gu.d], w=[mixT.d])

        S1a(0)
        S1a(1)
        for (c0_, c1_) in ((0, 640), (640, 1280)):
            pool.op(lambda e: e.dma_start(out=wev[:, :, c0_:c1_], in_=ev_w_in_d[:, c0_:c1_].rearrange("(c p) n -> p c n", p=128)), w=[wev_q], dma=True)
        S1b(0)
        S2(0)
        S3(0)
        for i in range(NSEQ):
            if i + 2 < NSEQ:
                S1a(i + 2)
            if i + 1 < NSEQ:
                S1b(i + 1)
            S4(i)
            if i + 1 < NSEQ:
                S2(i + 1)
            S5(i)
            if i + 1 < NSEQ:
                S3(i + 1)
            S6(i)
        S.close()
        if CUT == 10:
            SL.close(); return

        SBC = Scope(kb)
        wo_ev = SBC.sb([128, 8, D], BF16)
        load_w_bf16(wo_ev, ev_w_out_d, 8, D)
        S = Scope(kb)
        kTh = [S.sb([70, NSEQ * 128], BF16) for _ in range(2)]
        qTh = [S.sb([70, NT * 128], BF16) for _ in range(2)]
        Vg = [S.sb([128, NSEQ, 324], BF16) for _ in range(2)]
        for b_ in range(2):
            pool.op(lambda e: e.memset(Vg[b_][:, :, 260:324], 0.0), w=[Vg[b_].d])
        PT = [S.sb([128, 1024], BF16) for _ in range(3)]
        rc = [S.sb([128, 512], F32) for _ in range(2)]
        bcs = [S.sb([64, 512], F32) for _ in range(2)]
        otmp = [S.sb([64, 512], BF16) for _ in range(2)]
        QC = [(0, 128), (128, 512), (640, 512), (1152, 512), (1664, 512)]
        STP = [pbig[0], pbig[1], pbig[2]]
        STD = [Dep(), Dep(), Dep()]
        for k_ in range(3):
            pb[2 * k_].d = STD[k_]
            pb[2 * k_ + 1].d = STD[k_]
        free_slots = [0, 1, 2]

        def take_slot():
            return free_slots.pop(0)

        def give_slot(k_):
            free_slots.append(k_)

        def load_head(h):
            b = h % 2
            sp.op(lambda e: e.dma_start(out=kTh[b][:], in_=kT_d[h]), r=[kT_d.d], w=[kTh[b].d], dma=True)
            sp.op(lambda e: e.dma_start(out=qTh[b][:], in_=qT_d[h]), r=[qT_d.d], w=[qTh[b].d], dma=True)

        def load_vgroup(g):
            b = g % 2
            sp.op(lambda e: e.dma_start(out=Vg[b][:, :, 0:260], in_=V_d[:, :, g * 260:(g + 1) * 260].rearrange("t p c -> p t c")),
                  r=[V_d.d], w=[Vg[b].d], dma=True)

        load_head(0)
        load_vgroup(0)
        load_head(1)
        load_vgroup(1)
        units = []
        nchunk = 0
        for h in range(12):
            for (q0, nq) in QC:
                jlo = q0 // 128
                jhi = jlo + nq // 128 - 1
                last = NPRE + jhi
                if nq == 128:
                    for k0 in range(0, last + 1, 4):
                        subs = [(kt, (kt - k0) * 128, 0, 128, kt == last) for kt in range(k0, k0 + 4)]
                        units.append((h, q0, nq, last, nchunk, subs, 0, 512))
                else:
                    nfull = NPRE + jlo
                    for kt in range(0, nfull, 2):
                        units.append((h, q0, nq, last, nchunk, [(kt, 0, 0, 512, False), (kt + 1, 512, 0, 512, False)], 0, 1024))
                    for dj in range(4):
                        units.append((h, q0, nq, last, nchunk, [(nfull + dj, 0, dj * 128, 512, True)], dj * 128, 512))
                nchunk += 1
        NU = len(units)
        deferred = []
        uslot = {}

        def emit_qk(ui):
            h, q0, nq, last, ck, subs, lo, hi = units[ui]
            kt_, qt_ = kTh[h % 2], qTh[h % 2]
            k_ = take_slot()
            uslot[ui] = k_
            ST, sd = STP[k_], STD[k_]
            for (kt, off, c0, n, diag) in subs:
                lhs = kt_[:, kt * 128:(kt + 1) * 128]
                if diag:
                    pe.op(lambda e: e.matmul(ST[:, off + c0:off + c0 + 128], lhsT=lhs, rhs=qt_[:, q0 + c0:q0 + c0 + 128], start=True, stop=False),
                          r=[kt_.d, qt_.d], w=[sd])
                    pe.op(lambda e: e.matmul(ST[:, off + c0:off + c0 + 128], lhsT=ident_b, rhs=maskT_b, start=False, stop=True),
                          r=[cstb.d], w=[sd])
                    if c0 + 128 < n:
                        pe.op(lambda e: e.matmul(ST[:, off + c0 + 128:off + n], lhsT=lhs, rhs=qt_[:, q0 + c0 + 128:q0 + n], start=True, stop=True),
                              r=[kt_.d, qt_.d], w=[sd])
                else:
                    pe.op(lambda e: e.matmul(ST[:, off + c0:off + n], lhsT=lhs, rhs=qt_[:, q0 + c0:q0 + n], start=True, stop=True),
                          r=[kt_.d, qt_.d], w=[sd])
            if ui + 1 == NU or units[ui + 1][0] != h:
                if h + 2 < 12:
                    load_head(h + 2)

        def emit_exp_pv(ui):
            h, q0, nq, last, ck, subs, lo, hi = units[ui]
            k_ = uslot[ui]
            ST, sd = STP[k_], STD[k_]
            ptb = PT[ui % 3]
            OB = pb[6 + ck % 2]
            vg_ = Vg[(h // 4) % 2]
            hv = (h % 4) * 65
            act.op(lambda e: e.activation(out=ptb[:, lo:hi], in_=ST[:, lo:hi], func=AF.Exp), r=[sd], w=[ptb.d])
            give_slot(k_)
            for (kt, off, c0, n, diag) in subs:
                pe.op(lambda e: e.matmul(OB[:, c0:n], lhsT=vg_[:, kt, hv:hv + 128], rhs=ptb[:, off + c0:off + n], start=(kt == 0), stop=(kt == last),
                                         skip_group_check=True), r=[vg_.d, ptb.d], w=[OB.d])
            if subs[-1][0] == last:
                if h == 3 and q0 == QC[-1][0]:
                    load_vgroup(2)
                k2 = ck % 2
                rc_, bcs_, ot_ = rc[k2], bcs[k2], otmp[k2]
                ch = 2 + h // 2
                dve.op(lambda e: e.tensor_scalar(out=rc_[64:65, 0:nq], in0=OB[64:65, 0:nq], scalar1=1e-30, scalar2=None, op0=ALU.add), r=[OB.d], w=[rc_.d])
                dve.op(lambda e: e.reciprocal(out=rc_[64:65, 0:nq], in_=rc_[64:65, 0:nq]), r=[rc_.d], w=[rc_.d])

                def stage2():
                    kk = take_slot()
                    give_slot(kk)
                    BB, bd = STP[kk], STD[kk]
                    pe.op(lambda e: e.matmul(BB[0:64, 0:nq], lhsT=ones_f[64:65, 0:64], rhs=rc_[64:65, 0:nq], start=True, stop=True), r=[ones_f.d, rc_.d], w=[bd])
                    dve.op(lambda e: e.tensor_copy(out=bcs_[:, 0:nq], in_=BB[0:64, 0:nq]), r=[bd], w=[bcs_.d])
                    if h % 2 == 0:
                        dve.op(lambda e: e.tensor_tensor(out=mixT[0:64, ch, q0:q0 + nq], in0=OB[0:64, 0:nq], in1=bcs_[:, 0:nq], op=ALU.mult),
                               r=[OB.d, bcs_.d], w=[mixT.d])
                    else:
                        dve.op(lambda e: e.tensor_tensor(out=ot_[:, 0:nq], in0=OB[0:64, 0:nq], in1=bcs_[:, 0:nq], op=ALU.mult),
                               r=[OB.d, bcs_.d], w=[ot_.d])

                def stage3():
                    if h % 2 == 1:
                        kk = take_slot()
                        give_slot(kk)
                        SB_, bd = STP[kk], STD[kk]
                        pe.op(lambda e: e.matmul(SB_[:, 0:nq], lhsT=shift_b, rhs=ot_[:, 0:nq], start=True, stop=True), r=[cstb.d, ot_.d], w=[bd])
                        dve.op(lambda e: e.tensor_copy(out=mixT[64:128, ch, q0:q0 + nq], in_=SB_[64:128, 0:nq]), r=[bd], w=[mixT.d])
                deferred.append((ui + 2, stage2))
                deferred.append((ui + 4, stage3))

        LA = 2
        for i in range(NU + LA):
            if i < NU:
                emit_qk(i)
            j = i - LA
            if j >= 0:
                emit_exp_pv(j)
            while deferred and deferred[0][0] <= j:
                deferred.pop(0)[1]()
        for _, fn in deferred:
            fn()
        for k_ in range(6):
            pb[k_].d = Dep()
        S.close()
        S = Scope(kb)
        wout_phase(S, mixT, mixT.d, ev_w_out_d, list(range(NT)), lambda j: j * 128, wo=wo_ev)
        S.close()
        SBC.close()
        SL.close()

    def odd_mixer():
        SL = Scope(kb)
        NTOK = 16 * 128
        mixT = SL.sb([128, 8, NTOK], BF16, "mixT1")
        wo_od = SL.sb([128, 8, D], BF16)
        S = Scope(kb)
        wod = S.sb([128, 8, 2048], BF16)
        load_w_bf16(wod, od_w_in_d, 8, 2048, colsplit=512)
        load_w_bf16(wo_od, od_w_out_d, 8, D)
        wpl = S.sb([128, 4, 128], BF16)
        xnT = S.sb([128, 8, NT * 128], BF16)
        xnT_d = [Dep() for _ in range(NT)]
        S1s = Scope(kb)
        wpl32 = S1s.sb([128, 4, 128], F32)
        sp.op(lambda e: e.dma_start(out=wpl32[:], in_=od_w_pool_d.rearrange("g c d -> c g d")), w=[wpl32.d], dma=True)
        dve.op(lambda e: e.tensor_copy(out=wpl[:], in_=wpl32[:]), r=[wpl32.d], w=[wpl.d])
        gn = S1s.sb([128, D], F32)
        sp.op(lambda e: e.dma_start(out=gn[:], in_=gains_d[2]), w=[gn.d], dma=True)
        ss2 = [S1s.sb([128, 4], F32) for _ in range(2)]
        xs2 = [S1s.sb([128, D], BF16) for _ in range(2)]
        nt_sequence(lambda j: (xres[:, j, :], xres_d[j]), NT, gn, ss2, xs2, lambda j: (xnT[:, :, j * 128:(j + 1) * 128], xnT_d[j]))
        S1s.close()
        if CUT <= 1:
            S.close(); SL.close(); return
        HL = 16
        W = HL + 512
        zb = [S.sb([128, W], F32) for _ in range(2)]
        pbuf = [S.sb([128, W], F32) for _ in range(2)]
        bgs = [S.sb([128, 512], BF16) for _ in range(2)]
        zc = S.sb([128, 4, HL], F32)
        pcar = S.sb([128, 4, HL], F32)
        cgs = S.sb([128, 512], BF16)
        y = S.sb([128, 512], F32)
        s1 = S.sb([128, W], F32)
        s2 = S.sb([128, W], F32)
        pooled = cgs
        dve.op(lambda e: e.memset(s1[:], 0.0), w=[s1.d])
        dve.op(lambda e: e.memset(s2[:], 0.0), w=[s2.d])
        groups = [(128 - HL, HL)] + [(128 + 512 * i, 512) for i in range(4)]
        work = [(gi, k) for gi in range(len(groups)) for k in range(4)]
        PB7 = [0, 1, 2, 3, 5, 6, 7]
        rot = [0]
        state = {}

        def nextbank():
            bk = PB7[rot[0] % 7]
            rot[0] += 1
            return bk

        def Pstage(n):
            gi, k = work[n]
            tok0, nn_ = groups[gi]
            xd = [xnT_d[i] for i in range(tok0 // 128, (tok0 + nn_ - 1) // 128 + 1)]
            banks = {}
            cols = [("cg", 512 + k * 128), ("hc", 1024 + k * 128), ("p", 1536 + k * 128)]
            if gi > 0:
                cols.append(("bg", k * 128))
            for nm, col in cols:
                bk = nextbank()
                banks[nm] = bk
                for c in range(8):
                    pe.op(lambda e: e.matmul(pb[bk][:, 0:nn_], lhsT=wod[:, c, col:col + 128], rhs=xnT[:, c, tok0:tok0 + nn_],
                                             start=(c == 0), stop=(c == 7)), r=[wod.d] + xd, w=[pb[bk].d])
            state[n] = banks

        def Estage(n):
            gi, k = work[n]
            tok0, nn_ = groups[gi]
            banks = state[n]
            z, p = zb[n % 2], pbuf[n % 2]
            act.op(lambda e: e.copy(out=cgs[:, 0:nn_], in_=pb[banks["cg"]][:, 0:nn_]), r=[pb[banks["cg"]].d], w=[cgs.d])
            if gi == 0:
                dve.op(lambda e: e.tensor_tensor(out=zc[:, k, :], in0=cgs[:, 0:nn_], in1=pb[banks["hc"]][:, 0:nn_], op=ALU.mult),
                       r=[cgs.d, pb[banks["hc"]].d], w=[zc.d])
                act.op(lambda e: e.copy(out=pcar[:, k, :], in_=pb[banks["p"]][:, 0:nn_]), r=[pb[banks["p"]].d], w=[pcar.d])
                return
            act.op(lambda e: e.copy(out=p[:, HL:W], in_=pb[banks["p"]][:, 0:nn_]), r=[pb[banks["p"]].d], w=[p.d])
            act.op(lambda e: e.copy(out=bgs[n % 2][:], in_=pb[banks["bg"]][:, 0:nn_]), r=[pb[banks["bg"]].d], w=[bgs[n % 2].d])
            dve.op(lambda e: e.tensor_tensor(out=z[:, HL:W], in0=cgs[:, 0:nn_], in1=pb[banks["hc"]][:, 0:nn_], op=ALU.mult),
                   r=[cgs.d, pb[banks["hc"]].d], w=[z.d])

        def Bstage(n):
            gi, k = work[n]
            if gi == 0:
                return
            tok0, nn_ = groups[gi]
            mcol = tok0 - 128
            z, p = zb[n % 2], pbuf[n % 2]
            dve.op(lambda e: e.tensor_copy(out=z[:, 0:HL], in_=zc[:, k, :]), r=[zc.d], w=[z.d])
            dve.op(lambda e: e.tensor_copy(out=p[:, 0:HL], in_=pcar[:, k, :]), r=[pcar.d], w=[p.d])
            dve.op(lambda e: e.tensor_copy(out=zc[:, k, :], in_=z[:, W - HL:W]), r=[z.d], w=[zc.d])
            dve.op(lambda e: e.tensor_copy(out=pcar[:, k, :], in_=p[:, W - HL:W]), r=[p.d], w=[pcar.d])
            dve.op(lambda e: e.tensor_scalar(out=y[:], in0=z[:, HL:W], scalar1=cw[:, k * 3 + 2:k * 3 + 3], scalar2=None, op0=ALU.mult),
                   r=[z.d, sm.d], w=[y.d])
            dve.op(lambda e: e.scalar_tensor_tensor(out=y[:], in0=z[:, HL - 1:W - 1], scalar=cw[:, k * 3 + 1:k * 3 + 2], in1=y[:], op0=ALU.mult, op1=ALU.add),
                   r=[z.d, sm.d, y.d], w=[y.d])
            dve.op(lambda e: e.scalar_tensor_tensor(out=y[:], in0=z[:, HL - 2:W - 2], scalar=cw[:, k * 3:k * 3 + 1], in1=y[:], op0=ALU.mult, op1=ALU.add),
                   r=[z.d, sm.d, y.d], w=[y.d])
            dve.op(lambda e: e.tensor_tensor(out=mixT[:, k, mcol:mcol + 512], in0=y[:], in1=bgs[n % 2][:], op=ALU.mult), r=[y.d, bgs[n % 2].d], w=[mixT.d])
            src, sd = p[:, :], p.d
            bufs = [s1, s2]
            sh = 1
            for m in range(k + 1):
                dst = bufs[m % 2]
                pool.op(lambda e: e.tensor_tensor(out=dst[:, sh:W], in0=src[:, sh:W], in1=src[:, 0:W - sh], op=ALU.add),
                        r=[sd], w=[dst.d])
                src, sd = dst[:, :], dst.d
                sh *= 2
            win = 2 ** (k + 1)
            dve.op(lambda e: e.scalar_tensor_tensor(out=pooled[:], in0=src[:, HL:W], scalar=1.0 / win, in1=p[:, HL:W], op0=ALU.mult, op1=ALU.subtract),
                   r=[sd, p.d], w=[pooled.d])
            if gi == 1:
                dve.op(lambda e: e.tensor_tensor(out=y[:, 0:16], in0=src[:, HL:HL + 16], in1=icnt[:, k * 16:(k + 1) * 16], op=ALU.mult),
                       r=[sd, pc.d, y.d], w=[y.d])
                dve.op(lambda e: e.tensor_tensor(out=pooled[:, 0:16], in0=y[:, 0:16], in1=p[:, HL:HL + 16], op=ALU.subtract),
                       r=[y.d, p.d], w=[pooled.d])
            pe.op(lambda e: e.matmul(pb[4][:, 0:512], lhsT=wpl[:, k, :], rhs=pooled[:], start=True, stop=True), r=[wpl.d, pooled.d], w=[pb[4].d])
            act.op(lambda e: e.activation(out=mixT[:, 4 + k, mcol:mcol + 512], in_=pb[4][:, 0:512], func=AF.Copy, scale=pscale[:, k:k + 1]),
                   r=[pb[4].d, sm.d], w=[mixT.d])

        NW = len(work)
        Pstage(0)
        Estage(0)
        for n in range(NW):
            if n + 1 < NW:
                Pstage(n + 1)
            Bstage(n)
            if n + 1 < NW:
                Estage(n + 1)
        S.close()
        if CUT <= 4:
            SL.close(); return
        S = Scope(kb)
        wout_phase(S, mixT, mixT.d, od_w_out_d, list(range(1, NT)), lambda j: (j - 1) * 128, wo=wo_od)
        S.close()
        SL.close()

    if L0:
        even_mixer()
        if dbg == "m0":
            dbg_dump()
        if CUT != 10:
            moe_layer(0, list(range(NT)))
        if dbg == "e0":
            dbg_dump()
    if L1:
        if not L0:
            load_xres()
        if CUT > 0:
            odd_mixer()
        if dbg == "m1":
            dbg_dump()
        out_evs = []
        def store_tile(j):
            out_evs.append(sp.op(lambda e: e.dma_start(out=out_d[(j - 1) * 128:j * 128, :], in_=xres[:, j, :]), r=[xres_d[j]], dma=True))
        if CUT > 5:
            moe_layer(1, list(range(1, NT)), on_final=store_tile)
    evs = []
    if L1:
        if CUT <= 5:
            for j in range(1, NT):
                evs.append(sp.op(lambda e: e.dma_start(out=out_d[(j - 1) * 128:j * 128, :], in_=xres[:, j, :]), r=[xres_d[j]], dma=True))
    else:
        for j in range(NT):
            evs.append(sp.op(lambda e: e.dma_start(out=out_d[j * 128:(j + 1) * 128, :], in_=xres[:, j, :]), r=[xres_d[j]], dma=True))
    kb.barrier([sp])
    return nc, kb


def _consts():
    cst = np.zeros((128, 640), np.float32)
    idx = np.arange(128)
    cst[:, 0:128] = np.eye(128, dtype=np.float32)
    cst[:, 128:256] = (idx[:, None] <= idx[None, :]).astype(np.float32)
    cst[127, 256:384] = 1.0
    cst[:, 384:512] = np.where(idx[:, None] <= idx[None, :], 0.0, NEG)
    cst[np.arange(64), 512 + 64 + np.arange(64)] = 1.0
    selall = np.zeros((16, 2048), np.float32)
    for e in range(16):
        selall[e, e * 128:(e + 1) * 128] = 1.0
    return cst, selall


def _percore(hf):
    pc = np.zeros((128, 68), np.float32)
    v = 1.0 if hf == 1 else 0.0
    pc[:, 0] = v
    pc[:, 1] = -v
    pc[:, 2] = 1.0
    pc[:, 3] = -1.0
    t = np.arange(16)
    for k, win in enumerate((2, 4, 8, 16)):
        cnt = np.minimum(t + 1, win) if hf == 0 else np.full(16, win)
        pc[:, 4 + k * 16:4 + (k + 1) * 16] = (1.0 / cnt)[None, :]
    return pc


def _make_inputs(inputs):
    f = lambda a: np.ascontiguousarray(np.asarray(a, dtype=np.float32))
    x = f(inputs["x"])
    cst, selall = _consts()
    p = np.arange(128)
    sm = np.zeros((128, 454), np.float32)
    sm[:, 0:12] = f(inputs["ev_b_forget"])[0][None, :]
    sm[:, 12:76] = f(inputs["ev_g_q"])[0][None, :]
    sm[:, 76:140] = f(inputs["ev_g_k"])[0][None, :]
    gv = f(inputs["ev_g_v"])[0]
    bs = f(inputs["ev_b_s"])[0]
    for c in range(2):
        sm[:, 140 + c] = gv[2 * c + p // 64, p % 64]
        sm[:, 142 + c * 128:142 + (c + 1) * 128] = bs[2 * c + p // 64, :]
    cwv = f(inputs["od_conv_w"])[0]
    for k in range(4):
        for tap in range(3):
            sm[:, 398 + k * 3 + tap] = cwv[tap, k * 128 + p]
        sm[:, 410 + k] = f(inputs["od_pool_scale"])[0][k * 128 + p]
    bg = f(inputs["moe_b_group"])
    br = f(inputs["moe_b_router"]).reshape(2, 16)
    for l in range(2):
        sm[:, 414 + l * 20:414 + l * 20 + 4] = bg[l][None, :]
        sm[:, 414 + l * 20 + 4:414 + (l + 1) * 20] = br[l][None, :]
    gains = np.stack([np.broadcast_to(f(inputs["ev_norm"])[0], (128, D)), np.broadcast_to(f(inputs["moe_norm"])[0], (128, D)),
                      np.broadcast_to(f(inputs["od_norm"])[0], (128, D)), np.broadcast_to(f(inputs["moe_norm"])[1], (128, D))]).astype(np.float32)
    wr = np.concatenate([f(inputs["moe_w_group"]), f(inputs["moe_w_router"]).reshape(2, D, 16)], axis=2)
    shared = {
        "cst": cst, "selall": selall, "smalls": sm, "gains": np.ascontiguousarray(gains),
        "ev_w_in": f(inputs["ev_w_in"])[0], "ev_wsT": np.ascontiguousarray(f(inputs["ev_w_s"])[0].transpose(0, 2, 1)),
        "ev_w_out": f(inputs["ev_w_out"])[0], "od_w_in": f(inputs["od_w_in"])[0], "od_w_pool": f(inputs["od_w_pool"])[0],
        "od_w_out": f(inputs["od_w_out"])[0], "wr": np.ascontiguousarray(wr),
        "moe_w_gate": f(inputs["moe_w_gate"]), "moe_w_up": f(inputs["moe_w_up"]), "moe_w_down": f(inputs["moe_w_down"]),
    }
    in_maps = []
    for c in range(8):
        b, hf = c // 2, c % 2
        m = dict(shared)
        m["pc"] = _percore(hf)
        if hf == 1:
            m["xp"] = np.ascontiguousarray(x[b, 0:NPRE * 128])
            m["xo"] = np.ascontiguousarray(x[b, NPRE * 128:4096])
        else:
            m["xp"] = np.zeros((NPRE * 128, D), np.float32)
            m["xo"] = np.ascontiguousarray(np.concatenate([np.zeros((128, D), np.float32), x[b, 0:2048]], axis=0))
        in_maps.append(m)
    return in_maps


_CACHE = {}


def kernel(**inputs):
    in_maps = _make_inputs(inputs)
    if "nc" not in _CACHE:
        _CACHE["nc"] = build((0, 1))[0]
    nc = _CACHE["nc"]
    res = run_bass_kernel_spmd(nc, in_maps, core_ids=list(range(8)))
    out = np.zeros((4, 4096, D), np.float32)
    for c in range(8):
        b, hf = c // 2, c % 2
        out[b, hf * 2048:(hf + 1) * 2048] = res.results[c]["out"]
    return out
```

```python
import contextlib
import os
CUT = int(os.environ.get('KCUT', '99'))
KSUB = int(os.environ.get('KSUB', '0'))
import numpy as np
import concourse.bass as bass
import concourse.mybir as mybir
from concourse.bass_utils import run_bass_kernel_spmd

F32 = mybir.dt.float32
BF16 = mybir.dt.bfloat16
AF = mybir.ActivationFunctionType
ALU = mybir.AluOpType
AX = mybir.AxisListType

EPOCH = 28000
NT = 17
NPRE = 15
NSEQ = 32
D = 1024
EPS = 1e-6
NEG = -30000.0


class Dep:
    __slots__ = ("w", "rs")

    def __init__(self):
        self.w = None
        self.rs = {}


class Eng:
    def __init__(self, kb, eng, name, sync_self):
        self.kb = kb
        self.eng = eng
        self.name = name
        self.sync_self = sync_self
        self.sem = kb.nc.alloc_semaphore(name=f"s_{name}_0")
        self.nep = 0
        self.cnt = 0
        self.waited = {}
        self.nins = 0
        self.allsems = [self.sem]

    def _collect(self, reads, writes):
        need = {}

        def add(ev):
            if ev is None:
                return
            sem, val, src = ev
            if src is self and not self.sync_self:
                return
            k = id(sem)
            if self.waited.get(k, 0) >= val:
                return
            if k not in need or need[k][1] < val:
                need[k] = (sem, val)

        for d in reads:
            add(d.w)
        for d in writes:
            add(d.w)
            for ev in d.rs.values():
                add(ev)
        return list(need.values())

    def op(self, fn, r=(), w=(), dma=False):
        items = self._collect(r, w)
        if dma:
            sem, val, prev = self.kb.dma_slot("sw" if self.name == "pool" else "hw")
            if prev > 0 and self.waited.get(id(sem), 0) < prev:
                items.append((sem, prev))
        for (s, v) in items[1:]:
            self.eng.wait_ge(s, v)
            self.waited[id(s)] = max(self.waited.get(id(s), 0), v)
        ins = fn(self.eng)
        if items:
            s, v = items[0]
            ins._wait_ge(s, v)
            self.waited[id(s)] = max(self.waited.get(id(s), 0), v)
        self.nins += 1
        if dma:
            ins.then_inc(sem, 16)
            ev = (sem, val, None)
        else:
            if self.cnt >= EPOCH:
                self.nep += 1
                self.sem = self.kb.nc.alloc_semaphore(name=f"s_{self.name}_{self.nep}")
                self.allsems.append(self.sem)
                self.cnt = 0
            self.cnt += 1
            ins.then_inc(self.sem, 1)
            ev = (self.sem, self.cnt, self)
        for d in r:
            d.rs[id(ev[0])] = ev
        for d in w:
            d.w = ev
            d.rs = {}
        return ev

    def wait_sv(self, sem, val):
        if val <= 0 or self.waited.get(id(sem), 0) >= val:
            return
        self.eng.wait_ge(sem, val)
        self.waited[id(sem)] = val


class KB:
    def __init__(self, nc, n_dma_sems=48):
        self.nc = nc
        self.pe = Eng(self, nc.tensor, "pe", False)
        self.act = Eng(self, nc.scalar, "act", True)
        self.dve = Eng(self, nc.vector, "dve", True)
        self.pool = Eng(self, nc.gpsimd, "pool", True)
        self.sp = Eng(self, nc.sync, "sp", True)
        self.engs = [self.pe, self.act, self.dve, self.pool, self.sp]
        self.dsems_hw = [[nc.alloc_semaphore(name=f"dmah{i}"), 0] for i in range(32)]
        self.dsems_sw = [[nc.alloc_semaphore(name=f"dmas{i}"), 0] for i in range(24)]
        self.dsems = self.dsems_hw + self.dsems_sw
        self.dnext = {"hw": 0, "sw": 0}
        self.nt = 0

    def dma_slot(self, kind):
        lst = self.dsems_hw if kind == "hw" else self.dsems_sw
        slot = lst[self.dnext[kind]]
        self.dnext[kind] = (self.dnext[kind] + 1) % len(lst)
        prev = slot[1]
        slot[1] += 16
        return slot[0], slot[1], prev

    def barrier(self, engs=None):
        for e in (engs or self.engs):
            for x in self.engs:
                if x is e:
                    continue
                e.wait_sv(x.sem, x.cnt)
            for sem, val in self.dsems:
                e.wait_sv(sem, val)


class T:
    def __init__(self, t):
        self.t = t
        self.d = Dep()

    def __getitem__(self, k):
        return self.t[k]


class Scope:
    def __init__(self, kb):
        self.kb = kb
        self.st = contextlib.ExitStack()

    def sb(self, shape, dtype, name=None):
        self.kb.nt += 1
        t = self.st.enter_context(self.kb.nc.sbuf_tensor(f"sb{self.kb.nt}_{name or 't'}", list(shape), dtype))
        return T(t)

    def close(self):
        self.kb.barrier()
        self.st.close()


def build(layers=(0, 1), dbg=None):
    nc = bass.Bass("TRN2", target_bir_lowering=False)
    kb = KB(nc)
    pe, act, dve, pool, sp = kb.pe, kb.act, kb.dve, kb.pool, kb.sp
    L0 = 0 in layers
    L1 = 1 in layers

    def din(name, shape, dt=F32):
        return nc.dram_tensor(name, list(shape), dt, kind="ExternalInput").ap()

    xo_d = din("xo", [NT * 128, D])
    xp_d = din("xp", [NPRE * 128, D])
    cst_d = din("cst", [128, 640])
    selall_d = din("selall", [16, 2048])
    pc_d = din("pc", [128, 68])
    sm_d = din("smalls", [128, 454])
    gains_d = din("gains", [4, 128, D])
    ev_w_in_d = din("ev_w_in", [D, 2828])
    ev_wsT_d = din("ev_wsT", [4, 128, 128])
    ev_w_out_d = din("ev_w_out", [D, D])
    od_w_in_d = din("od_w_in", [D, 2048])
    od_w_pool_d = din("od_w_pool", [4, 128, 128])
    od_w_out_d = din("od_w_out", [D, D])
    wr_d = din("wr", [2, D, 20])
    wg_d = din("moe_w_gate", [2, 16, D, 256])
    wu_d = din("moe_w_up", [2, 16, D, 256])
    wd_d = din("moe_w_down", [2, 16, 256, D])
    if L1:
        out_d = nc.dram_tensor("out", [16 * 128, D], F32, kind="ExternalOutput").ap()
    else:
        out_d = nc.dram_tensor("out", [NT * 128, D], F32, kind="ExternalOutput").ap()
    dbg_d = None
    if dbg:
        dbg_d = nc.dram_tensor("dbg", [NT * 128, D], F32, kind="ExternalOutput").ap()
    kT_d = T(nc.dram_tensor("kT_scr", [12, 70, NSEQ * 128], BF16).ap())
    qT_d = T(nc.dram_tensor("qT_scr", [12, 70, NT * 128], BF16).ap())
    V_d = T(nc.dram_tensor("V_scr", [NSEQ, 128, 780], BF16).ap())

    pbig = [nc.alloc_psum_tensor(f"bankpair{i}", [128, 1024], F32) for i in range(4)]
    pb = [T(pbig[i // 2][:, (i % 2) * 512:(i % 2 + 1) * 512]) for i in range(8)]

    def pbf(i):
        return pb[i][:].bitcast(BF16).rearrange("p (a b) -> p a b", b=128)

    G = Scope(kb)
    xres = G.sb([128, NT, D], F32, "xres")
    xres_d = [Dep() for _ in range(NT)]
    cst32 = G.sb([128, 640], F32, "cst32")
    cstb = G.sb([128, 640], BF16, "cstb")
    pc = G.sb([128, 68], F32, "pc")
    sm = G.sb([128, 454], F32, "smalls_sb")
    ident_b = cstb[:, 0:128]
    maskT_b = cstb[:, 384:512]
    shift_b = cstb[0:64, 512:640]
    ident_f = cst32[:, 0:128]
    U_f = cst32[:, 128:256]
    sel127_f = cst32[:, 256:384]
    ones_f = G.sb([128, 128], F32, "ones_f")
    bfb = sm[:, 0:12]
    gq_b = sm[:, 12:76]
    gk_b = sm[:, 76:140]
    gvcol = sm[:, 140:142]
    bsl = sm[:, 142:398]
    cw = sm[:, 398:410]
    pscale = sm[:, 410:414]
    brt = sm[:, 414:454]
    valid = pc[:, 0:1]
    nvalid = pc[:, 1:2]
    icnt = pc[:, 4:68]

    sp.op(lambda e: e.dma_start(out=cst32[:], in_=cst_d), w=[cst32.d], dma=True)
    pool.op(lambda e: e.dma_start(out=cstb[:], in_=cst_d), w=[cstb.d], dma=True)
    sp.op(lambda e: e.dma_start(out=pc[:], in_=pc_d), w=[pc.d], dma=True)
    sp.op(lambda e: e.dma_start(out=sm[:], in_=sm_d), w=[sm.d], dma=True)
    dve.op(lambda e: e.memset(ones_f[:], 1.0), w=[ones_f.d])

    def load_xres():
        for j in range(NT):
            sp.op(lambda e: e.dma_start(out=xres[:, j, :], in_=xo_d[j * 128:(j + 1) * 128, :]), w=[xres_d[j]], dma=True)

    def rstd_from_ss(S, ss, n, scale, width):
        dve.op(lambda e: e.tensor_scalar(out=ss[:, 0:width], in0=ss[:, 0:width], scalar1=1.0 / n, scalar2=EPS,
                                         op0=ALU.mult, op1=ALU.add), r=[ss.d], w=[ss.d])
        act.op(lambda e: e.activation(out=ss[:, 0:width], in_=ss[:, 0:width], func=AF.Ln), r=[ss.d], w=[ss.d])
        act.op(lambda e: e.activation(out=ss[:, 0:width], in_=ss[:, 0:width], func=AF.Exp, scale=-0.5), r=[ss.d], w=[ss.d])

    def nt_a(xsrc_ap, xsrc_dep, gn, ss, xs):
        act.op(lambda e: e.activation(out=xs[:], in_=xsrc_ap, func=AF.Square, accum_out=ss[:, 0:1]),
               r=[xsrc_dep], w=[xs.d, ss.d])
        rstd_from_ss(None, ss, D, 1.0, 1)
        dve.op(lambda e: e.scalar_tensor_tensor(out=xs[:], in0=xsrc_ap, scalar=ss[:, 0:1], in1=gn[:],
                                                op0=ALU.mult, op1=ALU.mult), r=[xsrc_dep, ss.d, gn.d], w=[xs.d])

    def nt_pe(xs, bank):
        pv = pbf(bank)
        for c in range(8):
            pe.op(lambda e: e.transpose(out=pv[:, c, :], in_=xs[:, c * 128:(c + 1) * 128], identity=ident_b),
                  r=[xs.d, cstb.d], w=[pb[bank].d])

    def nt_copy(dstT_ap, dst_dep, bank, on_dve=False):
        if on_dve:
            dve.op(lambda e: e.tensor_copy(out=dstT_ap, in_=pbf(bank)), r=[pb[bank].d], w=[dst_dep])
        else:
            act.op(lambda e: e.copy(out=dstT_ap, in_=pbf(bank)), r=[pb[bank].d], w=[dst_dep])

    def norm_transpose(S, xsrc_ap, xsrc_dep, gn, junk, ss, xs, dstT_ap, dst_dep, bank):
        nt_a(xsrc_ap, xsrc_dep, gn, ss, xs)
        nt_pe(xs, bank)
        nt_copy(dstT_ap, dst_dep, bank)

    def nt_sequence(tile_src, n, gn, ss2, xs2, dst, after=None):
        a0, d0 = tile_src(0)
        nt_a(a0, d0, gn, ss2[0], xs2[0])
        for i in range(n):
            bank = 0 if i % 2 == 0 else 2
            nt_pe(xs2[i % 2], bank)
            if i + 1 < n:
                a1, d1 = tile_src(i + 1)
                nt_a(a1, d1, gn, ss2[(i + 1) % 2], xs2[(i + 1) % 2])
            da, dd = dst(i)
            nt_copy(da, dd, bank, on_dve=(i % 2 == 0))
            if after is not None and i >= 1:
                after(i - 1)
        if after is not None:
            after(n - 1)

    def load_w_bf16(dst, src_ap, nchunk, ncols, colsplit=1024):
        for c0 in range(0, ncols, colsplit):
            c1 = min(ncols, c0 + colsplit)
            pool.op(lambda e: e.dma_start(out=dst[:, :, c0:c1],
                                          in_=src_ap[:, c0:c1].rearrange("(c p) n -> p c n", p=128)),
                    w=[dst.d], dma=True)

    def dbg_dump():
        if dbg_d is None:
            return
        for j in range(NT):
            sp.op(lambda e: e.dma_start(out=dbg_d[j * 128:(j + 1) * 128, :], in_=xres[:, j, :]), r=[xres_d[j]], dma=True)

    def moe_layer(l, tiles, on_final=None):
        t0 = tiles[0]
        ntl = len(tiles)
        ntok = ntl * 128
        S = Scope(kb)
        gn = S.sb([128, D], F32)
        sp.op(lambda e: e.dma_start(out=gn[:], in_=gains_d[(1 if l == 0 else 3)]), w=[gn.d], dma=True)
        wr = S.sb([128, 8, 20], BF16)
        pool.op(lambda e: e.dma_start(out=wr[:], in_=wr_d[l].rearrange("(c p) n -> p c n", p=128)), w=[wr.d], dma=True)
        xnT = S.sb([128, 8, ntok], BF16)
        xnT_d = [Dep() for _ in range(ntl)]
        lg = S.sb([128, ntl, 20], F32)
        ss2 = [S.sb([128, 4], F32) for _ in range(2)]
        xs2 = [S.sb([128, D], BF16) for _ in range(2)]
        wg = [S.sb([128, 8, 256], BF16) for _ in range(2)]
        wu = [S.sb([128, 8, 256], BF16) for _ in range(2)]
        wd = [S.sb([128, 2, D], BF16) for _ in range(2)]

        def load_expert(e_):
            b = e_ % 2
            pool.op(lambda e: e.dma_start(out=wg[b][:], in_=wg_d[l, e_].rearrange("(c p) n -> p c n", p=128)), w=[wg[b].d], dma=True)
            pool.op(lambda e: e.dma_start(out=wu[b][:], in_=wu_d[l, e_].rearrange("(c p) n -> p c n", p=128)), w=[wu[b].d], dma=True)
            pool.op(lambda e: e.dma_start(out=wd[b][:], in_=wd_d[l, e_].rearrange("(c p) n -> p c n", p=128)), w=[wd[b].d], dma=True)

        load_expert(0)
        load_expert(1)
        def router_(i):
            for c in range(8):
                pe.op(lambda e: e.matmul(pb[1][:, 0:20], lhsT=xnT[:, c, i * 128:(i + 1) * 128], rhs=wr[:, c, :],
                                         start=(c == 0), stop=(c == 7)), r=[xnT_d[i], wr.d], w=[pb[1].d])
            dve.op(lambda e: e.tensor_tensor(out=lg[:, i, :], in0=pb[1][:, 0:20], in1=brt[:, l * 20:(l + 1) * 20], op=ALU.add),
                   r=[pb[1].d, sm.d], w=[lg.d])
        nt_sequence(lambda i: (xres[:, tiles[i], :], xres_d[tiles[i]]), ntl, gn, ss2, xs2,
                    lambda i: (xnT[:, :, i * 128:(i + 1) * 128], xnT_d[i]), after=router_)
        n4 = [128, ntl, 4]
        gmax = S.sb([128, ntl], F32)
        ge = S.sb(n4, F32)
        gsum = S.sb([128, ntl], F32)
        gsel = S.sb(n4, F32)
        tmp16 = S.sb([128, ntl, 16], F32)
        el = S.sb(n4, F32)
        emax = S.sb([128, ntl], F32)
        ee = S.sb(n4, F32)
        oh1 = S.sb(n4, F32)
        ee2 = S.sb(n4, F32)
        m2 = S.sb([128, ntl], F32)
        oh2 = S.sb(n4, F32)
        w1 = S.sb([128, ntl], F32)
        w2 = S.sb([128, ntl], F32)
        cw4 = S.sb(n4, F32)
        comb = S.sb([128, ntl, 16], F32)
        glog = lg[:, :, 0:4]
        elog = lg[:, :, 4:20]

        def bc_last(ap2, n):
            return ap2.unsqueeze(2).to_broadcast([128, ntl, n])

        dve.op(lambda e: e.tensor_reduce(out=gmax[:], in_=glog, axis=AX.X, op=ALU.max), r=[lg.d], w=[gmax.d])
        dve.op(lambda e: e.tensor_tensor(out=ge[:], in0=glog, in1=bc_last(gmax[:], 4), op=ALU.subtract), r=[lg.d, gmax.d], w=[ge.d])
        dve.op(lambda e: e.tensor_single_scalar(out=gsel[:], in_=ge[:], scalar=0.0, op=ALU.is_ge), r=[ge.d], w=[gsel.d])
        act.op(lambda e: e.activation(out=ge[:], in_=ge[:], func=AF.Exp), r=[ge.d], w=[ge.d])
        dve.op(lambda e: e.tensor_reduce(out=gsum[:], in_=ge[:], axis=AX.X, op=ALU.add), r=[ge.d], w=[gsum.d])
        dve.op(lambda e: e.reciprocal(out=gsum[:], in_=gsum[:]), r=[gsum.d], w=[gsum.d])
        dve.op(lambda e: e.tensor_tensor(out=tmp16[:].rearrange("p t (g e) -> p t g e", e=4),
                                         in0=elog.rearrange("p t (g e) -> p t g e", e=4),
                                         in1=gsel[:].unsqueeze(3).to_broadcast([128, ntl, 4, 4]), op=ALU.mult),
               r=[lg.d, gsel.d], w=[tmp16.d])
        dve.op(lambda e: e.tensor_reduce(out=el[:], in_=tmp16[:].rearrange("p t (g e) -> p t e g", e=4), axis=AX.X, op=ALU.add),
               r=[tmp16.d], w=[el.d])
        dve.op(lambda e: e.tensor_reduce(out=emax[:], in_=el[:], axis=AX.X, op=ALU.max), r=[el.d], w=[emax.d])
        dve.op(lambda e: e.tensor_tensor(out=ee[:], in0=el[:], in1=bc_last(emax[:], 4), op=ALU.subtract), r=[el.d, emax.d], w=[ee.d])
        dve.op(lambda e: e.tensor_single_scalar(out=oh1[:], in_=ee[:], scalar=0.0, op=ALU.is_ge), r=[ee.d], w=[oh1.d])
        act.op(lambda e: e.activation(out=ee[:], in_=ee[:], func=AF.Exp), r=[ee.d], w=[ee.d])
        dve.op(lambda e: e.scalar_tensor_tensor(out=ee2[:], in0=oh1[:], scalar=-4.0, in1=ee[:], op0=ALU.mult, op1=ALU.add),
               r=[oh1.d, ee.d], w=[ee2.d])
        dve.op(lambda e: e.tensor_reduce(out=m2[:], in_=ee2[:], axis=AX.X, op=ALU.max), r=[ee2.d], w=[m2.d])
        dve.op(lambda e: e.tensor_tensor(out=oh2[:], in0=ee2[:], in1=bc_last(m2[:], 4), op=ALU.is_ge), r=[ee2.d, m2.d], w=[oh2.d])
        dve.op(lambda e: e.tensor_scalar(out=w1[:], in0=m2[:], scalar1=1.0, scalar2=None, op0=ALU.add), r=[m2.d], w=[w1.d])
        dve.op(lambda e: e.reciprocal(out=w1[:], in_=w1[:]), r=[w1.d], w=[w1.d])
        dve.op(lambda e: e.tensor_tensor(out=w1[:], in0=w1[:], in1=gsum[:], op=ALU.mult), r=[w1.d, gsum.d], w=[w1.d])
        dve.op(lambda e: e.tensor_tensor(out=w2[:], in0=w1[:], in1=m2[:], op=ALU.mult), r=[w1.d, m2.d], w=[w2.d])
        dve.op(lambda e: e.tensor_tensor(out=cw4[:], in0=oh1[:], in1=bc_last(w1[:], 4), op=ALU.mult), r=[oh1.d, w1.d], w=[cw4.d])
        dve.op(lambda e: e.tensor_tensor(out=oh2[:], in0=oh2[:], in1=bc_last(w2[:], 4), op=ALU.mult), r=[oh2.d, w2.d], w=[oh2.d])
        dve.op(lambda e: e.tensor_tensor(out=cw4[:], in0=cw4[:], in1=oh2[:], op=ALU.add), r=[cw4.d, oh2.d], w=[cw4.d])
        dve.op(lambda e: e.tensor_copy(out=comb[:].rearrange("p t (g e) -> p t g e", e=4),
                                       in_=cw4[:].unsqueeze(2).to_broadcast([128, ntl, 4, 4])), r=[cw4.d], w=[comb.d])
        dve.op(lambda e: e.tensor_tensor(out=comb[:].rearrange("p t (g e) -> p t g e", e=4),
                                         in0=comb[:].rearrange("p t (g e) -> p t g e", e=4),
                                         in1=gsel[:].unsqueeze(3).to_broadcast([128, ntl, 4, 4]), op=ALU.mult),
               r=[comb.d, gsel.d], w=[comb.d])
        groups = []
        c0 = 0
        while c0 < ntok:
            n = min(512, ntok - c0)
            groups.append((c0, n))
            c0 += n
        sg = [S.sb([128, 512], F32) for _ in range(2)]
        hid = [[S.sb([128, 512], BF16) for _ in range(2)] for _ in range(2)]
        items = [(e_, tok0, n) for e_ in range(16) for (tok0, n) in groups]
        ybank = [0]

        def GU(i):
            e_, tok0, n = items[i]
            b = e_ % 2
            k2 = i % 2
            for hc in range(2):
                gb, ub = pb[hc * 2], pb[hc * 2 + 1]
                xd = [xnT_d[i_] for i_ in range(tok0 // 128, (tok0 + n) // 128)]
                for c in range(8):
                    pe.op(lambda e: e.matmul(gb[:, 0:n], lhsT=wg[b][:, c, hc * 128:(hc + 1) * 128], rhs=xnT[:, c, tok0:tok0 + n],
                                             start=(c == 0), stop=(c == 7)), r=[wg[b].d] + xd, w=[gb.d])
                for c in range(8):
                    pe.op(lambda e: e.matmul(ub[:, 0:n], lhsT=wu[b][:, c, hc * 128:(hc + 1) * 128], rhs=xnT[:, c, tok0:tok0 + n],
                                             start=(c == 0), stop=(c == 7)), r=[wu[b].d] + xd, w=[ub.d])
                act.op(lambda e: e.activation(out=sg[hc][:, 0:n], in_=gb[:, 0:n], func=AF.Silu), r=[gb.d], w=[sg[hc].d])
                dve.op(lambda e: e.tensor_tensor(out=hid[k2][hc][:, 0:n], in0=sg[hc][:, 0:n], in1=ub[:, 0:n], op=ALU.mult),
                       r=[sg[hc].d, ub.d], w=[hid[k2][hc].d])

        def DOWN(i):
            e_, tok0, n = items[i]
            b = e_ % 2
            k2 = i % 2
            for tt in range(n // 128):
                j = t0 + tok0 // 128 + tt
                for nn in range(2):
                    yb = pb[5 + ybank[0]]
                    ybank[0] = (ybank[0] + 1) % 3
                    for hc in range(2):
                        pe.op(lambda e: e.matmul(yb[:, :], lhsT=hid[k2][hc][:, tt * 128:(tt + 1) * 128], rhs=wd[b][:, hc, nn * 512:(nn + 1) * 512],
                                                 start=(hc == 0), stop=(hc == 1)), r=[hid[k2][hc].d, wd[b].d], w=[yb.d])
                    ti = tok0 // 128 + tt
                    dve.op(lambda e: e.scalar_tensor_tensor(out=xres[:, j, nn * 512:(nn + 1) * 512], in0=yb[:, :], scalar=comb[:, ti, e_:e_ + 1],
                                                            in1=xres[:, j, nn * 512:(nn + 1) * 512], op0=ALU.mult, op1=ALU.add),
                           r=[yb.d, comb.d, xres_d[j]], w=[xres_d[j]])
                if on_final is not None and e_ == 15:
                    on_final(j)
            if (i + 1 == len(items) or items[i + 1][0] != e_) and e_ + 2 < 16:
                load_expert(e_ + 2)

        GU(0)
        for i in range(len(items)):
            if i + 1 < len(items):
                GU(i + 1)
            DOWN(i)
        S.close()

    def wout_phase(S, mixT, mix_d, w_src, tiles, col_of_tile, wo=None):
        if wo is None:
            wo = S.sb([128, 8, D], BF16)
            load_w_bf16(wo, w_src, 8, D)
        bank = 0
        for j in tiles:
            cc = col_of_tile(j)
            for nn in range(2):
                yb = pb[bank]
                bank = (bank + 1) % 4
                for c in range(8):
                    pe.op(lambda e: e.matmul(yb[:, :], lhsT=mixT[:, c, cc:cc + 128], rhs=wo[:, c, nn * 512:(nn + 1) * 512],
                                             start=(c == 0), stop=(c == 7)), r=[mix_d, wo.d], w=[yb.d])
                dve.op(lambda e: e.tensor_tensor(out=xres[:, j, nn * 512:(nn + 1) * 512], in0=xres[:, j, nn * 512:(nn + 1) * 512],
                                                 in1=yb[:, :], op=ALU.add), r=[yb.d, xres_d[j]], w=[xres_d[j]])

    def even_mixer():
        SL = Scope(kb)
        mixT = SL.sb([128, 8, NT * 128], BF16, "mixT0")
        S = Scope(kb)
        gn = S.sb([128, D], F32)
        sp.op(lambda e: e.dma_start(out=gn[:], in_=gains_d[0]), w=[gn.d], dma=True)
        load_xres()
        wev = S.sb([128, 8, 2828], BF16)
        for (c0_, c1_) in ((1280, 2054), (2054, 2828)):
            pool.op(lambda e: e.dma_start(out=wev[:, :, c0_:c1_], in_=ev_w_in_d[:, c0_:c1_].rearrange("(c p) n -> p c n", p=128)), w=[wev.d], dma=True)
        wev_q = Dep()
        wsT = S.sb([128, 4, 128], BF16)
        pool.op(lambda e: e.dma_start(out=wsT[:], in_=ev_wsT_d.rearrange("g s t -> s g t")), w=[wsT.d], dma=True)
        Ub = cstb[:, 128:256]
        dve.op(lambda e: e.tensor_tensor(out=wsT[:], in0=wsT[:], in1=Ub.unsqueeze(1).to_broadcast([128, 4, 128]), op=ALU.mult),
               r=[wsT.d, cstb.d], w=[wsT.d])
        gqk = S.sb([128, 64], F32)
        dve.op(lambda e: e.scalar_tensor_tensor(out=gqk[:], in0=gq_b, scalar=0.125, in1=gk_b, op0=ALU.mult, op1=ALU.mult),
               r=[sm.d], w=[gqk.d])
        xin = [S.sb([128, D], F32) for _ in range(2)]
        ss2 = [S.sb([128, 4], F32) for _ in range(2)]
        xs2 = [S.sb([128, D], BF16) for _ in range(2)]
        xnT = [S.sb([128, 8, 128], BF16) for _ in range(2)]
        sq = S.sb([128, 768], BF16)
        Kraw = S.sb([128, 768], BF16)
        Qraw = S.sb([128, 768], BF16)
        ssall = S.sb([128, 28], F32)
        kaug = [S.sb([128, 12, 70], BF16) for _ in range(2)]
        qaug = [S.sb([128, 12, 70], BF16) for _ in range(2)]
        vaug = [S.sb([128, 12, 65], BF16) for _ in range(2)]
        kTs = [S.sb([70, 12, 128], BF16) for _ in range(2)]
        qTs = [S.sb([70, 12, 128], BF16) for _ in range(2)]
        zt = S.sb([128, 12], F32)
        ls = S.sb([128, 12], F32)
        Fc = [S.sb([128, 12], F32) for _ in range(2)]
        Fh = S.sb([128, 12], BF16)
        Fm = S.sb([128, 12], BF16)
        Fl = S.sb([128, 12], BF16)
        r1 = S.sb([128, 12], F32)
        qtmp = sq
        gv = S.sb([128, 256], F32)
        vpad = S.sb([128, 4, 128], BF16)
        guT = [S.sb([128, 256], F32) for _ in range(2)]
        stmp = S.sb([128, 256], F32)
        gv2 = stmp
        for b_ in range(2):
            dve.op(lambda e: e.memset(kaug[b_][:, :, 64:67], 1.0), w=[kaug[b_].d])
            dve.op(lambda e: e.memset(qaug[b_][:, :, 67:70], 1.0), w=[qaug[b_].d])
        dve.op(lambda e: e.memset(vpad[:], 0.0), w=[vpad.d])

        def own_(i):
            return i >= NPRE

        def S1a(i):
            b2 = i % 2
            if own_(i):
                j = i - NPRE
                xsrc, xdep = xres[:, j, :], xres_d[j]
            else:
                pool.op(lambda e: e.dma_start(out=xin[b2][:], in_=xp_d[i * 128:(i + 1) * 128, :]), w=[xin[b2].d], dma=True)
                xsrc, xdep = xin[b2][:], xin[b2].d
            nt_a(xsrc, xdep, gn, ss2[b2], xs2[b2])

        def S1b(i):
            b2 = i % 2
            nt_pe(xs2[b2], 0)
            nt_copy(xnT[b2][:], xnT[b2].d, 0, on_dve=(i % 2 == 0))

        def S2(i):
            xT = xnT[i % 2]

            def proj(bank, c_lo, c_hi, w_lo):
                n = c_hi - c_lo
                wdep = wev.d if w_lo >= 1280 else wev_q
                for c in range(8):
                    pe.op(lambda e: e.matmul(pb[bank][:, c_lo:c_hi], lhsT=xT[:, c, :], rhs=wev[:, c, w_lo:w_lo + n],
                                             start=(c == 0), stop=(c == 7)), r=[xT.d, wdep], w=[pb[bank].d])
            proj(1, 0, 512, 1280)
            proj(2, 0, 256, 1792)
            proj(2, 256, 512, 2048)
            proj(3, 0, 512, 2304)
            proj(4, 0, 12, 2816)
            if own_(i):
                proj(5, 0, 512, 512)
                proj(6, 0, 256, 1024)
                proj(6, 256, 512, 256)
                for uc in range(2):
                    for c in range(8):
                        pe.op(lambda e: e.matmul(pb[7][:, uc * 128:(uc + 1) * 128], lhsT=wev[:, c, uc * 128:(uc + 1) * 128], rhs=xT[:, c, :],
                                                 start=(c == 0), stop=(c == 7)), r=[xT.d, wev_q], w=[pb[7].d])

        def S3(i):
            b2 = i % 2
            vflag = valid if i <= NPRE else 1.0
            nvflag = nvalid if i <= NPRE else -1.0
            va = vaug[b2]
            dve.op(lambda e: e.tensor_tensor(out=zt[:], in0=pb[4][:, 0:12], in1=bfb, op=ALU.add), r=[pb[4].d, sm.d], w=[zt.d])
            act.op(lambda e: e.activation(out=zt[:], in_=zt[:], func=AF.Exp, scale=-1.0), r=[zt.d], w=[zt.d])
            act.op(lambda e: e.activation(out=Kraw[:, 0:512], in_=pb[1][:, 0:512], func=AF.Copy), r=[pb[1].d], w=[Kraw.d])
            dve.op(lambda e: e.tensor_copy(out=Kraw[:, 512:768], in_=pb[2][:, 0:256]), r=[pb[2].d], w=[Kraw.d])
            dve.op(lambda e: e.tensor_scalar(out=zt[:], in0=zt[:], scalar1=1.0, scalar2=None, op0=ALU.add), r=[zt.d], w=[zt.d])
            act.op(lambda e: e.activation(out=zt[:], in_=zt[:], func=AF.Ln), r=[zt.d], w=[zt.d])
            dve.op(lambda e: e.tensor_scalar(out=va[:, 0:4, 0:64], in0=pb[2][:, 256:512].rearrange("p (h d) -> p h d", d=64),
                                             scalar1=vflag, scalar2=None, op0=ALU.mult), r=[pb[2].d, pc.d], w=[va.d])
            dve.op(lambda e: e.tensor_scalar(out=va[:, 4:12, 0:64], in0=pb[3][:, 0:512].rearrange("p (h d) -> p h d", d=64),
                                             scalar1=vflag, scalar2=None, op0=ALU.mult), r=[pb[3].d, pc.d], w=[va.d])
            dve.op(lambda e: e.tensor_scalar(out=va[:, :, 64], in0=ones_f[:, 0:12], scalar1=vflag, scalar2=None, op0=ALU.mult),
                   r=[ones_f.d, pc.d], w=[va.d])
            sp.op(lambda e: e.dma_start(out=V_d[i], in_=va[:].rearrange("p h d -> p (h d)")), r=[va.d], w=[V_d.d], dma=True)
            dve.op(lambda e: e.tensor_scalar(out=ls[:], in0=zt[:], scalar1=nvflag, scalar2=None, op0=ALU.mult), r=[zt.d, pc.d], w=[ls.d])
            if own_(i):
                act.op(lambda e: e.activation(out=Qraw[:, 0:512], in_=pb[5][:, 0:512], func=AF.Copy), r=[pb[5].d], w=[Qraw.d])
                act.op(lambda e: e.activation(out=Qraw[:, 512:768], in_=pb[6][:, 0:256], func=AF.Copy), r=[pb[6].d], w=[Qraw.d])
                act.op(lambda e: e.activation(out=gv[:], in_=pb[6][:, 256:512], func=AF.Gelu_apprx_tanh), r=[pb[6].d], w=[gv.d])
                act.op(lambda e: e.activation(out=guT[b2][:], in_=pb[7][:, 0:256], func=AF.Gelu_apprx_tanh), r=[pb[7].d], w=[guT[b2].d])

        def S4(i):
            b2 = i % 2
            Fcur, Fprev = Fc[b2], Fc[1 - b2]
            ka, qa = kaug[b2], qaug[b2]
            pe.op(lambda e: e.matmul(pb[4][:, 16:28], lhsT=U_f, rhs=ls[:], start=True, stop=(i == 0)), r=[cst32.d, ls.d], w=[pb[4].d])
            if i > 0:
                pe.op(lambda e: e.matmul(pb[4][:, 16:28], lhsT=sel127_f, rhs=Fprev[:], start=False, stop=True), r=[cst32.d, Fprev.d], w=[pb[4].d])
            dve.op(lambda e: e.tensor_copy(out=Fcur[:], in_=pb[4][:, 16:28]), r=[pb[4].d], w=[Fcur.d])
            dve.op(lambda e: e.tensor_scalar(out=ka[:, :, 67], in0=Fcur[:], scalar1=-1.0, scalar2=None, op0=ALU.mult), r=[Fcur.d], w=[ka.d])
            dve.op(lambda e: e.tensor_tensor(out=r1[:], in0=Fcur[:], in1=ka[:, :, 67], op=ALU.add), r=[Fcur.d, ka.d], w=[r1.d])
            dve.op(lambda e: e.tensor_scalar(out=ka[:, :, 68], in0=r1[:], scalar1=-1.0, scalar2=None, op0=ALU.mult), r=[r1.d], w=[ka.d])
            dve.op(lambda e: e.tensor_tensor(out=r1[:], in0=r1[:], in1=ka[:, :, 68], op=ALU.add), r=[r1.d, ka.d], w=[r1.d])
            dve.op(lambda e: e.tensor_scalar(out=ka[:, :, 69], in0=r1[:], scalar1=-1.0, scalar2=None, op0=ALU.mult), r=[r1.d], w=[ka.d])
            if own_(i):
                dve.op(lambda e: e.tensor_scalar(out=qa[:, :, 64:67], in0=ka[:, :, 67:70], scalar1=-1.0, scalar2=None, op0=ALU.mult), r=[ka.d], w=[qa.d])

        def S5(i):
            b2 = i % 2
            ka, qa = kaug[b2], qaug[b2]
            own = own_(i)
            pool.op(lambda e: e.tensor_tensor(out=sq[:], in0=Kraw[:], in1=Kraw[:], op=ALU.mult), r=[Kraw.d], w=[sq.d])
            dve.op(lambda e: e.tensor_reduce(out=ssall[:, 0:12], in_=sq[:].rearrange("p (h d) -> p h d", d=64), axis=AX.X, op=ALU.add), r=[sq.d], w=[ssall.d])
            if own:
                pool.op(lambda e: e.tensor_tensor(out=sq[:], in0=Qraw[:], in1=Qraw[:], op=ALU.mult), r=[Qraw.d], w=[sq.d])
                dve.op(lambda e: e.tensor_reduce(out=ssall[:, 12:24], in_=sq[:].rearrange("p (h d) -> p h d", d=64), axis=AX.X, op=ALU.add), r=[sq.d], w=[ssall.d])
                pool.op(lambda e: e.tensor_tensor(out=gv2[:], in0=gv[:], in1=gv[:], op=ALU.mult), r=[gv.d], w=[gv2.d])
                dve.op(lambda e: e.tensor_reduce(out=ssall[:, 24:28], in_=gv2[:].rearrange("p (g d) -> p g d", d=64), axis=AX.X, op=ALU.add), r=[gv2.d], w=[ssall.d])
            rstd_from_ss(S, ssall, 64, 1.0, 28 if own else 12)
            dve.op(lambda e: e.tensor_tensor(out=ka[:, :, 0:64], in0=Kraw[:].rearrange("p (h d) -> p h d", d=64),
                                             in1=ssall[:, 0:12].unsqueeze(2).to_broadcast([128, 12, 64]), op=ALU.mult), r=[Kraw.d, ssall.d], w=[ka.d])
            if not own:
                return
            dve.op(lambda e: e.tensor_tensor(out=qtmp[:].rearrange("p (h d) -> p h d", d=64), in0=Qraw[:].rearrange("p (h d) -> p h d", d=64),
                                             in1=ssall[:, 12:24].unsqueeze(2).to_broadcast([128, 12, 64]), op=ALU.mult), r=[Qraw.d, ssall.d], w=[qtmp.d])
            dve.op(lambda e: e.tensor_tensor(out=qa[:, :, 0:64], in0=qtmp[:].rearrange("p (h d) -> p h d", d=64),
                                             in1=gqk[:].unsqueeze(1).to_broadcast([128, 12, 64]), op=ALU.mult), r=[qtmp.d, gqk.d], w=[qa.d])
            for g in range(4):
                off = 64 * (g % 2)
                dve.op(lambda e: e.tensor_scalar(out=vpad[:, g, off:off + 64], in0=gv[:, g * 64:(g + 1) * 64], scalar1=ssall[:, 24 + g:25 + g], scalar2=None,
                                                 op0=ALU.mult), r=[gv.d, ssall.d], w=[vpad.d])

        def S6(i):
            b2 = i % 2
            j = i - NPRE
            ka, qa = kaug[b2], qaug[b2]
            for h in range(12):
                bk = 1 if h < 8 else 2
                pe.op(lambda e: e.transpose(out=pbf(bk)[0:70, h % 8, :], in_=ka[:, h, :], identity=ident_b), r=[ka.d, cstb.d], w=[pb[bk].d])
            ks = kTs[b2]
            act.op(lambda e: e.copy(out=ks[:, 0:8, :], in_=pbf(1)[0:70, :, :]), r=[pb[1].d], w=[ks.d])
            dve.op(lambda e: e.tensor_copy(out=ks[:, 8:12, :], in_=pbf(2)[0:70, 0:4, :]), r=[pb[2].d], w=[ks.d])
            sp.op(lambda e: e.dma_start(out=kT_d[:, :, i * 128:(i + 1) * 128].rearrange("h r t -> r h t"), in_=ks[:]), r=[ks.d], w=[kT_d.d], dma=True)
            if not own_(i):
                return
            for h in range(12):
                bk = 5 if h < 8 else 6
                pe.op(lambda e: e.transpose(out=pbf(bk)[0:70, h % 8, :], in_=qa[:, h, :], identity=ident_b), r=[qa.d, cstb.d], w=[pb[bk].d])
            qs = qTs[b2]
            dve.op(lambda e: e.tensor_copy(out=qs[:, 0:8, :], in_=pbf(5)[0:70, :, :]), r=[pb[5].d], w=[qs.d])
            dve.op(lambda e: e.tensor_copy(out=qs[:, 8:12, :], in_=pbf(6)[0:70, 0:4, :]), r=[pb[6].d], w=[qs.d])
            sp.op(lambda e: e.dma_start(out=qT_d[:, :, j * 128:(j + 1) * 128].rearrange("h r t -> r h t"), in_=qs[:]), r=[qs.d], w=[qT_d.d], dma=True)
            for c in range(2):
                for gg in range(2):
                    g = 2 * c + gg
                    pe.op(lambda e: e.matmul(pb[4][:, 128 + c * 128:128 + (c + 1) * 128], lhsT=vpad[:, g, :], rhs=wsT[:, g, :], start=(gg == 0), stop=(gg == 1)),
                          r=[vpad.d, wsT.d], w=[pb[4].d])
            gu = guT[b2]
            for c in range(2):
                dve.op(lambda e: e.scalar_tensor_tensor(out=stmp[:, c * 128:(c + 1) * 128], in0=pb[4][:, 128 + c * 128:128 + (c + 1) * 128], scalar=gvcol[:, c:c + 1],
                                                        in1=bsl[:, c * 128:(c + 1) * 128], op0=ALU.mult, op1=ALU.add), r=[pb[4].d, sm.d], w=[stmp.d])
                dve.op(lambda e: e.tensor_tensor(out=mixT[:, c, j * 128:(j + 1) * 128], in0=stmp[:, c * 128:(c + 1) * 128], in1=gu[:, c * 128:(c + 1) * 128],
                                                 op=ALU.mult), r=[stmp.d, gu.d], w=[mixT.d])

        S1a(0)
        S1a(1)
        for (c0_, c1_) in ((0, 640), (640, 1280)):
            pool.op(lambda e: e.dma_start(out=wev[:, :, c0_:c1_], in_=ev_w_in_d[:, c0_:c1_].rearrange("(c p) n -> p c n", p=128)), w=[wev_q], dma=True)
        S1b(0)
        S2(0)
        S3(0)
        for i in range(NSEQ):
            if i + 2 < NSEQ:
                S1a(i + 2)
            if i + 1 < NSEQ:
                S1b(i + 1)
            S4(i)
            if i + 1 < NSEQ:
                S2(i + 1)
            S5(i)
            if i + 1 < NSEQ:
                S3(i + 1)
            S6(i)
        S.close()
        if CUT == 10:
            SL.close(); return

        SBC = Scope(kb)
        wo_ev = SBC.sb([128, 8, D], BF16)
        load_w_bf16(wo_ev, ev_w_out_d, 8, D)
        S = Scope(kb)
        kTh = [S.sb([70, NSEQ * 128], BF16) for _ in range(2)]
        qTh = [S.sb([70, NT * 128], BF16) for _ in range(2)]
        Vg = [S.sb([128, NSEQ, 324], BF16) for _ in range(2)]
        for b_ in range(2):
            pool.op(lambda e: e.memset(Vg[b_][:, :, 260:324], 0.0), w=[Vg[b_].d])
        PT = [S.sb([128, 1024], BF16) for _ in range(3)]
        rc = [S.sb([128, 512], F32) for _ in range(2)]
        bcs = [S.sb([64, 512], F32) for _ in range(2)]
        otmp = [S.sb([64, 512], BF16) for _ in range(2)]
        QC = [(0, 128), (128, 512), (640, 512), (1152, 512), (1664, 512)]
        STP = [pbig[0], pbig[1], pbig[2]]
        STD = [Dep(), Dep(), Dep()]
        for k_ in range(3):
            pb[2 * k_].d = STD[k_]
            pb[2 * k_ + 1].d = STD[k_]
        free_slots = [0, 1, 2]

        def take_slot():
            return free_slots.pop(0)

        def give_slot(k_):
            free_slots.append(k_)

        def load_head(h):
            b = h % 2
            sp.op(lambda e: e.dma_start(out=kTh[b][:], in_=kT_d[h]), r=[kT_d.d], w=[kTh[b].d], dma=True)
            sp.op(lambda e: e.dma_start(out=qTh[b][:], in_=qT_d[h]), r=[qT_d.d], w=[qTh[b].d], dma=True)

        def load_vgroup(g):
            b = g % 2
            sp.op(lambda e: e.dma_start(out=Vg[b][:, :, 0:260], in_=V_d[:, :, g * 260:(g + 1) * 260].rearrange("t p c -> p t c")),
                  r=[V_d.d], w=[Vg[b].d], dma=True)

        load_head(0)
        load_vgroup(0)
        load_head(1)
        load_vgroup(1)
        units = []
        nchunk = 0
        for h in range(12):
            for (q0, nq) in QC:
                jlo = q0 // 128
                jhi = jlo + nq // 128 - 1
                last = NPRE + jhi
                if nq == 128:
                    for k0 in range(0, last + 1, 4):
                        subs = [(kt, (kt - k0) * 128, 0, 128, kt == last) for kt in range(k0, k0 + 4)]
                        units.append((h, q0, nq, last, nchunk, subs, 0, 512))
                else:
                    nfull = NPRE + jlo
                    for kt in range(0, nfull, 2):
                        units.append((h, q0, nq, last, nchunk, [(kt, 0, 0, 512, False), (kt + 1, 512, 0, 512, False)], 0, 1024))
                    for dj in range(4):
                        units.append((h, q0, nq, last, nchunk, [(nfull + dj, 0, dj * 128, 512, True)], dj * 128, 512))
                nchunk += 1
        NU = len(units)
        deferred = []
        uslot = {}

        def emit_qk(ui):
            h, q0, nq, last, ck, subs, lo, hi = units[ui]
            kt_, qt_ = kTh[h % 2], qTh[h % 2]
            k_ = take_slot()
            uslot[ui] = k_
            ST, sd = STP[k_], STD[k_]
            for (kt, off, c0, n, diag) in subs:
                lhs = kt_[:, kt * 128:(kt + 1) * 128]
                if diag:
                    pe.op(lambda e: e.matmul(ST[:, off + c0:off + c0 + 128], lhsT=lhs, rhs=qt_[:, q0 + c0:q0 + c0 + 128], start=True, stop=False),
                          r=[kt_.d, qt_.d], w=[sd])
                    pe.op(lambda e: e.matmul(ST[:, off + c0:off + c0 + 128], lhsT=ident_b, rhs=maskT_b, start=False, stop=True),
                          r=[cstb.d], w=[sd])
                    if c0 + 128 < n:
                        pe.op(lambda e: e.matmul(ST[:, off + c0 + 128:off + n], lhsT=lhs, rhs=qt_[:, q0 + c0 + 128:q0 + n], start=True, stop=True),
                              r=[kt_.d, qt_.d], w=[sd])
                else:
                    pe.op(lambda e: e.matmul(ST[:, off + c0:off + n], lhsT=lhs, rhs=qt_[:, q0 + c0:q0 + n], start=True, stop=True),
                          r=[kt_.d, qt_.d], w=[sd])
            if ui + 1 == NU or units[ui + 1][0] != h:
                if h + 2 < 12:
                    load_head(h + 2)

        def emit_exp_pv(ui):
            h, q0, nq, last, ck, subs, lo, hi = units[ui]
            k_ = uslot[ui]
            ST, sd = STP[k_], STD[k_]
            ptb = PT[ui % 3]
            OB = pb[6 + ck % 2]
            vg_ = Vg[(h // 4) % 2]
            hv = (h % 4) * 65
            act.op(lambda e: e.activation(out=ptb[:, lo:hi], in_=ST[:, lo:hi], func=AF.Exp), r=[sd], w=[ptb.d])
            give_slot(k_)
            for (kt, off, c0, n, diag) in subs:
                pe.op(lambda e: e.matmul(OB[:, c0:n], lhsT=vg_[:, kt, hv:hv + 128], rhs=ptb[:, off + c0:off + n], start=(kt == 0), stop=(kt == last),
                                         skip_group_check=True), r=[vg_.d, ptb.d], w=[OB.d])
            if subs[-1][0] == last:
                if h == 3 and q0 == QC[-1][0]:
                    load_vgroup(2)
                k2 = ck % 2
                rc_, bcs_, ot_ = rc[k2], bcs[k2], otmp[k2]
                ch = 2 + h // 2
                dve.op(lambda e: e.tensor_scalar(out=rc_[64:65, 0:nq], in0=OB[64:65, 0:nq], scalar1=1e-30, scalar2=None, op0=ALU.add), r=[OB.d], w=[rc_.d])
                dve.op(lambda e: e.reciprocal(out=rc_[64:65, 0:nq], in_=rc_[64:65, 0:nq]), r=[rc_.d], w=[rc_.d])

                def stage2():
                    kk = take_slot()
                    give_slot(kk)
                    BB, bd = STP[kk], STD[kk]
                    pe.op(lambda e: e.matmul(BB[0:64, 0:nq], lhsT=ones_f[64:65, 0:64], rhs=rc_[64:65, 0:nq], start=True, stop=True), r=[ones_f.d, rc_.d], w=[bd])
                    dve.op(lambda e: e.tensor_copy(out=bcs_[:, 0:nq], in_=BB[0:64, 0:nq]), r=[bd], w=[bcs_.d])
                    if h % 2 == 0:
                        dve.op(lambda e: e.tensor_tensor(out=mixT[0:64, ch, q0:q0 + nq], in0=OB[0:64, 0:nq], in1=bcs_[:, 0:nq], op=ALU.mult),
                               r=[OB.d, bcs_.d], w=[mixT.d])
                    else:
                        dve.op(lambda e: e.tensor_tensor(out=ot_[:, 0:nq], in0=OB[0:64, 0:nq], in1=bcs_[:, 0:nq], op=ALU.mult),
                               r=[OB.d, bcs_.d], w=[ot_.d])

                def stage3():
                    if h % 2 == 1:
                        kk = take_slot()
                        give_slot(kk)
                        SB_, bd = STP[kk], STD[kk]
                        pe.op(lambda e: e.matmul(SB_[:, 0:nq], lhsT=shift_b, rhs=ot_[:, 0:nq], start=True, stop=True), r=[cstb.d, ot_.d], w=[bd])
                        dve.op(lambda e: e.tensor_copy(out=mixT[64:128, ch, q0:q0 + nq], in_=SB_[64:128, 0:nq]), r=[bd], w=[mixT.d])
                deferred.append((ui + 2, stage2))
                deferred.append((ui + 4, stage3))

        LA = 2
        for i in range(NU + LA):
            if i < NU:
                emit_qk(i)
            j = i - LA
            if j >= 0:
                emit_exp_pv(j)
            while deferred and deferred[0][0] <= j:
                deferred.pop(0)[1]()
        for _, fn in deferred:
            fn()
        for k_ in range(6):
            pb[k_].d = Dep()
        S.close()
        S = Scope(kb)
        wout_phase(S, mixT, mixT.d, ev_w_out_d, list(range(NT)), lambda j: j * 128, wo=wo_ev)
        S.close()
        SBC.close()
        SL.close()

    def odd_mixer():
        SL = Scope(kb)
        NTOK = 16 * 128
        mixT = SL.sb([128, 8, NTOK], BF16, "mixT1")
        wo_od = SL.sb([128, 8, D], BF16)
        S = Scope(kb)
        wod = S.sb([128, 8, 2048], BF16)
        load_w_bf16(wod, od_w_in_d, 8, 2048, colsplit=512)
        load_w_bf16(wo_od, od_w_out_d, 8, D)
        wpl = S.sb([128, 4, 128], BF16)
        xnT = S.sb([128, 8, NT * 128], BF16)
        xnT_d = [Dep() for _ in range(NT)]
        S1s = Scope(kb)
        wpl32 = S1s.sb([128, 4, 128], F32)
        sp.op(lambda e: e.dma_start(out=wpl32[:], in_=od_w_pool_d.rearrange("g c d -> c g d")), w=[wpl32.d], dma=True)
        dve.op(lambda e: e.tensor_copy(out=wpl[:], in_=wpl32[:]), r=[wpl32.d], w=[wpl.d])
        gn = S1s.sb([128, D], F32)
        sp.op(lambda e: e.dma_start(out=gn[:], in_=gains_d[2]), w=[gn.d], dma=True)
        ss2 = [S1s.sb([128, 4], F32) for _ in range(2)]
        xs2 = [S1s.sb([128, D], BF16) for _ in range(2)]
        nt_sequence(lambda j: (xres[:, j, :], xres_d[j]), NT, gn, ss2, xs2, lambda j: (xnT[:, :, j * 128:(j + 1) * 128], xnT_d[j]))
        S1s.close()
        if CUT <= 1:
            S.close(); SL.close(); return
        HL = 16
        W = HL + 512
        zb = [S.sb([128, W], F32) for _ in range(2)]
        pbuf = [S.sb([128, W], F32) for _ in range(2)]
        bgs = [S.sb([128, 512], BF16) for _ in range(2)]
        zc = S.sb([128, 4, HL], F32)
        pcar = S.sb([128, 4, HL], F32)
        cgs = S.sb([128, 512], BF16)
        y = S.sb([128, 512], F32)
        s1 = S.sb([128, W], F32)
        s2 = S.sb([128, W], F32)
        pooled = cgs
        dve.op(lambda e: e.memset(s1[:], 0.0), w=[s1.d])
        dve.op(lambda e: e.memset(s2[:], 0.0), w=[s2.d])
        groups = [(128 - HL, HL)] + [(128 + 512 * i, 512) for i in range(4)]
        work = [(gi, k) for gi in range(len(groups)) for k in range(4)]
        PB7 = [0, 1, 2, 3, 5, 6, 7]
        rot = [0]
        state = {}

        def nextbank():
            bk = PB7[rot[0] % 7]
            rot[0] += 1
            return bk

        def Pstage(n):
            gi, k = work[n]
            tok0, nn_ = groups[gi]
            xd = [xnT_d[i] for i in range(tok0 // 128, (tok0 + nn_ - 1) // 128 + 1)]
            banks = {}
            cols = [("cg", 512 + k * 128), ("hc", 1024 + k * 128), ("p", 1536 + k * 128)]
            if gi > 0:
                cols.append(("bg", k * 128))
            for nm, col in cols:
                bk = nextbank()
                banks[nm] = bk
                for c in range(8):
                    pe.op(lambda e: e.matmul(pb[bk][:, 0:nn_], lhsT=wod[:, c, col:col + 128], rhs=xnT[:, c, tok0:tok0 + nn_],
                                             start=(c == 0), stop=(c == 7)), r=[wod.d] + xd, w=[pb[bk].d])
            state[n] = banks

        def Estage(n):
            gi, k = work[n]
            tok0, nn_ = groups[gi]
            banks = state[n]
            z, p = zb[n % 2], pbuf[n % 2]
            act.op(lambda e: e.copy(out=cgs[:, 0:nn_], in_=pb[banks["cg"]][:, 0:nn_]), r=[pb[banks["cg"]].d], w=[cgs.d])
            if gi == 0:
                dve.op(lambda e: e.tensor_tensor(out=zc[:, k, :], in0=cgs[:, 0:nn_], in1=pb[banks["hc"]][:, 0:nn_], op=ALU.mult),
                       r=[cgs.d, pb[banks["hc"]].d], w=[zc.d])
                act.op(lambda e: e.copy(out=pcar[:, k, :], in_=pb[banks["p"]][:, 0:nn_]), r=[pb[banks["p"]].d], w=[pcar.d])
                return
            act.op(lambda e: e.copy(out=p[:, HL:W], in_=pb[banks["p"]][:, 0:nn_]), r=[pb[banks["p"]].d], w=[p.d])
            act.op(lambda e: e.copy(out=bgs[n % 2][:], in_=pb[banks["bg"]][:, 0:nn_]), r=[pb[banks["bg"]].d], w=[bgs[n % 2].d])
            dve.op(lambda e: e.tensor_tensor(out=z[:, HL:W], in0=cgs[:, 0:nn_], in1=pb[banks["hc"]][:, 0:nn_], op=ALU.mult),
                   r=[cgs.d, pb[banks["hc"]].d], w=[z.d])

        def Bstage(n):
            gi, k = work[n]
            if gi == 0:
                return
            tok0, nn_ = groups[gi]
            mcol = tok0 - 128
            z, p = zb[n % 2], pbuf[n % 2]
            dve.op(lambda e: e.tensor_copy(out=z[:, 0:HL], in_=zc[:, k, :]), r=[zc.d], w=[z.d])
            dve.op(lambda e: e.tensor_copy(out=p[:, 0:HL], in_=pcar[:, k, :]), r=[pcar.d], w=[p.d])
            dve.op(lambda e: e.tensor_copy(out=zc[:, k, :], in_=z[:, W - HL:W]), r=[z.d], w=[zc.d])
            dve.op(lambda e: e.tensor_copy(out=pcar[:, k, :], in_=p[:, W - HL:W]), r=[p.d], w=[pcar.d])
            dve.op(lambda e: e.tensor_scalar(out=y[:], in0=z[:, HL:W], scalar1=cw[:, k * 3 + 2:k * 3 + 3], scalar2=None, op0=ALU.mult),
                   r=[z.d, sm.d], w=[y.d])
            dve.op(lambda e: e.scalar_tensor_tensor(out=y[:], in0=z[:, HL - 1:W - 1], scalar=cw[:, k * 3 + 1:k * 3 + 2], in1=y[:], op0=ALU.mult, op1=ALU.add),
                   r=[z.d, sm.d, y.d], w=[y.d])
            dve.op(lambda e: e.scalar_tensor_tensor(out=y[:], in0=z[:, HL - 2:W - 2], scalar=cw[:, k * 3:k * 3 + 1], in1=y[:], op0=ALU.mult, op1=ALU.add),
                   r=[z.d, sm.d, y.d], w=[y.d])
            dve.op(lambda e: e.tensor_tensor(out=mixT[:, k, mcol:mcol + 512], in0=y[:], in1=bgs[n % 2][:], op=ALU.mult), r=[y.d, bgs[n % 2].d], w=[mixT.d])
            src, sd = p[:, :], p.d
            bufs = [s1, s2]
            sh = 1
            for m in range(k + 1):
                dst = bufs[m % 2]
                pool.op(lambda e: e.tensor_tensor(out=dst[:, sh:W], in0=src[:, sh:W], in1=src[:, 0:W - sh], op=ALU.add),
                        r=[sd], w=[dst.d])
                src, sd = dst[:, :], dst.d
                sh *= 2
            win = 2 ** (k + 1)
            dve.op(lambda e: e.scalar_tensor_tensor(out=pooled[:], in0=src[:, HL:W], scalar=1.0 / win, in1=p[:, HL:W], op0=ALU.mult, op1=ALU.subtract),
                   r=[sd, p.d], w=[pooled.d])
            if gi == 1:
                dve.op(lambda e: e.tensor_tensor(out=y[:, 0:16], in0=src[:, HL:HL + 16], in1=icnt[:, k * 16:(k + 1) * 16], op=ALU.mult),
                       r=[sd, pc.d, y.d], w=[y.d])
                dve.op(lambda e: e.tensor_tensor(out=pooled[:, 0:16], in0=y[:, 0:16], in1=p[:, HL:HL + 16], op=ALU.subtract),
                       r=[y.d, p.d], w=[pooled.d])
            pe.op(lambda e: e.matmul(pb[4][:, 0:512], lhsT=wpl[:, k, :], rhs=pooled[:], start=True, stop=True), r=[wpl.d, pooled.d], w=[pb[4].d])
            act.op(lambda e: e.activation(out=mixT[:, 4 + k, mcol:mcol + 512], in_=pb[4][:, 0:512], func=AF.Copy, scale=pscale[:, k:k + 1]),
                   r=[pb[4].d, sm.d], w=[mixT.d])

        NW = len(work)
        Pstage(0)
        Estage(0)
        for n in range(NW):
            if n + 1 < NW:
                Pstage(n + 1)
            Bstage(n)
            if n + 1 < NW:
                Estage(n + 1)
        S.close()
        if CUT <= 4:
            SL.close(); return
        S = Scope(kb)
        wout_phase(S, mixT, mixT.d, od_w_out_d, list(range(1, NT)), lambda j: (j - 1) * 128, wo=wo_od)
        S.close()
        SL.close()

    if L0:
        even_mixer()
        if dbg == "m0":
            dbg_dump()
        if CUT != 10:
            moe_layer(0, list(range(NT)))
        if dbg == "e0":
            dbg_dump()
    if L1:
        if not L0:
            load_xres()
        if CUT > 0:
            odd_mixer()
        if dbg == "m1":
            dbg_dump()
        out_evs = []
        def store_tile(j):
            out_evs.append(sp.op(lambda e: e.dma_start(out=out_d[(j - 1) * 128:j * 128, :], in_=xres[:, j, :]), r=[xres_d[j]], dma=True))
        if CUT > 5:
            moe_layer(1, list(range(1, NT)), on_final=store_tile)
    evs = []
    if L1:
        if CUT <= 5:
            for j in range(1, NT):
                evs.append(sp.op(lambda e: e.dma_start(out=out_d[(j - 1) * 128:j * 128, :], in_=xres[:, j, :]), r=[xres_d[j]], dma=True))
    else:
        for j in range(NT):
            evs.append(sp.op(lambda e: e.dma_start(out=out_d[j * 128:(j + 1) * 128, :], in_=xres[:, j, :]), r=[xres_d[j]], dma=True))
    kb.barrier([sp])
    return nc, kb


def _consts():
    cst = np.zeros((128, 640), np.float32)
    idx = np.arange(128)
    cst[:, 0:128] = np.eye(128, dtype=np.float32)
    cst[:, 128:256] = (idx[:, None] <= idx[None, :]).astype(np.float32)
    cst[127, 256:384] = 1.0
    cst[:, 384:512] = np.where(idx[:, None] <= idx[None, :], 0.0, NEG)
    cst[np.arange(64), 512 + 64 + np.arange(64)] = 1.0
    selall = np.zeros((16, 2048), np.float32)
    for e in range(16):
        selall[e, e * 128:(e + 1) * 128] = 1.0
    return cst, selall


def _percore(hf):
    pc = np.zeros((128, 68), np.float32)
    v = 1.0 if hf == 1 else 0.0
    pc[:, 0] = v
    pc[:, 1] = -v
    pc[:, 2] = 1.0
    pc[:, 3] = -1.0
    t = np.arange(16)
    for k, win in enumerate((2, 4, 8, 16)):
        cnt = np.minimum(t + 1, win) if hf == 0 else np.full(16, win)
        pc[:, 4 + k * 16:4 + (k + 1) * 16] = (1.0 / cnt)[None, :]
    return pc


def _make_inputs(inputs):
    f = lambda a: np.ascontiguousarray(np.asarray(a, dtype=np.float32))
    x = f(inputs["x"])
    cst, selall = _consts()
    p = np.arange(128)
    sm = np.zeros((128, 454), np.float32)
    sm[:, 0:12] = f(inputs["ev_b_forget"])[0][None, :]
    sm[:, 12:76] = f(inputs["ev_g_q"])[0][None, :]
    sm[:, 76:140] = f(inputs["ev_g_k"])[0][None, :]
    gv = f(inputs["ev_g_v"])[0]
    bs = f(inputs["ev_b_s"])[0]
    for c in range(2):
        sm[:, 140 + c] = gv[2 * c + p // 64, p % 64]
        sm[:, 142 + c * 128:142 + (c + 1) * 128] = bs[2 * c + p // 64, :]
    cwv = f(inputs["od_conv_w"])[0]
    for k in range(4):
        for tap in range(3):
            sm[:, 398 + k * 3 + tap] = cwv[tap, k * 128 + p]
        sm[:, 410 + k] = f(inputs["od_pool_scale"])[0][k * 128 + p]
    bg = f(inputs["moe_b_group"])
    br = f(inputs["moe_b_router"]).reshape(2, 16)
    for l in range(2):
        sm[:, 414 + l * 20:414 + l * 20 + 4] = bg[l][None, :]
        sm[:, 414 + l * 20 + 4:414 + (l + 1) * 20] = br[l][None, :]
    gains = np.stack([np.broadcast_to(f(inputs["ev_norm"])[0], (128, D)), np.broadcast_to(f(inputs["moe_norm"])[0], (128, D)),
                      np.broadcast_to(f(inputs["od_norm"])[0], (128, D)), np.broadcast_to(f(inputs["moe_norm"])[1], (128, D))]).astype(np.float32)
    wr = np.concatenate([f(inputs["moe_w_group"]), f(inputs["moe_w_router"]).reshape(2, D, 16)], axis=2)
    shared = {
        "cst": cst, "selall": selall, "smalls": sm, "gains": np.ascontiguousarray(gains),
        "ev_w_in": f(inputs["ev_w_in"])[0], "ev_wsT": np.ascontiguousarray(f(inputs["ev_w_s"])[0].transpose(0, 2, 1)),
        "ev_w_out": f(inputs["ev_w_out"])[0], "od_w_in": f(inputs["od_w_in"])[0], "od_w_pool": f(inputs["od_w_pool"])[0],
        "od_w_out": f(inputs["od_w_out"])[0], "wr": np.ascontiguousarray(wr),
        "moe_w_gate": f(inputs["moe_w_gate"]), "moe_w_up": f(inputs["moe_w_up"]), "moe_w_down": f(inputs["moe_w_down"]),
    }
    in_maps = []
    for c in range(8):
        b, hf = c // 2, c % 2
        m = dict(shared)
        m["pc"] = _percore(hf)
        if hf == 1:
            m["xp"] = np.ascontiguousarray(x[b, 0:NPRE * 128])
            m["xo"] = np.ascontiguousarray(x[b, NPRE * 128:4096])
        else:
            m["xp"] = np.zeros((NPRE * 128, D), np.float32)
            m["xo"] = np.ascontiguousarray(np.concatenate([np.zeros((128, D), np.float32), x[b, 0:2048]], axis=0))
        in_maps.append(m)
    return in_maps


_CACHE = {}


def kernel(**inputs):
    in_maps = _make_inputs(inputs)
    if "nc" not in _CACHE:
        _CACHE["nc"] = build((0, 1))[0]
    nc = _CACHE["nc"]
    res = run_bass_kernel_spmd(nc, in_maps, core_ids=list(range(8)))
    out = np.zeros((4, 4096, D), np.float32)
    for c in range(8):
        b, hf = c // 2, c % 2
        out[b, hf * 2048:(hf + 1) * 2048] = res.results[c]["out"]
    return out
```

```python
import contextlib
import os
CUT = int(os.environ.get('KCUT', '99'))
KSUB = int(os.environ.get('KSUB', '0'))
import numpy as np
import concourse.bass as bass
import concourse.mybir as mybir
from concourse.bass_utils import run_bass_kernel_spmd

F32 = mybir.dt.float32
BF16 = mybir.dt.bfloat16
AF = mybir.ActivationFunctionType
ALU = mybir.AluOpType
AX = mybir.AxisListType

EPOCH = 28000
NT = 17
NPRE = 15
NSEQ = 32
D = 1024
EPS = 1e-6
NEG = -30000.0


class Dep:
    __slots__ = ("w", "rs")

    def __init__(self):
        self.w = None
        self.rs = {}


class Eng:
    def __init__(self, kb, eng, name, sync_self):
        self.kb = kb
        self.eng = eng
        self.name = name
        self.sync_self = sync_self
        self.sem = kb.nc.alloc_semaphore(name=f"s_{name}_0")
        self.nep = 0
        self.cnt = 0
        self.waited = {}
        self.nins = 0
        self.allsems = [self.sem]

    def _collect(self, reads, writes):
        need = {}

        def add(ev):
            if ev is None:
                return
            sem, val, src = ev
            if src is self and not self.sync_self:
                return
            k = id(sem)
            if self.waited.get(k, 0) >= val:
                return
            if k not in need or need[k][1] < val:
                need[k] = (sem, val)

        for d in reads:
            add(d.w)
        for d in writes:
            add(d.w)
            for ev in d.rs.values():
                add(ev)
        return list(need.values())

    def op(self, fn, r=(), w=(), dma=False):
        items = self._collect(r, w)
        if dma:
            sem, val, prev = self.kb.dma_slot("sw" if self.name == "pool" else "hw")
            if prev > 0 and self.waited.get(id(sem), 0) < prev:
                items.append((sem, prev))
        for (s, v) in items[1:]:
            self.eng.wait_ge(s, v)
            self.waited[id(s)] = max(self.waited.get(id(s), 0), v)
        ins = fn(self.eng)
        if items:
            s, v = items[0]
            ins._wait_ge(s, v)
            self.waited[id(s)] = max(self.waited.get(id(s), 0), v)
        self.nins += 1
        if dma:
            ins.then_inc(sem, 16)
            ev = (sem, val, None)
        else:
            if self.cnt >= EPOCH:
                self.nep += 1
                self.sem = self.kb.nc.alloc_semaphore(name=f"s_{self.name}_{self.nep}")
                self.allsems.append(self.sem)
                self.cnt = 0
            self.cnt += 1
            ins.then_inc(self.sem, 1)
            ev = (self.sem, self.cnt, self)
        for d in r:
            d.rs[id(ev[0])] = ev
        for d in w:
            d.w = ev
            d.rs = {}
        return ev

    def wait_sv(self, sem, val):
        if val <= 0 or self.waited.get(id(sem), 0) >= val:
            return
        self.eng.wait_ge(sem, val)
        self.waited[id(sem)] = val


class KB:
    def __init__(self, nc, n_dma_sems=48):
        self.nc = nc
        self.pe = Eng(self, nc.tensor, "pe", False)
        self.act = Eng(self, nc.scalar, "act", True)
        self.dve = Eng(self, nc.vector, "dve", True)
        self.pool = Eng(self, nc.gpsimd, "pool", True)
        self.sp = Eng(self, nc.sync, "sp", True)
        self.engs = [self.pe, self.act, self.dve, self.pool, self.sp]
        self.dsems_hw = [[nc.alloc_semaphore(name=f"dmah{i}"), 0] for i in range(32)]
        self.dsems_sw = [[nc.alloc_semaphore(name=f"dmas{i}"), 0] for i in range(24)]
        self.dsems = self.dsems_hw + self.dsems_sw
        self.dnext = {"hw": 0, "sw": 0}
        self.nt = 0

    def dma_slot(self, kind):
        lst = self.dsems_hw if kind == "hw" else self.dsems_sw
        slot = lst[self.dnext[kind]]
        self.dnext[kind] = (self.dnext[kind] + 1) % len(lst)
        prev = slot[1]
        slot[1] += 16
        return slot[0], slot[1], prev

    def barrier(self, engs=None):
        for e in (engs or self.engs):
            for x in self.engs:
                if x is e:
                    continue
                e.wait_sv(x.sem, x.cnt)
            for sem, val in self.dsems:
                e.wait_sv(sem, val)


class T:
    def __init__(self, t):
        self.t = t
        self.d = Dep()

    def __getitem__(self, k):
        return self.t[k]


class Scope:
    def __init__(self, kb):
        self.kb = kb
        self.st = contextlib.ExitStack()

    def sb(self, shape, dtype, name=None):
        self.kb.nt += 1
        t = self.st.enter_context(self.kb.nc.sbuf_tensor(f"sb{self.kb.nt}_{name or 't'}", list(shape), dtype))
        return T(t)

    def close(self):
        self.kb.barrier()
        self.st.close()


def build(layers=(0, 1), dbg=None):
    nc = bass.Bass("TRN2", target_bir_lowering=False)
    kb = KB(nc)
    pe, act, dve, pool, sp = kb.pe, kb.act, kb.dve, kb.pool, kb.sp
    L0 = 0 in layers
    L1 = 1 in layers

    def din(name, shape, dt=F32):
        return nc.dram_tensor(name, list(shape), dt, kind="ExternalInput").ap()

    xo_d = din("xo", [NT * 128, D])
    xp_d = din("xp", [NPRE * 128, D])
    cst_d = din("cst", [128, 640])
    selall_d = din("selall", [16, 2048])
    pc_d = din("pc", [128, 68])
    sm_d = din("smalls", [128, 454])
    gains_d = din("gains", [4, 128, D])
    ev_w_in_d = din("ev_w_in", [D, 2828])
    ev_wsT_d = din("ev_wsT", [4, 128, 128])
    ev_w_out_d = din("ev_w_out", [D, D])
    od_w_in_d = din("od_w_in", [D, 2048])
    od_w_pool_d = din("od_w_pool", [4, 128, 128])
    od_w_out_d = din("od_w_out", [D, D])
    wr_d = din("wr", [2, D, 20])
    wg_d = din("moe_w_gate", [2, 16, D, 256])
    wu_d = din("moe_w_up", [2, 16, D, 256])
    wd_d = din("moe_w_down", [2, 16, 256, D])
    if L1:
        out_d = nc.dram_tensor("out", [16 * 128, D], F32, kind="ExternalOutput").ap()
    else:
        out_d = nc.dram_tensor("out", [NT * 128, D], F32, kind="ExternalOutput").ap()
    dbg_d = None
    if dbg:
        dbg_d = nc.dram_tensor("dbg", [NT * 128, D], F32, kind="ExternalOutput").ap()
    kT_d = T(nc.dram_tensor("kT_scr", [12, 70, NSEQ * 128], BF16).ap())
    qT_d = T(nc.dram_tensor("qT_scr", [12, 70, NT * 128], BF16).ap())
    V_d = T(nc.dram_tensor("V_scr", [NSEQ, 128, 780], BF16).ap())

    pbig = [nc.alloc_psum_tensor(f"bankpair{i}", [128, 1024], F32) for i in range(4)]
    pb = [T(pbig[i // 2][:, (i % 2) * 512:(i % 2 + 1) * 512]) for i in range(8)]

    def pbf(i):
        return pb[i][:].bitcast(BF16).rearrange("p (a b) -> p a b", b=128)

    G = Scope(kb)
    xres = G.sb([128, NT, D], F32, "xres")
    xres_d = [Dep() for _ in range(NT)]
    cst32 = G.sb([128, 640], F32, "cst32")
    cstb = G.sb([128, 640], BF16, "cstb")
    pc = G.sb([128, 68], F32, "pc")
    sm = G.sb([128, 454], F32, "smalls_sb")
    ident_b = cstb[:, 0:128]
    maskT_b = cstb[:, 384:512]
    shift_b = cstb[0:64, 512:640]
    ident_f = cst32[:, 0:128]
    U_f = cst32[:, 128:256]
    sel127_f = cst32[:, 256:384]
    ones_f = G.sb([128, 128], F32, "ones_f")
    bfb = sm[:, 0:12]
    gq_b = sm[:, 12:76]
    gk_b = sm[:, 76:140]
    gvcol = sm[:, 140:142]
    bsl = sm[:, 142:398]
    cw = sm[:, 398:410]
    pscale = sm[:, 410:414]
    brt = sm[:, 414:454]
    valid = pc[:, 0:1]
    nvalid = pc[:, 1:2]
    icnt = pc[:, 4:68]

    sp.op(lambda e: e.dma_start(out=cst32[:], in_=cst_d), w=[cst32.d], dma=True)
    pool.op(lambda e: e.dma_start(out=cstb[:], in_=cst_d), w=[cstb.d], dma=True)
    sp.op(lambda e: e.dma_start(out=pc[:], in_=pc_d), w=[pc.d], dma=True)
    sp.op(lambda e: e.dma_start(out=sm[:], in_=sm_d), w=[sm.d], dma=True)
    dve.op(lambda e: e.memset(ones_f[:], 1.0), w=[ones_f.d])

    def load_xres():
        for j in range(NT):
            sp.op(lambda e: e.dma_start(out=xres[:, j, :], in_=xo_d[j * 128:(j + 1) * 128, :]), w=[xres_d[j]], dma=True)

    def rstd_from_ss(S, ss, n, scale, width):
        dve.op(lambda e: e.tensor_scalar(out=ss[:, 0:width], in0=ss[:, 0:width], scalar1=1.0 / n, scalar2=EPS,
                                         op0=ALU.mult, op1=ALU.add), r=[ss.d], w=[ss.d])
        act.op(lambda e: e.activation(out=ss[:, 0:width], in_=ss[:, 0:width], func=AF.Ln), r=[ss.d], w=[ss.d])
        act.op(lambda e: e.activation(out=ss[:, 0:width], in_=ss[:, 0:width], func=AF.Exp, scale=-0.5), r=[ss.d], w=[ss.d])

    def nt_a(xsrc_ap, xsrc_dep, gn, ss, xs):
        act.op(lambda e: e.activation(out=xs[:], in_=xsrc_ap, func=AF.Square, accum_out=ss[:, 0:1]),
               r=[xsrc_dep], w=[xs.d, ss.d])
        rstd_from_ss(None, ss, D, 1.0, 1)
        dve.op(lambda e: e.scalar_tensor_tensor(out=xs[:], in0=xsrc_ap, scalar=ss[:, 0:1], in1=gn[:],
                                                op0=ALU.mult, op1=ALU.mult), r=[xsrc_dep, ss.d, gn.d], w=[xs.d])

    def nt_pe(xs, bank):
        pv = pbf(bank)
        for c in range(8):
            pe.op(lambda e: e.transpose(out=pv[:, c, :], in_=xs[:, c * 128:(c + 1) * 128], identity=ident_b),
                  r=[xs.d, cstb.d], w=[pb[bank].d])

    def nt_copy(dstT_ap, dst_dep, bank, on_dve=False):
        if on_dve:
            dve.op(lambda e: e.tensor_copy(out=dstT_ap, in_=pbf(bank)), r=[pb[bank].d], w=[dst_dep])
        else:
            act.op(lambda e: e.copy(out=dstT_ap, in_=pbf(bank)), r=[pb[bank].d], w=[dst_dep])

    def norm_transpose(S, xsrc_ap, xsrc_dep, gn, junk, ss, xs, dstT_ap, dst_dep, bank):
        nt_a(xsrc_ap, xsrc_dep, gn, ss, xs)
        nt_pe(xs, bank)
        nt_copy(dstT_ap, dst_dep, bank)

    def nt_sequence(tile_src, n, gn, ss2, xs2, dst, after=None):
        a0, d0 = tile_src(0)
        nt_a(a0, d0, gn, ss2[0], xs2[0])
        for i in range(n):
            bank = 0 if i % 2 == 0 else 2
            nt_pe(xs2[i % 2], bank)
            if i + 1 < n:
                a1, d1 = tile_src(i + 1)
                nt_a(a1, d1, gn, ss2[(i + 1) % 2], xs2[(i + 1) % 2])
            da, dd = dst(i)
            nt_copy(da, dd, bank, on_dve=(i % 2 == 0))
            if after is not None and i >= 1:
                after(i - 1)
        if after is not None:
            after(n - 1)

    def load_w_bf16(dst, src_ap, nchunk, ncols, colsplit=1024):
        for c0 in range(0, ncols, colsplit):
            c1 = min(ncols, c0 + colsplit)
            pool.op(lambda e: e.dma_start(out=dst[:, :, c0:c1],
                                          in_=src_ap[:, c0:c1].rearrange("(c p) n -> p c n", p=128)),
                    w=[dst.d], dma=True)

    def dbg_dump():
        if dbg_d is None:
            return
        for j in range(NT):
            sp.op(lambda e: e.dma_start(out=dbg_d[j * 128:(j + 1) * 128, :], in_=xres[:, j, :]), r=[xres_d[j]], dma=True)

    def moe_layer(l, tiles, on_final=None):
        t0 = tiles[0]
        ntl = len(tiles)
        ntok = ntl * 128
        S = Scope(kb)
        gn = S.sb([128, D], F32)
        sp.op(lambda e: e.dma_start(out=gn[:], in_=gains_d[(1 if l == 0 else 3)]), w=[gn.d], dma=True)
        wr = S.sb([128, 8, 20], BF16)
        pool.op(lambda e: e.dma_start(out=wr[:], in_=wr_d[l].rearrange("(c p) n -> p c n", p=128)), w=[wr.d], dma=True)
        xnT = S.sb([128, 8, ntok], BF16)
        xnT_d = [Dep() for _ in range(ntl)]
        lg = S.sb([128, ntl, 20], F32)
        ss2 = [S.sb([128, 4], F32) for _ in range(2)]
        xs2 = [S.sb([128, D], BF16) for _ in range(2)]
        wg = [S.sb([128, 8, 256], BF16) for _ in range(2)]
        wu = [S.sb([128, 8, 256], BF16) for _ in range(2)]
        wd = [S.sb([128, 2, D], BF16) for _ in range(2)]

        def load_expert(e_):
            b = e_ % 2
            pool.op(lambda e: e.dma_start(out=wg[b][:], in_=wg_d[l, e_].rearrange("(c p) n -> p c n", p=128)), w=[wg[b].d], dma=True)
            pool.op(lambda e: e.dma_start(out=wu[b][:], in_=wu_d[l, e_].rearrange("(c p) n -> p c n", p=128)), w=[wu[b].d], dma=True)
            pool.op(lambda e: e.dma_start(out=wd[b][:], in_=wd_d[l, e_].rearrange("(c p) n -> p c n", p=128)), w=[wd[b].d], dma=True)

        load_expert(0)
        load_expert(1)
        def router_(i):
            for c in range(8):
                pe.op(lambda e: e.matmul(pb[1][:, 0:20], lhsT=xnT[:, c, i * 128:(i + 1) * 128], rhs=wr[:, c, :],
                                         start=(c == 0), stop=(c == 7)), r=[xnT_d[i], wr.d], w=[pb[1].d])
            dve.op(lambda e: e.tensor_tensor(out=lg[:, i, :], in0=pb[1][:, 0:20], in1=brt[:, l * 20:(l + 1) * 20], op=ALU.add),
                   r=[pb[1].d, sm.d], w=[lg.d])
        nt_sequence(lambda i: (xres[:, tiles[i], :], xres_d[tiles[i]]), ntl, gn, ss2, xs2,
                    lambda i: (xnT[:, :, i * 128:(i + 1) * 128], xnT_d[i]), after=router_)
        n4 = [128, ntl, 4]
        gmax = S.sb([128, ntl], F32)
        ge = S.sb(n4, F32)
        gsum = S.sb([128, ntl], F32)
        gsel = S.sb(n4, F32)
        tmp16 = S.sb([128, ntl, 16], F32)
        el = S.sb(n4, F32)
        emax = S.sb([128, ntl], F32)
        ee = S.sb(n4, F32)
        oh1 = S.sb(n4, F32)
        ee2 = S.sb(n4, F32)
        m2 = S.sb([128, ntl], F32)
        oh2 = S.sb(n4, F32)
        w1 = S.sb([128, ntl], F32)
        w2 = S.sb([128, ntl], F32)
        cw4 = S.sb(n4, F32)
        comb = S.sb([128, ntl, 16], F32)
        glog = lg[:, :, 0:4]
        elog = lg[:, :, 4:20]

        def bc_last(ap2, n):
            return ap2.unsqueeze(2).to_broadcast([128, ntl, n])

        dve.op(lambda e: e.tensor_reduce(out=gmax[:], in_=glog, axis=AX.X, op=ALU.max), r=[lg.d], w=[gmax.d])
        dve.op(lambda e: e.tensor_tensor(out=ge[:], in0=glog, in1=bc_last(gmax[:], 4), op=ALU.subtract), r=[lg.d, gmax.d], w=[ge.d])
        dve.op(lambda e: e.tensor_single_scalar(out=gsel[:], in_=ge[:], scalar=0.0, op=ALU.is_ge), r=[ge.d], w=[gsel.d])
        act.op(lambda e: e.activation(out=ge[:], in_=ge[:], func=AF.Exp), r=[ge.d], w=[ge.d])
        dve.op(lambda e: e.tensor_reduce(out=gsum[:], in_=ge[:], axis=AX.X, op=ALU.add), r=[ge.d], w=[gsum.d])
        dve.op(lambda e: e.reciprocal(out=gsum[:], in_=gsum[:]), r=[gsum.d], w=[gsum.d])
        dve.op(lambda e: e.tensor_tensor(out=tmp16[:].rearrange("p t (g e) -> p t g e", e=4),
                                         in0=elog.rearrange("p t (g e) -> p t g e", e=4),
                                         in1=gsel[:].unsqueeze(3).to_broadcast([128, ntl, 4, 4]), op=ALU.mult),
               r=[lg.d, gsel.d], w=[tmp16.d])
        dve.op(lambda e: e.tensor_reduce(out=el[:], in_=tmp16[:].rearrange("p t (g e) -> p t e g", e=4), axis=AX.X, op=ALU.add),
               r=[tmp16.d], w=[el.d])
        dve.op(lambda e: e.tensor_reduce(out=emax[:], in_=el[:], axis=AX.X, op=ALU.max), r=[el.d], w=[emax.d])
        dve.op(lambda e: e.tensor_tensor(out=ee[:], in0=el[:], in1=bc_last(emax[:], 4), op=ALU.subtract), r=[el.d, emax.d], w=[ee.d])
        dve.op(lambda e: e.tensor_single_scalar(out=oh1[:], in_=ee[:], scalar=0.0, op=ALU.is_ge), r=[ee.d], w=[oh1.d])
        act.op(lambda e: e.activation(out=ee[:], in_=ee[:], func=AF.Exp), r=[ee.d], w=[ee.d])
        dve.op(lambda e: e.scalar_tensor_tensor(out=ee2[:], in0=oh1[:], scalar=-4.0, in1=ee[:], op0=ALU.mult, op1=ALU.add),
               r=[oh1.d, ee.d], w=[ee2.d])
        dve.op(lambda e: e.tensor_reduce(out=m2[:], in_=ee2[:], axis=AX.X, op=ALU.max), r=[ee2.d], w=[m2.d])
        dve.op(lambda e: e.tensor_tensor(out=oh2[:], in0=ee2[:], in1=bc_last(m2[:], 4), op=ALU.is_ge), r=[ee2.d, m2.d], w=[oh2.d])
        dve.op(lambda e: e.tensor_scalar(out=w1[:], in0=m2[:], scalar1=1.0, scalar2=None, op0=ALU.add), r=[m2.d], w=[w1.d])
        dve.op(lambda e: e.reciprocal(out=w1[:], in_=w1[:]), r=[w1.d], w=[w1.d])
        dve.op(lambda e: e.tensor_tensor(out=w1[:], in0=w1[:], in1=gsum[:], op=ALU.mult), r=[w1.d, gsum.d], w=[w1.d])
        dve.op(lambda e: e.tensor_tensor(out=w2[:], in0=w1[:], in1=m2[:], op=ALU.mult), r=[w1.d, m2.d], w=[w2.d])
        dve.op(lambda e: e.tensor_tensor(out=cw4[:], in0=oh1[:], in1=bc_last(w1[:], 4), op=ALU.mult), r=[oh1.d, w1.d], w=[cw4.d])
        dve.op(lambda e: e.tensor_tensor(out=oh2[:], in0=oh2[:], in1=bc_last(w2[:], 4), op=ALU.mult), r=[oh2.d, w2.d], w=[oh2.d])
        dve.op(lambda e: e.tensor_tensor(out=cw4[:], in0=cw4[:], in1=oh2[:], op=ALU.add), r=[cw4.d, oh2.d], w=[cw4.d])
        dve.op(lambda e: e.tensor_copy(out=comb[:].rearrange("p t (g e) -> p t g e", e=4),
                                       in_=cw4[:].unsqueeze(2).to_broadcast([128, ntl, 4, 4])), r=[cw4.d], w=[comb.d])
        dve.op(lambda e: e.tensor_tensor(out=comb[:].rearrange("p t (g e) -> p t g e", e=4),
                                         in0=comb[:].rearrange("p t (g e) -> p t g e", e=4),
                                         in1=gsel[:].unsqueeze(3).to_broadcast([128, ntl, 4, 4]), op=ALU.mult),
               r=[comb.d, gsel.d], w=[comb.d])
        groups = []
        c0 = 0
        while c0 < ntok:
            n = min(512, ntok - c0)
            groups.append((c0, n))
            c0 += n
        sg = [S.sb([128, 512], F32) for _ in range(2)]
        hid = [[S.sb([128, 512], BF16) for _ in range(2)] for _ in range(2)]
        items = [(e_, tok0, n) for e_ in range(16) for (tok0, n) in groups]
        ybank = [0]

        def GU(i):
            e_, tok0, n = items[i]
            b = e_ % 2
            k2 = i % 2
            for hc in range(2):
                gb, ub = pb[hc * 2], pb[hc * 2 + 1]
                xd = [xnT_d[i_] for i_ in range(tok0 // 128, (tok0 + n) // 128)]
                for c in range(8):
                    pe.op(lambda e: e.matmul(gb[:, 0:n], lhsT=wg[b][:, c, hc * 128:(hc + 1) * 128], rhs=xnT[:, c, tok0:tok0 + n],
                                             start=(c == 0), stop=(c == 7)), r=[wg[b].d] + xd, w=[gb.d])
                for c in range(8):
                    pe.op(lambda e: e.matmul(ub[:, 0:n], lhsT=wu[b][:, c, hc * 128:(hc + 1) * 128], rhs=xnT[:, c, tok0:tok0 + n],
                                             start=(c == 0), stop=(c == 7)), r=[wu[b].d] + xd, w=[ub.d])
                act.op(lambda e: e.activation(out=sg[hc][:, 0:n], in_=gb[:, 0:n], func=AF.Silu), r=[gb.d], w=[sg[hc].d])
                dve.op(lambda e: e.tensor_tensor(out=hid[k2][hc][:, 0:n], in0=sg[hc][:, 0:n], in1=ub[:, 0:n], op=ALU.mult),
                       r=[sg[hc].d, ub.d], w=[hid[k2][hc].d])

        def DOWN(i):
            e_, tok0, n = items[i]
            b = e_ % 2
            k2 = i % 2
            for tt in range(n // 128):
                j = t0 + tok0 // 128 + tt
                for nn in range(2):
                    yb = pb[5 + ybank[0]]
                    ybank[0] = (ybank[0] + 1) % 3
                    for hc in range(2):
                        pe.op(lambda e: e.matmul(yb[:, :], lhsT=hid[k2][hc][:, tt * 128:(tt + 1) * 128], rhs=wd[b][:, hc, nn * 512:(nn + 1) * 512],
                                                 start=(hc == 0), stop=(hc == 1)), r=[hid[k2][hc].d, wd[b].d], w=[yb.d])
                    ti = tok0 // 128 + tt
                    dve.op(lambda e: e.scalar_tensor_tensor(out=xres[:, j, nn * 512:(nn + 1) * 512], in0=yb[:, :], scalar=comb[:, ti, e_:e_ + 1],
                                                            in1=xres[:, j, nn * 512:(nn + 1) * 512], op0=ALU.mult, op1=ALU.add),
                           r=[yb.d, comb.d, xres_d[j]], w=[xres_d[j]])
                if on_final is not None and e_ == 15:
                    on_final(j)
            if (i + 1 == len(items) or items[i + 1][0] != e_) and e_ + 2 < 16:
                load_expert(e_ + 2)

        GU(0)
        for i in range(len(items)):
            if i + 1 < len(items):
                GU(i + 1)
            DOWN(i)
        S.close()

    def wout_phase(S, mixT, mix_d, w_src, tiles, col_of_tile, wo=None):
        if wo is None:
            wo = S.sb([128, 8, D], BF16)
            load_w_bf16(wo, w_src, 8, D)
        bank = 0
        for j in tiles:
            cc = col_of_tile(j)
            for nn in range(2):
                yb = pb[bank]
                bank = (bank + 1) % 4
                for c in range(8):
                    pe.op(lambda e: e.matmul(yb[:, :], lhsT=mixT[:, c, cc:cc + 128], rhs=wo[:, c, nn * 512:(nn + 1) * 512],
                                             start=(c == 0), stop=(c == 7)), r=[mix_d, wo.d], w=[yb.d])
                dve.op(lambda e: e.tensor_tensor(out=xres[:, j, nn * 512:(nn + 1) * 512], in0=xres[:, j, nn * 512:(nn + 1) * 512],
                                                 in1=yb[:, :], op=ALU.add), r=[yb.d, xres_d[j]], w=[xres_d[j]])

    def even_mixer():
        SL = Scope(kb)
        mixT = SL.sb([128, 8, NT * 128], BF16, "mixT0")
        S = Scope(kb)
        gn = S.sb([128, D], F32)
        sp.op(lambda e: e.dma_start(out=gn[:], in_=gains_d[0]), w=[gn.d], dma=True)
        wev = S.sb([128, 8, 2828], BF16)
        for (c0_, c1_) in ((1280, 2054), (2054, 2828)):
            pool.op(lambda e: e.dma_start(out=wev[:, :, c0_:c1_], in_=ev_w_in_d[:, c0_:c1_].rearrange("(c p) n -> p c n", p=128)), w=[wev.d], dma=True)
        wev_q = Dep()
        wsT = S.sb([128, 4, 128], BF16)
        pool.op(lambda e: e.dma_start(out=wsT[:], in_=ev_wsT_d.rearrange("g s t -> s g t")), w=[wsT.d], dma=True)
        Ub = cstb[:, 128:256]
        dve.op(lambda e: e.tensor_tensor(out=wsT[:], in0=wsT[:], in1=Ub.unsqueeze(1).to_broadcast([128, 4, 128]), op=ALU.mult),
               r=[wsT.d, cstb.d], w=[wsT.d])
        gqk = S.sb([128, 64], F32)
        dve.op(lambda e: e.scalar_tensor_tensor(out=gqk[:], in0=gq_b, scalar=0.125, in1=gk_b, op0=ALU.mult, op1=ALU.mult),
               r=[sm.d], w=[gqk.d])
        xin = [S.sb([128, D], F32) for _ in range(2)]
        ss2 = [S.sb([128, 4], F32) for _ in range(2)]
        xs2 = [S.sb([128, D], BF16) for _ in range(2)]
        xnT = [S.sb([128, 8, 128], BF16) for _ in range(2)]
        sq = S.sb([128, 768], BF16)
        Kraw = S.sb([128, 768], BF16)
        Qraw = S.sb([128, 768], BF16)
        ssall = S.sb([128, 28], F32)
        kaug = [S.sb([128, 12, 70], BF16) for _ in range(2)]
        qaug = [S.sb([128, 12, 70], BF16) for _ in range(2)]
        vaug = [S.sb([128, 12, 65], BF16) for _ in range(2)]
        kTs = [S.sb([70, 12, 128], BF16) for _ in range(2)]
        qTs = [S.sb([70, 12, 128], BF16) for _ in range(2)]
        zt = S.sb([128, 12], F32)
        ls = S.sb([128, 12], F32)
        Fc = [S.sb([128, 12], F32) for _ in range(2)]
        Fh = S.sb([128, 12], BF16)
        Fm = S.sb([128, 12], BF16)
        Fl = S.sb([128, 12], BF16)
        r1 = S.sb([128, 12], F32)
        qtmp = sq
        gv = S.sb([128, 256], F32)
        vpad = S.sb([128, 4, 128], BF16)
        guT = [S.sb([128, 256], F32) for _ in range(2)]
        stmp = S.sb([128, 256], F32)
        gv2 = stmp
        for b_ in range(2):
            dve.op(lambda e: e.memset(kaug[b_][:, :, 64:67], 1.0), w=[kaug[b_].d])
            dve.op(lambda e: e.memset(qaug[b_][:, :, 67:70], 1.0), w=[qaug[b_].d])
        dve.op(lambda e: e.memset(vpad[:], 0.0), w=[vpad.d])

        def own_(i):
            return i >= NPRE

        def S1a(i):
            b2 = i % 2
            if own_(i):
                j = i - NPRE
                xsrc, xdep = xres[:, j, :], xres_d[j]
            else:
                pool.op(lambda e: e.dma_start(out=xin[b2][:], in_=xp_d[i * 128:(i + 1) * 128, :]), w=[xin[b2].d], dma=True)
                xsrc, xdep = xin[b2][:], xin[b2].d
            nt_a(xsrc, xdep, gn, ss2[b2], xs2[b2])

        def S1b(i):
            b2 = i % 2
            nt_pe(xs2[b2], 0)
            nt_copy(xnT[b2][:], xnT[b2].d, 0, on_dve=(i % 2 == 0))

        def S2(i):
            xT = xnT[i % 2]

            def proj(bank, c_lo, c_hi, w_lo):
                n = c_hi - c_lo
                wdep = wev.d if w_lo >= 1280 else wev_q
                for c in range(8):
                    pe.op(lambda e: e.matmul(pb[bank][:, c_lo:c_hi], lhsT=xT[:, c, :], rhs=wev[:, c, w_lo:w_lo + n],
                                             start=(c == 0), stop=(c == 7)), r=[xT.d, wdep], w=[pb[bank].d])
            proj(1, 0, 512, 1280)
            proj(2, 0, 256, 1792)
            proj(2, 256, 512, 2048)
            proj(3, 0, 512, 2304)
            proj(4, 0, 12, 2816)
            if own_(i):
                proj(5, 0, 512, 512)
                proj(6, 0, 256, 1024)
                proj(6, 256, 512, 256)
                for uc in range(2):
                    for c in range(8):
                        pe.op(lambda e: e.matmul(pb[7][:, uc * 128:(uc + 1) * 128], lhsT=wev[:, c, uc * 128:(uc + 1) * 128], rhs=xT[:, c, :],
                                                 start=(c == 0), stop=(c == 7)), r=[xT.d, wev_q], w=[pb[7].d])

        def S3(i):
            b2 = i % 2
            vflag = valid if i <= NPRE else 1.0
            nvflag = nvalid if i <= NPRE else -1.0
            va = vaug[b2]
            dve.op(lambda e: e.tensor_tensor(out=zt[:], in0=pb[4][:, 0:12], in1=bfb, op=ALU.add), r=[pb[4].d, sm.d], w=[zt.d])
            act.op(lambda e: e.activation(out=zt[:], in_=zt[:], func=AF.Exp, scale=-1.0), r=[zt.d], w=[zt.d])
            act.op(lambda e: e.activation(out=Kraw[:, 0:512], in_=pb[1][:, 0:512], func=AF.Copy), r=[pb[1].d], w=[Kraw.d])
            act.op(lambda e: e.activation(out=Kraw[:, 512:768], in_=pb[2][:, 0:256], func=AF.Copy), r=[pb[2].d], w=[Kraw.d])
            dve.op(lambda e: e.tensor_scalar(out=zt[:], in0=zt[:], scalar1=1.0, scalar2=None, op0=ALU.add), r=[zt.d], w=[zt.d])
            act.op(lambda e: e.activation(out=zt[:], in_=zt[:], func=AF.Ln), r=[zt.d], w=[zt.d])
            dve.op(lambda e: e.tensor_scalar(out=va[:, 0:4, 0:64], in0=pb[2][:, 256:512].rearrange("p (h d) -> p h d", d=64),
                                             scalar1=vflag, scalar2=None, op0=ALU.mult), r=[pb[2].d, pc.d], w=[va.d])
            dve.op(lambda e: e.tensor_scalar(out=va[:, 4:12, 0:64], in0=pb[3][:, 0:512].rearrange("p (h d) -> p h d", d=64),
                                             scalar1=vflag, scalar2=None, op0=ALU.mult), r=[pb[3].d, pc.d], w=[va.d])
            dve.op(lambda e: e.tensor_scalar(out=va[:, :, 64], in0=ones_f[:, 0:12], scalar1=vflag, scalar2=None, op0=ALU.mult),
                   r=[ones_f.d, pc.d], w=[va.d])
            sp.op(lambda e: e.dma_start(out=V_d[i], in_=va[:].rearrange("p h d -> p (h d)")), r=[va.d], w=[V_d.d], dma=True)
            dve.op(lambda e: e.tensor_scalar(out=ls[:], in0=zt[:], scalar1=nvflag, scalar2=None, op0=ALU.mult), r=[zt.d, pc.d], w=[ls.d])
            if own_(i):
                act.op(lambda e: e.activation(out=Qraw[:, 0:512], in_=pb[5][:, 0:512], func=AF.Copy), r=[pb[5].d], w=[Qraw.d])
                act.op(lambda e: e.activation(out=Qraw[:, 512:768], in_=pb[6][:, 0:256], func=AF.Copy), r=[pb[6].d], w=[Qraw.d])
                act.op(lambda e: e.activation(out=gv[:], in_=pb[6][:, 256:512], func=AF.Gelu_apprx_tanh), r=[pb[6].d], w=[gv.d])
                act.op(lambda e: e.activation(out=guT[b2][:], in_=pb[7][:, 0:256], func=AF.Gelu_apprx_tanh), r=[pb[7].d], w=[guT[b2].d])

        def S4(i):
            b2 = i % 2
            Fcur, Fprev = Fc[b2], Fc[1 - b2]
            ka, qa = kaug[b2], qaug[b2]
            pe.op(lambda e: e.matmul(pb[4][:, 16:28], lhsT=U_f, rhs=ls[:], start=True, stop=(i == 0)), r=[cst32.d, ls.d], w=[pb[4].d])
            if i > 0:
                pe.op(lambda e: e.matmul(pb[4][:, 16:28], lhsT=sel127_f, rhs=Fprev[:], start=False, stop=True), r=[cst32.d, Fprev.d], w=[pb[4].d])
            dve.op(lambda e: e.tensor_copy(out=Fcur[:], in_=pb[4][:, 16:28]), r=[pb[4].d], w=[Fcur.d])
            dve.op(lambda e: e.tensor_scalar(out=ka[:, :, 67], in0=Fcur[:], scalar1=-1.0, scalar2=None, op0=ALU.mult), r=[Fcur.d], w=[ka.d])
            dve.op(lambda e: e.tensor_tensor(out=r1[:], in0=Fcur[:], in1=ka[:, :, 67], op=ALU.add), r=[Fcur.d, ka.d], w=[r1.d])
            dve.op(lambda e: e.tensor_scalar(out=ka[:, :, 68], in0=r1[:], scalar1=-1.0, scalar2=None, op0=ALU.mult), r=[r1.d], w=[ka.d])
            dve.op(lambda e: e.tensor_tensor(out=r1[:], in0=r1[:], in1=ka[:, :, 68], op=ALU.add), r=[r1.d, ka.d], w=[r1.d])
            dve.op(lambda e: e.tensor_scalar(out=ka[:, :, 69], in0=r1[:], scalar1=-1.0, scalar2=None, op0=ALU.mult), r=[r1.d], w=[ka.d])
            if own_(i):
                dve.op(lambda e: e.tensor_scalar(out=qa[:, :, 64:67], in0=ka[:, :, 67:70], scalar1=-1.0, scalar2=None, op0=ALU.mult), r=[ka.d], w=[qa.d])

        def S5(i):
            b2 = i % 2
            ka, qa = kaug[b2], qaug[b2]
            own = own_(i)
            pool.op(lambda e: e.tensor_tensor(out=sq[:], in0=Kraw[:], in1=Kraw[:], op=ALU.mult), r=[Kraw.d], w=[sq.d])
            dve.op(lambda e: e.tensor_reduce(out=ssall[:, 0:12], in_=sq[:].rearrange("p (h d) -> p h d", d=64), axis=AX.X, op=ALU.add), r=[sq.d], w=[ssall.d])
            if own:
                pool.op(lambda e: e.tensor_tensor(out=sq[:], in0=Qraw[:], in1=Qraw[:], op=ALU.mult), r=[Qraw.d], w=[sq.d])
                dve.op(lambda e: e.tensor_reduce(out=ssall[:, 12:24], in_=sq[:].rearrange("p (h d) -> p h d", d=64), axis=AX.X, op=ALU.add), r=[sq.d], w=[ssall.d])
                pool.op(lambda e: e.tensor_tensor(out=gv2[:], in0=gv[:], in1=gv[:], op=ALU.mult), r=[gv.d], w=[gv2.d])
                dve.op(lambda e: e.tensor_reduce(out=ssall[:, 24:28], in_=gv2[:].rearrange("p (g d) -> p g d", d=64), axis=AX.X, op=ALU.add), r=[gv2.d], w=[ssall.d])
            rstd_from_ss(S, ssall, 64, 1.0, 28 if own else 12)
            dve.op(lambda e: e.tensor_tensor(out=ka[:, :, 0:64], in0=Kraw[:].rearrange("p (h d) -> p h d", d=64),
                                             in1=ssall[:, 0:12].unsqueeze(2).to_broadcast([128, 12, 64]), op=ALU.mult), r=[Kraw.d, ssall.d], w=[ka.d])
            if not own:
                return
            dve.op(lambda e: e.tensor_tensor(out=qtmp[:].rearrange("p (h d) -> p h d", d=64), in0=Qraw[:].rearrange("p (h d) -> p h d", d=64),
                                             in1=ssall[:, 12:24].unsqueeze(2).to_broadcast([128, 12, 64]), op=ALU.mult), r=[Qraw.d, ssall.d], w=[qtmp.d])
            dve.op(lambda e: e.tensor_tensor(out=qa[:, :, 0:64], in0=qtmp[:].rearrange("p (h d) -> p h d", d=64),
                                             in1=gqk[:].unsqueeze(1).to_broadcast([128, 12, 64]), op=ALU.mult), r=[qtmp.d, gqk.d], w=[qa.d])
            for g in range(4):
                off = 64 * (g % 2)
                dve.op(lambda e: e.tensor_scalar(out=vpad[:, g, off:off + 64], in0=gv[:, g * 64:(g + 1) * 64], scalar1=ssall[:, 24 + g:25 + g], scalar2=None,
                                                 op0=ALU.mult), r=[gv.d, ssall.d], w=[vpad.d])

        def S6(i):
            b2 = i % 2
            j = i - NPRE
            ka, qa = kaug[b2], qaug[b2]
            for h in range(12):
                bk = 1 if h < 8 else 2
                pe.op(lambda e: e.transpose(out=pbf(bk)[0:70, h % 8, :], in_=ka[:, h, :], identity=ident_b), r=[ka.d, cstb.d], w=[pb[bk].d])
            ks = kTs[b2]
            act.op(lambda e: e.copy(out=ks[:, 0:8, :], in_=pbf(1)[0:70, :, :]), r=[pb[1].d], w=[ks.d])
            dve.op(lambda e: e.tensor_copy(out=ks[:, 8:12, :], in_=pbf(2)[0:70, 0:4, :]), r=[pb[2].d], w=[ks.d])
            sp.op(lambda e: e.dma_start(out=kT_d[:, :, i * 128:(i + 1) * 128].rearrange("h r t -> r h t"), in_=ks[:]), r=[ks.d], w=[kT_d.d], dma=True)
            if not own_(i):
                return
            for h in range(12):
                bk = 5 if h < 8 else 6
                pe.op(lambda e: e.transpose(out=pbf(bk)[0:70, h % 8, :], in_=qa[:, h, :], identity=ident_b), r=[qa.d, cstb.d], w=[pb[bk].d])
            qs = qTs[b2]
            dve.op(lambda e: e.tensor_copy(out=qs[:, 0:8, :], in_=pbf(5)[0:70, :, :]), r=[pb[5].d], w=[qs.d])
            dve.op(lambda e: e.tensor_copy(out=qs[:, 8:12, :], in_=pbf(6)[0:70, 0:4, :]), r=[pb[6].d], w=[qs.d])
            sp.op(lambda e: e.dma_start(out=qT_d[:, :, j * 128:(j + 1) * 128].rearrange("h r t -> r h t"), in_=qs[:]), r=[qs.d], w=[qT_d.d], dma=True)
            for c in range(2):
                for gg in range(2):
                    g = 2 * c + gg
                    pe.op(lambda e: e.matmul(pb[4][:, 128 + c * 128:128 + (c + 1) * 128], lhsT=vpad[:, g, :], rhs=wsT[:, g, :], start=(gg == 0), stop=(gg == 1)),
                          r=[vpad.d, wsT.d], w=[pb[4].d])
            gu = guT[b2]
            for c in range(2):
                dve.op(lambda e: e.scalar_tensor_tensor(out=stmp[:, c * 128:(c + 1) * 128], in0=pb[4][:, 128 + c * 128:128 + (c + 1) * 128], scalar=gvcol[:, c:c + 1],
                                                        in1=bsl[:, c * 128:(c + 1) * 128], op0=ALU.mult, op1=ALU.add), r=[pb[4].d, sm.d], w=[stmp.d])
                dve.op(lambda e: e.tensor_tensor(out=mixT[:, c, j * 128:(j + 1) * 128], in0=stmp[:, c * 128:(c + 1) * 128], in1=gu[:, c * 128:(c + 1) * 128],
                                                 op=ALU.mult), r=[stmp.d, gu.d], w=[mixT.d])

        S1a(0)
        S1a(1)
        for (c0_, c1_) in ((0, 640), (640, 1280)):
            pool.op(lambda e: e.dma_start(out=wev[:, :, c0_:c1_], in_=ev_w_in_d[:, c0_:c1_].rearrange("(c p) n -> p c n", p=128)), w=[wev_q], dma=True)
        S1b(0)
        S2(0)
        S3(0)
        for i in range(NSEQ):
            if i < NT:
                sp.op(lambda e: e.dma_start(out=xres[:, i, :], in_=xo_d[i * 128:(i + 1) * 128, :]), w=[xres_d[i]], dma=True)
            if i + 2 < NSEQ:
                S1a(i + 2)
            if i + 1 < NSEQ:
                S1b(i + 1)
            S4(i)
            if i + 1 < NSEQ:
                S2(i + 1)
            S5(i)
            if i + 1 < NSEQ:
                S3(i + 1)
            S6(i)
        S.close()
        if CUT == 10:
            SL.close(); return

        SBC = Scope(kb)
        wo_ev = SBC.sb([128, 8, D], BF16)
        load_w_bf16(wo_ev, ev_w_out_d, 8, D)
        S = Scope(kb)
        kTh = [S.sb([70, NSEQ * 128], BF16) for _ in range(2)]
        qTh = [S.sb([70, NT * 128], BF16) for _ in range(2)]
        Vg = [S.sb([128, NSEQ, 324], BF16) for _ in range(2)]
        for b_ in range(2):
            pool.op(lambda e: e.memset(Vg[b_][:, :, 260:324], 0.0), w=[Vg[b_].d])
        PT = [S.sb([128, 1024], BF16) for _ in range(3)]
        rc = [S.sb([128, 512], F32) for _ in range(2)]
        bcs = [S.sb([64, 512], F32) for _ in range(2)]
        otmp = [S.sb([64, 512], BF16) for _ in range(2)]
        QC = [(0, 128), (128, 512), (640, 512), (1152, 512), (1664, 512)]
        STP = [pbig[0], pbig[1], pbig[2]]
        STD = [Dep(), Dep(), Dep()]
        for k_ in range(3):
            pb[2 * k_].d = STD[k_]
            pb[2 * k_ + 1].d = STD[k_]
        free_slots = [0, 1, 2]

        def take_slot():
            return free_slots.pop(0)

        def give_slot(k_):
            free_slots.append(k_)

        def load_head(h):
            b = h % 2
            sp.op(lambda e: e.dma_start(out=kTh[b][:], in_=kT_d[h]), r=[kT_d.d], w=[kTh[b].d], dma=True)
            sp.op(lambda e: e.dma_start(out=qTh[b][:], in_=qT_d[h]), r=[qT_d.d], w=[qTh[b].d], dma=True)

        def load_vgroup(g):
            b = g % 2
            sp.op(lambda e: e.dma_start(out=Vg[b][:, :, 0:260], in_=V_d[:, :, g * 260:(g + 1) * 260].rearrange("t p c -> p t c")),
                  r=[V_d.d], w=[Vg[b].d], dma=True)

        load_head(0)
        load_vgroup(0)
        load_head(1)
        load_vgroup(1)
        units = []
        nchunk = 0
        for h in range(12):
            for (q0, nq) in QC:
                jlo = q0 // 128
                jhi = jlo + nq // 128 - 1
                last = NPRE + jhi
                if nq == 128:
                    for k0 in range(0, last + 1, 4):
                        subs = [(kt, (kt - k0) * 128, 0, 128, kt == last) for kt in range(k0, k0 + 4)]
                        units.append((h, q0, nq, last, nchunk, subs, 0, 512))
                else:
                    nfull = NPRE + jlo
                    for kt in range(0, nfull, 2):
                        units.append((h, q0, nq, last, nchunk, [(kt, 0, 0, 512, False), (kt + 1, 512, 0, 512, False)], 0, 1024))
                    for dj in range(4):
                        units.append((h, q0, nq, last, nchunk, [(nfull + dj, 0, dj * 128, 512, True)], dj * 128, 512))
                nchunk += 1
        NU = len(units)
        deferred = []
        uslot = {}

        def emit_qk(ui):
            h, q0, nq, last, ck, subs, lo, hi = units[ui]
            kt_, qt_ = kTh[h % 2], qTh[h % 2]
            k_ = take_slot()
            uslot[ui] = k_
            ST, sd = STP[k_], STD[k_]
            for (kt, off, c0, n, diag) in subs:
                lhs = kt_[:, kt * 128:(kt + 1) * 128]
                if diag:
                    pe.op(lambda e: e.matmul(ST[:, off + c0:off + c0 + 128], lhsT=lhs, rhs=qt_[:, q0 + c0:q0 + c0 + 128], start=True, stop=False),
                          r=[kt_.d, qt_.d], w=[sd])
                    pe.op(lambda e: e.matmul(ST[:, off + c0:off + c0 + 128], lhsT=ident_b, rhs=maskT_b, start=False, stop=True),
                          r=[cstb.d], w=[sd])
                    if c0 + 128 < n:
                        pe.op(lambda e: e.matmul(ST[:, off + c0 + 128:off + n], lhsT=lhs, rhs=qt_[:, q0 + c0 + 128:q0 + n], start=True, stop=True),
                              r=[kt_.d, qt_.d], w=[sd])
                else:
                    pe.op(lambda e: e.matmul(ST[:, off + c0:off + n], lhsT=lhs, rhs=qt_[:, q0 + c0:q0 + n], start=True, stop=True),
                          r=[kt_.d, qt_.d], w=[sd])
            if ui + 1 == NU or units[ui + 1][0] != h:
                if h + 2 < 12:
                    load_head(h + 2)

        def emit_exp_pv(ui):
            h, q0, nq, last, ck, subs, lo, hi = units[ui]
            k_ = uslot[ui]
            ST, sd = STP[k_], STD[k_]
            ptb = PT[ui % 3]
            OB = pb[6 + ck % 2]
            vg_ = Vg[(h // 4) % 2]
            hv = (h % 4) * 65
            act.op(lambda e: e.activation(out=ptb[:, lo:hi], in_=ST[:, lo:hi], func=AF.Exp), r=[sd], w=[ptb.d])
            give_slot(k_)
            for (kt, off, c0, n, diag) in subs:
                pe.op(lambda e: e.matmul(OB[:, c0:n], lhsT=vg_[:, kt, hv:hv + 128], rhs=ptb[:, off + c0:off + n], start=(kt == 0), stop=(kt == last),
                                         skip_group_check=True), r=[vg_.d, ptb.d], w=[OB.d])
            if subs[-1][0] == last:
                if h == 3 and q0 == QC[-1][0]:
                    load_vgroup(2)
                k2 = ck % 2
                rc_, bcs_, ot_ = rc[k2], bcs[k2], otmp[k2]
                ch = 2 + h // 2
                dve.op(lambda e: e.tensor_scalar(out=rc_[64:65, 0:nq], in0=OB[64:65, 0:nq], scalar1=1e-30, scalar2=None, op0=ALU.add), r=[OB.d], w=[rc_.d])
                dve.op(lambda e: e.reciprocal(out=rc_[64:65, 0:nq], in_=rc_[64:65, 0:nq]), r=[rc_.d], w=[rc_.d])

                def stage2():
                    kk = take_slot()
                    give_slot(kk)
                    BB, bd = STP[kk], STD[kk]
                    pe.op(lambda e: e.matmul(BB[0:64, 0:nq], lhsT=ones_f[64:65, 0:64], rhs=rc_[64:65, 0:nq], start=True, stop=True), r=[ones_f.d, rc_.d], w=[bd])
                    dve.op(lambda e: e.tensor_copy(out=bcs_[:, 0:nq], in_=BB[0:64, 0:nq]), r=[bd], w=[bcs_.d])
                    if h % 2 == 0:
                        dve.op(lambda e: e.tensor_tensor(out=mixT[0:64, ch, q0:q0 + nq], in0=OB[0:64, 0:nq], in1=bcs_[:, 0:nq], op=ALU.mult),
                               r=[OB.d, bcs_.d], w=[mixT.d])
                    else:
                        dve.op(lambda e: e.tensor_tensor(out=ot_[:, 0:nq], in0=OB[0:64, 0:nq], in1=bcs_[:, 0:nq], op=ALU.mult),
                               r=[OB.d, bcs_.d], w=[ot_.d])

                def stage3():
                    if h % 2 == 1:
                        kk = take_slot()
                        give_slot(kk)
                        SB_, bd = STP[kk], STD[kk]
                        pe.op(lambda e: e.matmul(SB_[:, 0:nq], lhsT=shift_b, rhs=ot_[:, 0:nq], start=True, stop=True), r=[cstb.d, ot_.d], w=[bd])
                        dve.op(lambda e: e.tensor_copy(out=mixT[64:128, ch, q0:q0 + nq], in_=SB_[64:128, 0:nq]), r=[bd], w=[mixT.d])
                deferred.append((ui + 2, stage2))
                deferred.append((ui + 4, stage3))

        LA = 2
        for i in range(NU + LA):
            if i < NU:
                emit_qk(i)
            j = i - LA
            if j >= 0:
                emit_exp_pv(j)
            while deferred and deferred[0][0] <= j:
                deferred.pop(0)[1]()
        for _, fn in deferred:
            fn()
        for k_ in range(6):
            pb[k_].d = Dep()
        S.close()
        S = Scope(kb)
        wout_phase(S, mixT, mixT.d, ev_w_out_d, list(range(NT)), lambda j: j * 128, wo=wo_ev)
        S.close()
        SBC.close()
        SL.close()

    def odd_mixer():
        SL = Scope(kb)
        NTOK = 16 * 128
        mixT = SL.sb([128, 8, NTOK], BF16, "mixT1")
        wo_od = SL.sb([128, 8, D], BF16)
        S = Scope(kb)
        wod = S.sb([128, 8, 2048], BF16)
        load_w_bf16(wod, od_w_in_d, 8, 2048, colsplit=512)
        load_w_bf16(wo_od, od_w_out_d, 8, D)
        wpl = S.sb([128, 4, 128], BF16)
        xnT = S.sb([128, 8, NT * 128], BF16)
        xnT_d = [Dep() for _ in range(NT)]
        S1s = Scope(kb)
        wpl32 = S1s.sb([128, 4, 128], F32)
        sp.op(lambda e: e.dma_start(out=wpl32[:], in_=od_w_pool_d.rearrange("g c d -> c g d")), w=[wpl32.d], dma=True)
        dve.op(lambda e: e.tensor_copy(out=wpl[:], in_=wpl32[:]), r=[wpl32.d], w=[wpl.d])
        gn = S1s.sb([128, D], F32)
        sp.op(lambda e: e.dma_start(out=gn[:], in_=gains_d[2]), w=[gn.d], dma=True)
        ss2 = [S1s.sb([128, 4], F32) for _ in range(2)]
        xs2 = [S1s.sb([128, D], BF16) for _ in range(2)]
        nt_sequence(lambda j: (xres[:, j, :], xres_d[j]), NT, gn, ss2, xs2, lambda j: (xnT[:, :, j * 128:(j + 1) * 128], xnT_d[j]))
        S1s.close()
        if CUT <= 1:
            S.close(); SL.close(); return
        HL = 16
        W = HL + 512
        zb = [S.sb([128, W], F32) for _ in range(2)]
        pbuf = [S.sb([128, W], F32) for _ in range(2)]
        bgs = [S.sb([128, 512], BF16) for _ in range(2)]
        zc = S.sb([128, 4, HL], F32)
        pcar = S.sb([128, 4, HL], F32)
        cgs = S.sb([128, 512], BF16)
        y = S.sb([128, 512], F32)
        s1 = S.sb([128, W], F32)
        s2 = S.sb([128, W], F32)
        pooled = cgs
        dve.op(lambda e: e.memset(s1[:], 0.0), w=[s1.d])
        dve.op(lambda e: e.memset(s2[:], 0.0), w=[s2.d])
        groups = [(128 - HL, HL)] + [(128 + 512 * i, 512) for i in range(4)]
        work = [(gi, k) for gi in range(len(groups)) for k in range(4)]
        PB7 = [0, 1, 2, 3, 5, 6, 7]
        rot = [0]
        state = {}

        def nextbank():
            bk = PB7[rot[0] % 7]
            rot[0] += 1
            return bk

        def Pstage(n):
            gi, k = work[n]
            tok0, nn_ = groups[gi]
            xd = [xnT_d[i] for i in range(tok0 // 128, (tok0 + nn_ - 1) // 128 + 1)]
            banks = {}
            cols = [("cg", 512 + k * 128), ("hc", 1024 + k * 128), ("p", 1536 + k * 128)]
            if gi > 0:
                cols.append(("bg", k * 128))
            for nm, col in cols:
                bk = nextbank()
                banks[nm] = bk
                for c in range(8):
                    pe.op(lambda e: e.matmul(pb[bk][:, 0:nn_], lhsT=wod[:, c, col:col + 128], rhs=xnT[:, c, tok0:tok0 + nn_],
                                             start=(c == 0), stop=(c == 7)), r=[wod.d] + xd, w=[pb[bk].d])
            state[n] = banks

        def Estage(n):
            gi, k = work[n]
            tok0, nn_ = groups[gi]
            banks = state[n]
            z, p = zb[n % 2], pbuf[n % 2]
            act.op(lambda e: e.copy(out=cgs[:, 0:nn_], in_=pb[banks["cg"]][:, 0:nn_]), r=[pb[banks["cg"]].d], w=[cgs.d])
            if gi == 0:
                dve.op(lambda e: e.tensor_tensor(out=zc[:, k, :], in0=cgs[:, 0:nn_], in1=pb[banks["hc"]][:, 0:nn_], op=ALU.mult),
                       r=[cgs.d, pb[banks["hc"]].d], w=[zc.d])
                act.op(lambda e: e.copy(out=pcar[:, k, :], in_=pb[banks["p"]][:, 0:nn_]), r=[pb[banks["p"]].d], w=[pcar.d])
                return
            act.op(lambda e: e.copy(out=p[:, HL:W], in_=pb[banks["p"]][:, 0:nn_]), r=[pb[banks["p"]].d], w=[p.d])
            act.op(lambda e: e.copy(out=bgs[n % 2][:], in_=pb[banks["bg"]][:, 0:nn_]), r=[pb[banks["bg"]].d], w=[bgs[n % 2].d])
            dve.op(lambda e: e.tensor_tensor(out=z[:, HL:W], in0=cgs[:, 0:nn_], in1=pb[banks["hc"]][:, 0:nn_], op=ALU.mult),
                   r=[cgs.d, pb[banks["hc"]].d], w=[z.d])

        def Bstage(n):
            gi, k = work[n]
            if gi == 0:
                return
            tok0, nn_ = groups[gi]
            mcol = tok0 - 128
            z, p = zb[n % 2], pbuf[n % 2]
            dve.op(lambda e: e.tensor_copy(out=z[:, 0:HL], in_=zc[:, k, :]), r=[zc.d], w=[z.d])
            dve.op(lambda e: e.tensor_copy(out=p[:, 0:HL], in_=pcar[:, k, :]), r=[pcar.d], w=[p.d])
            dve.op(lambda e: e.tensor_copy(out=zc[:, k, :], in_=z[:, W - HL:W]), r=[z.d], w=[zc.d])
            dve.op(lambda e: e.tensor_copy(out=pcar[:, k, :], in_=p[:, W - HL:W]), r=[p.d], w=[pcar.d])
            dve.op(lambda e: e.tensor_scalar(out=y[:], in0=z[:, HL:W], scalar1=cw[:, k * 3 + 2:k * 3 + 3], scalar2=None, op0=ALU.mult),
                   r=[z.d, sm.d], w=[y.d])
            dve.op(lambda e: e.scalar_tensor_tensor(out=y[:], in0=z[:, HL - 1:W - 1], scalar=cw[:, k * 3 + 1:k * 3 + 2], in1=y[:], op0=ALU.mult, op1=ALU.add),
                   r=[z.d, sm.d, y.d], w=[y.d])
            dve.op(lambda e: e.scalar_tensor_tensor(out=y[:], in0=z[:, HL - 2:W - 2], scalar=cw[:, k * 3:k * 3 + 1], in1=y[:], op0=ALU.mult, op1=ALU.add),
                   r=[z.d, sm.d, y.d], w=[y.d])
            dve.op(lambda e: e.tensor_tensor(out=mixT[:, k, mcol:mcol + 512], in0=y[:], in1=bgs[n % 2][:], op=ALU.mult), r=[y.d, bgs[n % 2].d], w=[mixT.d])
            src, sd = p[:, :], p.d
            bufs = [s1, s2]
            sh = 1
            for m in range(k + 1):
                dst = bufs[m % 2]
                pool.op(lambda e: e.tensor_tensor(out=dst[:, sh:W], in0=src[:, sh:W], in1=src[:, 0:W - sh], op=ALU.add),
                        r=[sd], w=[dst.d])
                src, sd = dst[:, :], dst.d
                sh *= 2
            win = 2 ** (k + 1)
            dve.op(lambda e: e.scalar_tensor_tensor(out=pooled[:], in0=src[:, HL:W], scalar=1.0 / win, in1=p[:, HL:W], op0=ALU.mult, op1=ALU.subtract),
                   r=[sd, p.d], w=[pooled.d])
            if gi == 1:
                dve.op(lambda e: e.tensor_tensor(out=y[:, 0:16], in0=src[:, HL:HL + 16], in1=icnt[:, k * 16:(k + 1) * 16], op=ALU.mult),
                       r=[sd, pc.d, y.d], w=[y.d])
                dve.op(lambda e: e.tensor_tensor(out=pooled[:, 0:16], in0=y[:, 0:16], in1=p[:, HL:HL + 16], op=ALU.subtract),
                       r=[y.d, p.d], w=[pooled.d])
            pe.op(lambda e: e.matmul(pb[4][:, 0:512], lhsT=wpl[:, k, :], rhs=pooled[:], start=True, stop=True), r=[wpl.d, pooled.d], w=[pb[4].d])
            act.op(lambda e: e.activation(out=mixT[:, 4 + k, mcol:mcol + 512], in_=pb[4][:, 0:512], func=AF.Copy, scale=pscale[:, k:k + 1]),
                   r=[pb[4].d, sm.d], w=[mixT.d])

        NW = len(work)
        Pstage(0)
        Estage(0)
        for n in range(NW):
            if n + 1 < NW:
                Pstage(n + 1)
            Bstage(n)
            if n + 1 < NW:
                Estage(n + 1)
        S.close()
        if CUT <= 4:
            SL.close(); return
        S = Scope(kb)
        wout_phase(S, mixT, mixT.d, od_w_out_d, list(range(1, NT)), lambda j: (j - 1) * 128, wo=wo_od)
        S.close()
        SL.close()

    if L0:
        even_mixer()
        if dbg == "m0":
            dbg_dump()
        if CUT != 10:
            moe_layer(0, list(range(NT)))
        if dbg == "e0":
            dbg_dump()
    if L1:
        if not L0:
            load_xres()
        if CUT > 0:
            odd_mixer()
        if dbg == "m1":
            dbg_dump()
        out_evs = []
        def store_tile(j):
            out_evs.append(sp.op(lambda e: e.dma_start(out=out_d[(j - 1) * 128:j * 128, :], in_=xres[:, j, :]), r=[xres_d[j]], dma=True))
        if CUT > 5:
            moe_layer(1, list(range(1, NT)), on_final=store_tile)
    evs = []
    if L1:
        if CUT <= 5:
            for j in range(1, NT):
                evs.append(sp.op(lambda e: e.dma_start(out=out_d[(j - 1) * 128:j * 128, :], in_=xres[:, j, :]), r=[xres_d[j]], dma=True))
    else:
        for j in range(NT):
            evs.append(sp.op(lambda e: e.dma_start(out=out_d[j * 128:(j + 1) * 128, :], in_=xres[:, j, :]), r=[xres_d[j]], dma=True))
    kb.barrier([sp])
    return nc, kb


def _consts():
    cst = np.zeros((128, 640), np.float32)
    idx = np.arange(128)
    cst[:, 0:128] = np.eye(128, dtype=np.float32)
    cst[:, 128:256] = (idx[:, None] <= idx[None, :]).astype(np.float32)
    cst[127, 256:384] = 1.0
    cst[:, 384:512] = np.where(idx[:, None] <= idx[None, :], 0.0, NEG)
    cst[np.arange(64), 512 + 64 + np.arange(64)] = 1.0
    selall = np.zeros((16, 2048), np.float32)
    for e in range(16):
        selall[e, e * 128:(e + 1) * 128] = 1.0
    return cst, selall


def _percore(hf):
    pc = np.zeros((128, 68), np.float32)
    v = 1.0 if hf == 1 else 0.0
    pc[:, 0] = v
    pc[:, 1] = -v
    pc[:, 2] = 1.0
    pc[:, 3] = -1.0
    t = np.arange(16)
    for k, win in enumerate((2, 4, 8, 16)):
        cnt = np.minimum(t + 1, win) if hf == 0 else np.full(16, win)
        pc[:, 4 + k * 16:4 + (k + 1) * 16] = (1.0 / cnt)[None, :]
    return pc


def _make_inputs(inputs):
    f = lambda a: np.ascontiguousarray(np.asarray(a, dtype=np.float32))
    x = f(inputs["x"])
    cst, selall = _consts()
    p = np.arange(128)
    sm = np.zeros((128, 454), np.float32)
    sm[:, 0:12] = f(inputs["ev_b_forget"])[0][None, :]
    sm[:, 12:76] = f(inputs["ev_g_q"])[0][None, :]
    sm[:, 76:140] = f(inputs["ev_g_k"])[0][None, :]
    gv = f(inputs["ev_g_v"])[0]
    bs = f(inputs["ev_b_s"])[0]
    for c in range(2):
        sm[:, 140 + c] = gv[2 * c + p // 64, p % 64]
        sm[:, 142 + c * 128:142 + (c + 1) * 128] = bs[2 * c + p // 64, :]
    cwv = f(inputs["od_conv_w"])[0]
    for k in range(4):
        for tap in range(3):
            sm[:, 398 + k * 3 + tap] = cwv[tap, k * 128 + p]
        sm[:, 410 + k] = f(inputs["od_pool_scale"])[0][k * 128 + p]
    bg = f(inputs["moe_b_group"])
    br = f(inputs["moe_b_router"]).reshape(2, 16)
    for l in range(2):
        sm[:, 414 + l * 20:414 + l * 20 + 4] = bg[l][None, :]
        sm[:, 414 + l * 20 + 4:414 + (l + 1) * 20] = br[l][None, :]
    gains = np.stack([np.broadcast_to(f(inputs["ev_norm"])[0], (128, D)), np.broadcast_to(f(inputs["moe_norm"])[0], (128, D)),
                      np.broadcast_to(f(inputs["od_norm"])[0], (128, D)), np.broadcast_to(f(inputs["moe_norm"])[1], (128, D))]).astype(np.float32)
    wr = np.concatenate([f(inputs["moe_w_group"]), f(inputs["moe_w_router"]).reshape(2, D, 16)], axis=2)
    shared = {
        "cst": cst, "selall": selall, "smalls": sm, "gains": np.ascontiguousarray(gains),
        "ev_w_in": f(inputs["ev_w_in"])[0], "ev_wsT": np.ascontiguousarray(f(inputs["ev_w_s"])[0].transpose(0, 2, 1)),
        "ev_w_out": f(inputs["ev_w_out"])[0], "od_w_in": f(inputs["od_w_in"])[0], "od_w_pool": f(inputs["od_w_pool"])[0],
        "od_w_out": f(inputs["od_w_out"])[0], "wr": np.ascontiguousarray(wr),
        "moe_w_gate": f(inputs["moe_w_gate"]), "moe_w_up": f(inputs["moe_w_up"]), "moe_w_down": f(inputs["moe_w_down"]),
    }
    in_maps = []
    for c in range(8):
        b, hf = c // 2, c % 2
        m = dict(shared)
        m["pc"] = _percore(hf)
        if hf == 1:
            m["xp"] = np.ascontiguousarray(x[b, 0:NPRE * 128])
            m["xo"] = np.ascontiguousarray(x[b, NPRE * 128:4096])
        else:
            m["xp"] = np.zeros((NPRE * 128, D), np.float32)
            m["xo"] = np.ascontiguousarray(np.concatenate([np.zeros((128, D), np.float32), x[b, 0:2048]], axis=0))
        in_maps.append(m)
    return in_maps


_CACHE = {}


def kernel(**inputs):
    in_maps = _make_inputs(inputs)
    if "nc" not in _CACHE:
        _CACHE["nc"] = build((0, 1))[0]
    nc = _CACHE["nc"]
    res = run_bass_kernel_spmd(nc, in_maps, core_ids=list(range(8)))
    out = np.zeros((4, 4096, D), np.float32)
    for c in range(8):
        b, hf = c // 2, c % 2
        out[b, hf * 2048:(hf + 1) * 2048] = res.results[c]["out"]
    return out
```
